# Optimizing a Trainium2 kernel written in Bass

```python
import math, functools
import jax, jax.numpy as jnp
from jax import lax
import numpy as np

D_MODEL = 2048
BATCH = 16
SEQ = 2048
DEPTH = 2

MIX_WIDTH = D_MODEL
GDN_HEAD_DIM = 128
GDN_HEADS = MIX_WIDTH // (2 * GDN_HEAD_DIM)
GDN_W = GDN_HEADS * GDN_HEAD_DIM
RET_HEAD_DIM = 256
RET_HEADS = MIX_WIDTH // (2 * RET_HEAD_DIM)
RET_W = RET_HEADS * RET_HEAD_DIM
EV_IN_SIZES = (3 * GDN_W, GDN_HEADS, GDN_HEADS, GDN_W, RET_W, RET_W, RET_W, RET_W)
EV_IN_WIDTH = 4 * GDN_W + 2 * GDN_HEADS + 4 * RET_W
CHUNK = 64
CONV_WIDTH = 4
ROPE_BASE = 10000.0
LRU_WIDTH = D_MODEL
LRU_BLOCK = 256
LRU_BLOCKS = LRU_WIDTH // LRU_BLOCK
LRU_C = 8.0
D_FF = (11 * D_MODEL) // 4
N_EVEN = (DEPTH + 1) // 2
N_ODD = DEPTH // 2
N_SUB = 3
EPS = 1e-6

kernel_name = 'hybrid_gdn_retention_rglru_macaron_block'


def rms_norm(x, gain, eps=EPS):
    xf = x.astype(jnp.float32)
    y = xf * lax.rsqrt(jnp.mean(xf * xf, axis=-1, keepdims=True) + eps)
    return (y * gain.astype(jnp.float32)).astype(x.dtype)


def l2_normalize(x, eps=EPS):
    return x * lax.rsqrt(jnp.sum(x * x, axis=-1, keepdims=True) + eps)


def causal_depthwise_conv(x, w):
    width, t_len = w.shape[0], x.shape[1]
    xp = jnp.pad(x, ((0, 0), (width - 1, 0), (0, 0)))
    out = xp[:, 0:t_len] * w[0]
    for tap in range(1, width):
        out = out + xp[:, tap:tap + t_len] * w[tap]
    return out


def rotary(x, pos):
    half = x.shape[-1] // 2
    inv_freq = ROPE_BASE ** (-jnp.arange(half, dtype=jnp.float32) / half)
    ang = pos[:, None] * inv_freq[None, :]
    cos, sin = jnp.cos(ang)[None, :, None, :], jnp.sin(ang)[None, :, None, :]
    x1, x2 = x[..., :half], x[..., half:]
    return jnp.concatenate([x1 * cos - x2 * sin, x2 * cos + x1 * sin], axis=-1)


def heads_first(t, n_heads):
    b, t_len, _ = t.shape
    return t.reshape(b, t_len, n_heads, -1).transpose(0, 2, 1, 3)


def to_chunks(t, n_chunks):
    return t.reshape(t.shape[0], t.shape[1], n_chunks, CHUNK, t.shape[-1])


def chunk_gated_delta_rule(q, k, v, log_decay, beta):
    b, h, t_len, dk = q.shape
    dv = v.shape[-1]
    n = t_len // CHUNK
    q, k, v = to_chunks(q * dk ** -0.5, n), to_chunks(k, n), to_chunks(v, n)
    g = jnp.cumsum(log_decay.reshape(b, h, n, CHUNK), axis=-1)
    beta = beta.reshape(b, h, n, CHUNK)[..., None]
    idx = jnp.arange(CHUNK)
    causal = idx[:, None] >= idx[None, :]
    strict = idx[:, None] > idx[None, :]
    decay = jnp.exp(jnp.where(causal, g[..., :, None] - g[..., None, :], -jnp.inf))
    k_beta = k * beta
    a_mat = jnp.where(strict, jnp.einsum('bhnid,bhnjd->bhnij', k_beta, k) * decay, 0.0)
    rhs = jnp.concatenate([v * beta, k_beta * jnp.exp(g)[..., None]], axis=-1)
    sol = lax.linalg.triangular_solve(a_mat, rhs, left_side=True, lower=True, unit_diagonal=True)
    u, w = sol[..., :dv], sol[..., dv:]
    qk = jnp.where(causal, jnp.einsum('bhnid,bhnjd->bhnij', q, k) * decay, 0.0)
    g_last = g[..., -1:]
    q_st = q * jnp.exp(g)[..., None]
    k_st = k * jnp.exp(g_last - g)[..., None]
    chunk_decay = jnp.exp(g_last)[..., None]

    def step(state, inp):
        q_c, k_c, u_c, w_c, qk_c, dec_c = inp
        v_new = u_c - jnp.einsum('bhik,bhkv->bhiv', w_c, state)
        o = jnp.einsum('bhik,bhkv->bhiv', q_c, state) + jnp.einsum('bhij,bhjv->bhiv', qk_c, v_new)
        state = state * dec_c + jnp.einsum('bhjk,bhjv->bhkv', k_c, v_new)
        return state, o

    xs = tuple(jnp.moveaxis(t, 2, 0) for t in (q_st, k_st, u, w, qk, chunk_decay))
    _, o = lax.scan(step, jnp.zeros((b, h, dk, dv), q.dtype), xs)
    return jnp.moveaxis(o, 0, 2).reshape(b, h, t_len, dv)


def chunk_retention(q, k, v):
    b, h, t_len, dk = q.shape
    dv = v.shape[-1]
    n = t_len // CHUNK
    log_gamma = jnp.log1p(-jnp.exp2(-5.0 - jnp.arange(h, dtype=jnp.float32)))
    q, k, v = to_chunks(q, n), to_chunks(k * dk ** -0.5, n), to_chunks(v, n)
    idx = jnp.arange(CHUNK, dtype=jnp.float32)
    causal = idx[:, None] >= idx[None, :]
    intra_decay = jnp.exp(jnp.where(causal, (idx[:, None] - idx[None, :]) * log_gamma[:, None, None], -jnp.inf))
    scores = jnp.einsum('bhnid,bhnjd->bhnij', q, k) * intra_decay[:, None]
    o_intra = jnp.einsum('bhnij,bhnjd->bhnid', scores, v)
    lg4 = log_gamma[:, None, None, None]
    q_in = q * jnp.exp((idx[:, None] + 1.0) * lg4)
    k_st = k * jnp.exp((CHUNK - 1.0 - idx[:, None]) * lg4)
    chunk_decay = jnp.exp(CHUNK * log_gamma)[:, None, None]

    def step(state, inp):
        q_c, k_c, v_c = inp
        o = jnp.einsum('bhid,bhde->bhie', q_c, state)
        state = state * chunk_decay + jnp.einsum('bhjd,bhje->bhde', k_c, v_c)
        return state, o

    xs = tuple(jnp.moveaxis(t, 2, 0) for t in (q_in, k_st, v))
    _, o_inter = lax.scan(step, jnp.zeros((b, h, dk, dv), q.dtype), xs)
    return (o_intra + jnp.moveaxis(o_inter, 0, 2)).reshape(b, h, t_len, dv)


def gdn_retention_mixer(h, w_in, conv_w, a_log, dt_bias, o_norm, ret_norm, w_out):
    b, t_len, _ = h.shape
    f32 = jnp.float32
    cuts = [int(o) for o in np.cumsum(EV_IN_SIZES)[:-1]]
    qkv_a, beta_in, alpha_in, z_a, q_b, k_b, v_b, g_b = jnp.split(h @ w_in, cuts, axis=-1)
    qkv = jax.nn.silu(causal_depthwise_conv(qkv_a, conv_w)).astype(f32)
    q_a, k_a, v_a = [heads_first(t, GDN_HEADS) for t in jnp.split(qkv, 3, axis=-1)]
    q_a, k_a = l2_normalize(q_a), l2_normalize(k_a)
    beta = jax.nn.sigmoid(beta_in.astype(f32)).transpose(0, 2, 1)
    log_decay = (-jnp.exp(a_log.astype(f32)) * jax.nn.softplus(alpha_in.astype(f32) + dt_bias.astype(f32))).transpose(0, 2, 1)
    o_a = chunk_gated_delta_rule(q_a, k_a, v_a, log_decay, beta).transpose(0, 2, 1, 3)
    z = z_a.astype(f32).reshape(b, t_len, GDN_HEADS, GDN_HEAD_DIM)
    o_a = (rms_norm(o_a, o_norm) * jax.nn.silu(z)).reshape(b, t_len, GDN_W)
    pos = jnp.arange(t_len, dtype=f32)
    q_r = rotary(q_b.astype(f32).reshape(b, t_len, RET_HEADS, RET_HEAD_DIM), pos).transpose(0, 2, 1, 3)
    k_r = rotary(k_b.astype(f32).reshape(b, t_len, RET_HEADS, RET_HEAD_DIM), pos).transpose(0, 2, 1, 3)
    v_r = heads_first(v_b.astype(f32), RET_HEADS)
    o_b = chunk_retention(q_r, k_r, v_r).transpose(0, 2, 1, 3)
    gate_b = jax.nn.silu(g_b.astype(f32).reshape(b, t_len, RET_HEADS, RET_HEAD_DIM))
    o_b = (rms_norm(o_b, ret_norm.reshape(RET_HEADS, RET_HEAD_DIM)) * gate_b).reshape(b, t_len, RET_W)
    o = jnp.concatenate([o_a, o_b], axis=-1).astype(h.dtype)
    return o @ w_out


def rglru_mixer(h, w_in, conv_w, conv_b, gate_a_w, gate_a_b, gate_x_w, gate_x_b, lam, w_out):
    b, t_len, _ = h.shape
    f32 = jnp.float32
    y_branch, x_branch = jnp.split(h @ w_in, 2, axis=-1)
    y = jax.nn.gelu(y_branch, approximate=True)
    xc = (causal_depthwise_conv(x_branch, conv_w) + conv_b).astype(f32)
    xb = xc.reshape(b, t_len, LRU_BLOCKS, LRU_BLOCK)
    r = jax.nn.sigmoid(jnp.einsum('btni,nij->btnj', xb, gate_a_w.astype(f32)).reshape(b, t_len, LRU_WIDTH) + gate_a_b.astype(f32))
    i = jax.nn.sigmoid(jnp.einsum('btni,nij->btnj', xb, gate_x_w.astype(f32)).reshape(b, t_len, LRU_WIDTH) + gate_x_b.astype(f32))
    log_a = -LRU_C * r * jax.nn.softplus(-lam.astype(f32))
    a = jnp.exp(log_a)
    inp = jnp.sqrt(-jnp.expm1(2.0 * log_a)) * (i * xc)

    def combine(left, right):
        a_l, b_l = left
        a_r, b_r = right
        return a_l * a_r, a_r * b_l + b_r

    _, hs = lax.associative_scan(combine, (a, inp), axis=1)
    return (hs.astype(h.dtype) * y) @ w_out


def swiglu_ffn(h, w13, w2):
    gate, up = jnp.split(h @ w13, 2, axis=-1)
    return (jax.nn.silu(gate) * up) @ w2


def sandwich(x, fn, mod_s, g_pre, g_post, res_w):
    shift, scale, gate = mod_s[:, 0, None], mod_s[:, 1, None], mod_s[:, 2, None]
    h = rms_norm(x, g_pre) * (1.0 + scale) + shift
    return x + res_w * (1.0 + gate) * rms_norm(fn(h), g_post)


def setup_inputs(seed: int = 0) -> dict:
    key = jax.random.key(seed)
    ks = jax.random.split(key, 24)
    f32 = jnp.float32

    def nrm(k, shape, scale):
        return scale * jax.random.normal(k, shape, f32)

    dt = jnp.exp(jax.random.uniform(ks[11], (N_EVEN, GDN_HEADS), f32, math.log(1e-3), math.log(1e-1)))
    a_pow = jax.random.uniform(ks[22], (N_ODD, LRU_WIDTH), f32, 0.9, 0.999)
    s = a_pow ** (1.0 / LRU_C)
    return {
        'x': nrm(ks[0], (BATCH, SEQ, D_MODEL), 1.0),
        'c': nrm(ks[1], (BATCH, D_MODEL), 1.0),
        'ada_w': nrm(ks[2], (DEPTH, D_MODEL, N_SUB * 3 * D_MODEL), 0.5 * D_MODEL ** -0.5),
        'ada_b': nrm(ks[3], (DEPTH, N_SUB * 3 * D_MODEL), 0.01),
        'norm_pre': 1.0 + nrm(ks[4], (DEPTH, N_SUB, D_MODEL), 0.05),
        'norm_post': 1.0 + nrm(ks[5], (DEPTH, N_SUB, D_MODEL), 0.05),
        'ffn_w13': nrm(ks[6], (DEPTH, 2, D_MODEL, 2 * D_FF), D_MODEL ** -0.5),
        'ffn_w2': nrm(ks[7], (DEPTH, 2, D_FF, D_MODEL), D_FF ** -0.5),
        'ev_w_in': nrm(ks[8], (N_EVEN, D_MODEL, EV_IN_WIDTH), D_MODEL ** -0.5),
        'ev_conv_w': nrm(ks[9], (N_EVEN, CONV_WIDTH, 3 * GDN_W), CONV_WIDTH ** -0.5),
        'ev_a_log': jnp.log(jax.random.uniform(ks[10], (N_EVEN, GDN_HEADS), f32, 1.0, 16.0)),
        'ev_dt_bias': dt + jnp.log(-jnp.expm1(-dt)),
        'ev_o_norm': 1.0 + nrm(ks[12], (N_EVEN, GDN_HEAD_DIM), 0.05),
        'ev_ret_norm': 1.0 + nrm(ks[13], (N_EVEN, RET_W), 0.05),
        'ev_w_out': nrm(ks[14], (N_EVEN, MIX_WIDTH, D_MODEL), MIX_WIDTH ** -0.5),
        'od_w_in': nrm(ks[15], (N_ODD, D_MODEL, 2 * LRU_WIDTH), D_MODEL ** -0.5),
        'od_conv_w': nrm(ks[16], (N_ODD, CONV_WIDTH, LRU_WIDTH), CONV_WIDTH ** -0.5),
        'od_conv_b': nrm(ks[17], (N_ODD, LRU_WIDTH), 0.01),
        'od_gate_a_w': nrm(ks[18], (N_ODD, LRU_BLOCKS, LRU_BLOCK, LRU_BLOCK), LRU_BLOCK ** -0.5),
        'od_gate_a_b': nrm(ks[19], (N_ODD, LRU_WIDTH), 0.01),
        'od_gate_x_w': nrm(ks[20], (N_ODD, LRU_BLOCKS, LRU_BLOCK, LRU_BLOCK), LRU_BLOCK ** -0.5),
        'od_gate_x_b': nrm(ks[21], (N_ODD, LRU_WIDTH), 0.01),
        'od_lambda': jnp.log(s) - jnp.log1p(-s),
        'od_w_out': nrm(ks[23], (N_ODD, LRU_WIDTH, D_MODEL), LRU_WIDTH ** -0.5),
    }


def reference(x, c, ada_w, ada_b, norm_pre, norm_post, ffn_w13, ffn_w2,
              ev_w_in, ev_conv_w, ev_a_log, ev_dt_bias, ev_o_norm, ev_ret_norm, ev_w_out,
              od_w_in, od_conv_w, od_conv_b, od_gate_a_w, od_gate_a_b, od_gate_x_w, od_gate_x_b,
              od_lambda, od_w_out):
    b = x.shape[0]
    for layer in range(DEPTH):
        mod = (jax.nn.silu(c) @ ada_w[layer] + ada_b[layer]).reshape(b, N_SUB, 3, D_MODEL)
        ffn_pre = functools.partial(swiglu_ffn, w13=ffn_w13[layer, 0], w2=ffn_w2[layer, 0])
        ffn_post = functools.partial(swiglu_ffn, w13=ffn_w13[layer, 1], w2=ffn_w2[layer, 1])
        if layer % 2 == 0:
            e = layer // 2
            mixer = functools.partial(gdn_retention_mixer, w_in=ev_w_in[e], conv_w=ev_conv_w[e],
                                      a_log=ev_a_log[e], dt_bias=ev_dt_bias[e], o_norm=ev_o_norm[e],
                                      ret_norm=ev_ret_norm[e], w_out=ev_w_out[e])
        else:
            o = layer // 2
            mixer = functools.partial(rglru_mixer, w_in=od_w_in[o], conv_w=od_conv_w[o], conv_b=od_conv_b[o],
                                      gate_a_w=od_gate_a_w[o], gate_a_b=od_gate_a_b[o],
                                      gate_x_w=od_gate_x_w[o], gate_x_b=od_gate_x_b[o],
                                      lam=od_lambda[o], w_out=od_w_out[o])
        x = sandwich(x, ffn_pre, mod[:, 0], norm_pre[layer, 0], norm_post[layer, 0], 0.5)
        x = sandwich(x, mixer, mod[:, 1], norm_pre[layer, 1], norm_post[layer, 1], 1.0)
        x = sandwich(x, ffn_post, mod[:, 2], norm_pre[layer, 2], norm_post[layer, 2], 0.5)
    return x
```

```python
import numpy as np
import concourse.bass as bass
import concourse.mybir as mybir
from concourse.bass_utils import run_bass_kernel_spmd


EPOCH = 24000


class _St:
    __slots__ = ("lw", "rd", "excl")

    def __init__(self, excl):
        self.lw = None
        self.rd = {}
        self.excl = excl


class Buf:
    __slots__ = ("ap", "st", "name")

    def __init__(self, ap, name="", excl=False, st=None):
        self.ap = ap
        self.st = st if st is not None else _St(excl)
        self.name = name

    def view(self, ap):
        return Buf(ap, self.name, st=self.st)

    @property
    def lw(self):
        return self.st.lw

    @lw.setter
    def lw(self, v):
        self.st.lw = v

    @property
    def rd(self):
        return self.st.rd

    @rd.setter
    def rd(self, v):
        self.st.rd = v

    @property
    def excl(self):
        return self.st.excl

    def __getitem__(self, k):
        return self.ap[k]


class Track:
    def __init__(self, fw, name, step):
        self.fw = fw
        self.name = name
        self.step = step
        self.epoch = 0
        self.idx = 0
        self.sems = [fw._new_sem(f"{name}_e0")]

    def next_token(self):
        if (self.idx + 1) * self.step > EPOCH:
            self.epoch += 1
            self.idx = 0
            self.sems.append(self.fw._new_sem(f"{self.name}_e{self.epoch}"))
        self.idx += 1
        return (self, self.epoch, self.idx)


class Engine:
    def __init__(self, fw, name, strict_self):
        self.fw = fw
        self.name = name
        self.track = Track(fw, name, 1)
        self.ops = []
        self.seen = {}
        self.strict_self = strict_self


class FW:
    def __init__(self, nc):
        self.nc = nc
        self._stack = []
        self._semcount = 0
        self.eng = {
            "pe": Engine(self, "pe", False),
            "act": Engine(self, "act", True),
            "dve": Engine(self, "dve", True),
            "pool": Engine(self, "pool", True),
            "sp": Engine(self, "sp", True),
        }
        self.lanes = {}
        self.n_inst = 0

    def _new_sem(self, name):
        cm = self.nc.semaphore(name)
        s = cm.__enter__()
        self._stack.append(cm)
        self._semcount += 1
        return s

    def sbuf(self, name, shape, dtype):
        cm = self.nc.sbuf_tensor(name, list(shape), dtype)
        t = cm.__enter__()
        self._stack.append(cm)
        return t

    def psum(self, name, shape, dtype):
        cm = self.nc.psum_tensor(name, list(shape), dtype)
        t = cm.__enter__()
        self._stack.append(cm)
        return t

    def lane_pool(self, name, n):
        self.lanes[name] = dict(tracks=[Track(self, f"{name}{i}", 16) for i in range(n)], rr=0)

    def _waits(self, engine, reads, writes):
        need = {}
        for b in reads:
            if b.lw is not None:
                k = (b.lw[0], b.lw[1])
                need[k] = max(need.get(k, 0), b.lw[2])
        for b in writes:
            if b.lw is not None:
                k = (b.lw[0], b.lw[1])
                need[k] = max(need.get(k, 0), b.lw[2])
            for k, i in b.rd.items():
                need[k] = max(need.get(k, 0), i)
        out = []
        for k, i in need.items():
            tr, ep = k
            if tr is engine.track and not engine.strict_self:
                continue
            if engine.seen.get(k, 0) >= i:
                continue
            engine.seen[k] = i
            out.append((tr.sems[ep], i * tr.step))
        return out

    def _commit(self, tok, reads, writes):
        k = (tok[0], tok[1])
        for b in reads:
            b.rd[k] = max(b.rd.get(k, 0), tok[2])
        for b in writes:
            b.lw = tok
            b.rd = {}

    def op(self, ename, fn, reads=(), writes=()):
        e = self.eng[ename]
        if any(b.excl for b in reads):
            writes = list(writes) + [b for b in reads if b.excl]
            reads = [b for b in reads if not b.excl]
        waits = self._waits(e, reads, writes)
        tok = e.track.next_token()
        sem = tok[0].sems[tok[1]]
        e.seen[(tok[0], tok[1])] = max(e.seen.get((tok[0], tok[1]), 0), 0)

        def run(eng, waits=waits, fn=fn, sem=sem):
            for s, v in waits:
                eng.wait_ge(s, v)
            fn(eng).then_inc(sem, 1)
        e.ops.append(run)
        self._commit(tok, reads, writes)
        self.n_inst += 1
        return tok

    def dma(self, ename, pool, out_ap, in_ap, reads=(), writes=(), **kw):
        e = self.eng[ename]
        lp = self.lanes[pool]
        tr = lp["tracks"][lp["rr"] % len(lp["tracks"])]
        lp["rr"] += 1
        waits = self._waits(e, reads, writes)
        if tr.idx > 0:
            k = (tr, tr.epoch)
            if e.seen.get(k, 0) < tr.idx:
                e.seen[k] = tr.idx
                waits.append((tr.sems[tr.epoch], tr.idx * 16))
        tok = tr.next_token()
        sem = tr.sems[tok[1]]

        def run(eng, waits=waits, sem=sem, out_ap=out_ap, in_ap=in_ap, kw=kw):
            for s, v in waits:
                eng.wait_ge(s, v)
            eng.dma_start(out=out_ap, in_=in_ap, **kw).then_inc(sem, 16)
        e.ops.append(run)
        self._commit(tok, reads, writes)
        self.n_inst += 1
        return tok

    def final_wait(self, ename, bufs):
        e = self.eng[ename]
        waits = self._waits(e, bufs, ())

        def run(eng, waits=waits):
            for s, v in waits:
                eng.wait_ge(s, v)
        e.ops.append(run)

    def emit(self):
        nc = self.nc
        with nc.Block() as block:
            @block.tensor
            def _(eng):
                for f in self.eng["pe"].ops:
                    f(eng)

            @block.scalar
            def _(eng):
                for f in self.eng["act"].ops:
                    f(eng)

            @block.vector
            def _(eng):
                for f in self.eng["dve"].ops:
                    f(eng)

            @block.gpsimd
            def _(eng):
                for f in self.eng["pool"].ops:
                    f(eng)

            @block.sync
            def _(eng):
                for f in self.eng["sp"].ops:
                    f(eng)

    def close(self):
        while self._stack:
            self._stack.pop().__exit__(None, None, None)


def _fence(fw):
    tracks = [e.track for e in fw.eng.values()]
    for lp in fw.lanes.values():
        tracks += lp["tracks"]
    for e in fw.eng.values():
        waits = []
        for tr in tracks:
            if tr is e.track or tr.idx == 0:
                continue
            k = (tr, tr.epoch)
            if e.seen.get(k, 0) >= tr.idx:
                continue
            e.seen[k] = tr.idx
            waits.append((tr.sems[tr.epoch], tr.idx * tr.step))

        def run(eng, waits=waits):
            for s, v in waits:
                eng.wait_ge(s, v)
        e.ops.append(run)


F32 = mybir.dt.float32
BF16 = mybir.dt.bfloat16
AF = mybir.ActivationFunctionType
ALU = mybir.AluOpType

D = 2048
KC = 16
DFF = 5632
FC = 44
NBC = 2
NCORES = 8
EPS = 1e-6
LRU_C = 8.0

_SM = {}
_off = 0
for _n, _w in [("cT", 32), ("adab", 288), ("gpre", 96), ("gpost", 96), ("evconv", 96),
               ("alog", 8), ("dtb", 8), ("onorm", 128), ("retnorm", 1024),
               ("odconv", 64), ("odconvb", 16), ("gab", 16), ("gxb", 16), ("lam", 16),
               ("ident", 128), ("ones", 128), ("tri", 128), ("maskT", 128), ("smask", 128),
               ("decayT", 512), ("gq", 512), ("gk", 4)]:
    _SM[_n] = (_off, _w)
    _off += _w
NS = _off


FULL_PLAN = [("ffn", 0, 0, 0), ("gdn", 1), ("ffn", 0, 1, 2), ("ffn", 1, 0, 3), ("rglru", 4), ("ffn", 1, 1, 5)]


def build(plan=FULL_PLAN, T=2048):
    NTOK = NBC * T
    nc = bass.Bass("TRN2", target_bir_lowering=False)

    def din(name, shape, dt=F32):
        return nc.dram_tensor(name, list(shape), dt, kind="ExternalInput").ap()

    x_in = din("x", [NTOK, D])
    smalls_d = din("smalls", [128, NS])
    cs_d = din("cs", [128, 2, T])
    adaw_d = din("adaw", [2 * 144, 128, 2048])
    w13_d = din("w13r", [2 * 2 * FC, 128, 4096])
    w2_d = din("w2r", [2 * 2 * 16 * 2, 128, 22 * 128])
    evin_d = din("evin", [64, 128, 2048])
    evtail_d = din("evtail", [128, 256])
    evout_d = din("evout", [16, 128, 2048])
    odin_d = din("odin", [32, 128, 2048])
    odout_d = din("odout", [16, 128, 2048])
    gatew_d = din("gatew", [8, 128, 1024])
    out_d = nc.dram_tensor("out", [NTOK, D], F32, kind="ExternalOutput").ap()
    xs_d = nc.dram_tensor("xs", [KC, 128, NTOK], F32).ap()
    xs_v = xs_d.rearrange("k p t -> p k t")
    wcs_ffn = [nc.dram_tensor(f"wcs{i}", [128, 44 * 4096 + 32 * 2816], BF16).ap() for i in range(4)]
    wcs_mix = nc.dram_tensor("wcsm", [128, 64 * 2048 + 256 + 16 * 2048 + 32 * 2048 + 16 * 2048], BF16).ap()

    fw = FW(nc)
    fw_halo = fw.sbuf("halo", [128, 96], F32)
    fw_hst = fw.sbuf("hst", [128, 16], F32)
    fw_c1 = fw.sbuf("c1", [128, 32], F32)
    fw.lane_pool("w", 6)
    fw.lane_pool("wb", 6)
    fw.lane_pool("ws", 4)
    fw.lane_pool("a", 6)

    sm = fw.sbuf("sm", [128, NS], F32)
    smb = Buf(sm)

    def S(name, lo=0, hi=None):
        o, w = _SM[name]
        hi = w if hi is None else hi
        return sm[:, o + lo:o + hi]

    XYt = fw.sbuf("XY", [128, KC, 512], F32)
    XY = [Buf(XYt[:, k, :]) for k in range(KC)]
    hTt = fw.sbuf("hT", [128, KC, 512], BF16)
    hT = [Buf(hTt[:, k, :]) for k in range(KC)]
    BIG = fw.sbuf("BIG", [128, 11264], F32)
    WS = [Buf(fw.sbuf(f"ws{i}", [128, 4096], BF16)) for i in range(5)]
    wsi = [0]

    def wslot():
        b = WS[wsi[0] % len(WS)]
        wsi[0] += 1
        return b
    WSH = [Buf(WS[i][:, j * 2048:(j + 1) * 2048]) for i in range(len(WS)) for j in range(2)]
    wshi = [0]

    def wslot_h():
        b = WSH[wshi[0] % len(WSH)]
        wshi[0] += 1
        return b
    xch = [Buf(fw.sbuf(f"xch{i}", [128, 512], F32)) for i in range(2)]
    sqb = [Buf(fw.sbuf(f"sq{i}", [128, 512], F32)) for i in range(2)]
    tmpb = [Buf(fw.sbuf(f"tmp{i}", [128, 512], F32)) for i in range(3)]
    rstd = Buf(fw.sbuf("rstd", [128, 512], F32))
    modT = fw.sbuf("modT", [128, 2 * 144 * 2], F32)
    modTb = Buf(modT)
    Ab = fw.sbuf("Ab", [128, 6 * KC * 2], F32)
    Bb = fw.sbuf("Bb", [128, 6 * KC * 2], F32)
    Cb = fw.sbuf("Cb", [128, 6 * KC * 2], F32)
    ABCb = Buf(Ab)
    silc = Buf(fw.sbuf("silc", [128, 32], BF16))
    fws = [Buf(fw.sbuf(f"fws{i}", [128, 1024], F32)) for i in range(2)]
    gxb_ = [Buf(fw.sbuf(f"gx{i}", [128, 512], F32)) for i in range(4)]

    pst = [fw.psum(f"ps{i}", [128, 512], F32) for i in range(8)]
    BANK = [Buf(pst[i], excl=True) for i in range(8)]
    PSB = BANK[0:4]
    PSQ = [BANK[2 + i].view(pst[2 + i][:, 0:128]) for i in range(6)]
    pbi = [0]
    pqi = [0]

    def psb():
        b = PSB[pbi[0] % 4]
        pbi[0] += 1
        return b

    def psq():
        b = PSQ[pqi[0] % 6]
        pqi[0] += 1
        return b

    xsb = [Buf(None, f"xs{i}") for i in range(NTOK // 256)]
    outb = Buf(None, "out")

    rr = [0]

    def ew2():
        rr[0] += 1
        return "dve" if rr[0] % 2 else "act"

    def mm(ps, ps_ap, lhsT, rhs, start, stop, reads):
        fw.op("pe", lambda e: e.matmul(ps_ap, lhsT, rhs, start=start, stop=stop), reads=reads, writes=[ps])

    def tr(ps, ps_ap, in_ap, reads):
        fw.op("pe", lambda e: e.transpose(ps_ap, in_ap, S("ident")), reads=reads + [smb], writes=[ps])

    def act(out, in_, func, reads, writes, bias=None, scale=None):
        kw = {}
        if bias is not None:
            kw["bias"] = bias
        if scale is not None:
            kw["scale"] = scale
        fw.op("act", lambda e: e.activation(out=out, in_=in_, func=func, **kw), reads=reads, writes=writes)

    def copy(eng, out, in_, reads, writes):
        if eng == "act":
            act(out, in_, AF.Copy, reads, writes)
        else:
            fw.op(eng, lambda e: e.tensor_copy(out, in_), reads=reads, writes=writes)

    def ts(eng, out, in0, s1, s2, op0, op1, reads, writes):
        if op1 is None:
            fw.op(eng, lambda e: e.tensor_scalar(out, in0, s1, None, op0), reads=reads, writes=writes)
        else:
            fw.op(eng, lambda e: e.tensor_scalar(out, in0, s1, s2, op0, op1), reads=reads, writes=writes)

    def stt(eng, out, in0, sc, in1, op0, op1, reads, writes):
        fw.op(eng, lambda e: e.scalar_tensor_tensor(out, in0, sc, in1, op0, op1), reads=reads, writes=writes)

    def tt(eng, out, in0, in1, op, reads, writes):
        fw.op(eng, lambda e: e.tensor_tensor(out, in0, in1, op), reads=reads, writes=writes)

    def wload(slot, dst_ap, src_ap):
        fw.dma("pool", "w", dst_ap, src_ap, writes=[slot])

    wcache = {}
    wc_off = {}

    def wget(key, slot, ncols, src_ap):
        dst = slot[:, 0:ncols]
        if key in wcache:
            dap, dbuf = wcache[key]
            fw.dma("sp", "wb", dst, dap, reads=[dbuf], writes=[slot])
        else:
            wload(slot, dst, src_ap)
            if key[0] == "w13":
                gidx = key[1] * 2 + key[2]
            elif key[0] == "w2":
                gidx = key[1] // 32
            else:
                gidx = 4
            wt = wcs_ffn[gidx] if gidx < 4 else wcs_mix
            o = wc_off.get(gidx, 0)
            wc_off[gidx] = o + ncols
            dap = wt[:, o:o + ncols]
            dbuf = Buf(None, "wc")
            fw.dma("sp", "ws", dap, dst, reads=[slot], writes=[dbuf])
            wcache[key] = (dap, dbuf)

    fw.dma("sp", "a", sm[:, :], smalls_d[:, :], writes=[smb])
    act(silc[:], S("cT"), AF.Silu, [smb], [silc])
    for l in range(2):
        for q in range(144):
            slot = wslot()
            wload(slot, slot[:, 0:2048], adaw_d[l * 144 + q])
            ps = psq()
            for k in range(KC):
                mm(ps, ps[:, 0:2], slot[:, k * 128:(k + 1) * 128], silc[:, k * 2:k * 2 + 2], k == 0, k == KC - 1, [slot, silc])
            o = (l * 144 + q) * 2
            oa = _SM["adab"][0] + l * 144 + q
            ts("dve", modT[:, o:o + 2], ps[:, 0:2], sm[:, oa:oa + 1], None, ALU.add, None, [ps, smb], [modTb])
    mv = modT[:, :].rearrange("p (l q b) -> p l q b", l=2, q=144)
    Av = Ab[:, :].rearrange("p (s k b) -> p s k b", s=6, k=KC)
    Bv = Bb[:, :].rearrange("p (s k b) -> p s k b", s=6, k=KC)
    Cv = Cb[:, :].rearrange("p (s k b) -> p s k b", s=6, k=KC)
    for l in range(2):
        for s in range(3):
            ls = l * 3 + s
            resw = 1.0 if s == 1 else 0.5
            gp = S("gpre", ls * KC, (ls + 1) * KC)
            gq_ = S("gpost", ls * KC, (ls + 1) * KC)
            for b in range(NBC):
                sh = mv[:, l, (s * 3 + 0) * KC:(s * 3 + 1) * KC, b]
                sc = mv[:, l, (s * 3 + 1) * KC:(s * 3 + 2) * KC, b]
                ga = mv[:, l, (s * 3 + 2) * KC:(s * 3 + 3) * KC, b]
                stt("dve", Av[:, ls, :, b], sc, 1.0, gp, ALU.add, ALU.mult, [modTb, smb], [ABCb])
                copy("dve", Bv[:, ls, :, b], sh, [modTb], [ABCb])
                stt("dve", Cv[:, ls, :, b], ga, 1.0, gq_, ALU.add, ALU.mult, [modTb, smb], [ABCb])
                ts("dve", Cv[:, ls, :, b], Cv[:, ls, :, b], resw, None, ALU.mult, None, [ABCb], [ABCb])

    def Asc(tab, ls, k, b):
        return tab[:, ls, k, b:b + 1]

    def sumsq_rstd(srcs, TT, scale, eps):
        ps = psb()
        n = len(srcs)
        for k, (b, ap) in enumerate(srcs):
            sq = sqb[k % 2]
            act(sq[:, :TT], ap, AF.Square, [b], [sq])
            mm(ps, ps[:, :TT], S("ones"), sq[:, :TT], k == 0, k == n - 1, [sq, smb])
        act(rstd[:, :TT], ps[:, :TT], AF.Sqrt, [ps], [rstd], bias=eps, scale=scale)
        fw.op("dve", lambda e: e.reciprocal(rstd[:, :TT], rstd[:, :TT]), reads=[rstd], writes=[rstd])

    def prologue(ls, t0, TT, b):
        nb = TT // 256
        xbufs = [xsb[t0 // 256 + i] for i in range(nb)]
        fw.dma("sp", "a", XYt[:, :, :TT], xs_v[:, :, t0:t0 + TT], reads=xbufs, writes=XY)
        sumsq_rstd([(XY[k], XY[k][:, :TT]) for k in range(KC)], TT, 1.0 / D, EPS)
        for k in range(KC):
            tm = tmpb[k % 3]
            stt("dve", tm[:, :TT], XY[k][:, :TT], Asc(Av, ls, k, b), rstd[:, :TT], ALU.mult, ALU.mult, [XY[k], rstd, ABCb], [tm])
            act(hT[k][:, :TT], tm[:, :TT], AF.Identity, [tm, ABCb], [hT[k]], bias=Asc(Bv, ls, k, b))

    def epilogue(ls, t0, TT, b):
        nb = TT // 256
        xbufs = [xsb[t0 // 256 + i] for i in range(nb)]
        sumsq_rstd([(XY[k], XY[k][:, :TT]) for k in range(KC)], TT, 1.0 / D, EPS)
        for k in range(KC):
            xc = xch[k % 2]
            fw.dma("sp", "a", xc[:, :TT], xs_d[k, :, t0:t0 + TT], reads=xbufs, writes=[xc])
            tm = tmpb[k % 3]
            stt("dve", tm[:, :TT], XY[k][:, :TT], Asc(Cv, ls, k, b), rstd[:, :TT], ALU.mult, ALU.mult, [XY[k], rstd, ABCb], [tm])
            tt("dve", XY[k][:, :TT], tm[:, :TT], xc[:, :TT], ALU.add, [tm, xc], [XY[k]])
        fw.dma("sp", "a", xs_v[:, :, t0:t0 + TT], XYt[:, :, :TT], reads=XY, writes=xbufs)

    def proj_fm(wd_piece, TT, consume, key):
        slot = wslot_h()
        wget(key, slot, 2048, wd_piece)
        ps = psb()
        for k in range(KC):
            mm(ps, ps[:, :TT], slot[:, k * 128:(k + 1) * 128], hT[k][:, :TT], k == 0, k == KC - 1, [slot, hT[k]])
        consume(ps)

    xin = [Buf(BIG[:, i * 2048:(i + 1) * 2048]) for i in range(4)]
    for t0 in range(0, NTOK, 512):
        for blk in range(4):
            fw.dma("sp", "a", xin[blk][:], x_in[t0 + blk * 128:t0 + (blk + 1) * 128, :], writes=[xin[blk]])
        for k in range(KC):
            ps = psb()
            for blk in range(4):
                tr(ps, ps[:, blk * 128:(blk + 1) * 128], xin[blk][:, k * 128:(k + 1) * 128], [xin[blk]])
            copy(ew2(), XY[k][:], ps[:], [ps], [XY[k]])
        fw.dma("sp", "a", xs_v[:, :, t0:t0 + 512], XYt[:, :, :], reads=XY, writes=[xsb[t0 // 256], xsb[t0 // 256 + 1]])
    _fence(fw)

    def ffn_sandwich(l, f, ls):
        actT = BIG[:, :].bitcast(BF16)
        actb = [Buf(actT[:, j * 512:(j + 1) * 512]) for j in range(FC)]
        TT = 512
        xcb = xch
        sq_, acc_ = sqb
        rstdP = rstd
        tmP, tmE0, tmE1, rstdE = gxb_
        tmE = [tmE0, tmE1]

        def sumsq_acc(k, src_b, src_ap):
            act(sq_[:], src_ap, AF.Square, [src_b], [sq_])
            if k == 0:
                copy("pool", acc_[:], sq_[:], [sq_], [acc_])
            else:
                tt("pool", acc_[:], acc_[:], sq_[:], ALU.add, [acc_, sq_], [acc_])

        def finish_rstd(dst):
            ps = psb()
            mm(ps, ps[:], S("ones"), acc_[:], True, True, [acc_, smb])
            act(dst[:], ps[:], AF.Sqrt, [ps], [dst], bias=EPS, scale=1.0 / D)
            fw.op("dve", lambda e: e.reciprocal(dst[:], dst[:]), reads=[dst], writes=[dst])

        def pro_gen(t0):
            b = t0 // T
            xbufs = [xsb[t0 // 256], xsb[t0 // 256 + 1]]
            for k in range(KC):
                xc = xcb[k % 2]
                fw.dma("sp", "a", xc[:], xs_d[k, :, t0:t0 + TT], reads=xbufs, writes=[xc])
                sumsq_acc(k, xc, xc[:])
                yield
            finish_rstd(rstdP)
            yield
            for k in range(KC):
                xc = xcb[k % 2]
                fw.dma("sp", "a", xc[:], xs_d[k, :, t0:t0 + TT], reads=xbufs, writes=[xc])
                stt("dve", tmP[:], xc[:], Asc(Av, ls, k, b), rstdP[:], ALU.mult, ALU.mult, [xc, rstdP, ABCb], [tmP])
                act(hT[k][:], tmP[:], AF.Identity, [tmP, ABCb], [hT[k]], bias=Asc(Bv, ls, k, b))
                yield

        def epi_gen(t0):
            b = t0 // T
            xbufs = [xsb[t0 // 256], xsb[t0 // 256 + 1]]
            for k in range(KC):
                sumsq_acc(k, XY[k], XY[k][:])
                yield
            finish_rstd(rstdE)
            yield
            for k in range(KC):
                xc = xcb[k % 2]
                te = tmE[k % 2]
                fw.dma("sp", "a", xc[:], xs_d[k, :, t0:t0 + TT], reads=xbufs, writes=[xc])
                stt("dve", te[:], XY[k][:], Asc(Cv, ls, k, b), rstdE[:], ALU.mult, ALU.mult, [XY[k], rstdE, ABCb], [te])
                tt("pool", te[:], te[:], xc[:], ALU.add, [te, xc], [te])
                fw.dma("sp", "a", xs_d[k, :, t0:t0 + TT], te[:], reads=[te], writes=xbufs)
                yield

        def pull(g, n):
            if g is None:
                return None
            for _ in range(n):
                try:
                    next(g)
                except StopIteration:
                    return None
            return g

        def drain(g):
            while g is not None:
                g = pull(g, 1)

        tiles = list(range(0, NTOK, TT))
        drain(pro_gen(tiles[0]))
        eg = None
        for ti, t0 in enumerate(tiles):
            for j in range(FC):
                slot = wslot()
                wget(("w13", l, f, j), slot, 4096, w13_d[(l * 2 + f) * FC + j])
                pg = psb()
                pu = psb()
                for k in range(KC):
                    mm(pg, pg[:], slot[:, k * 128:(k + 1) * 128], hT[k][:], k == 0, k == KC - 1, [slot, hT[k]])
                for k in range(KC):
                    mm(pu, pu[:], slot[:, 2048 + k * 128:2048 + (k + 1) * 128], hT[k][:], k == 0, k == KC - 1, [slot, hT[k]])
                tm = tmpb[j % 3]
                act(tm[:], pg[:], AF.Silu, [pg], [tm])
                tt("dve", actb[j][:], tm[:], pu[:], ALU.mult, [tm, pu], [actb[j]])
                eg = pull(eg, 1)
            drain(eg)
            pgn = pro_gen(tiles[ti + 1]) if ti + 1 < len(tiles) else None
            for m in range(KC):
                s0 = wslot()
                s1 = wslot()
                base = (((l * 2 + f) * 16) + m) * 2
                wget(("w2", base), s0, 2816, w2_d[base])
                wget(("w2", base + 1), s1, 2816, w2_d[base + 1])
                ps = psb()
                for c in range(FC):
                    sl = s0 if c < 22 else s1
                    cc = c % 22
                    mm(ps, ps[:], sl[:, cc * 128:(cc + 1) * 128], actb[c][:], c == 0, c == FC - 1, [sl, actb[c]])
                copy(ew2(), XY[m][:], ps[:], [ps], [XY[m]])
                pgn = pull(pgn, 3)
            drain(pgn)
            eg = epi_gen(t0)
        drain(eg)
        _fence(fw)

    def rglru_sandwich(ls):
        TT = 512
        ygt = BIG[:, 0:4096].bitcast(BF16)
        yg = [Buf(ygt[:, k * 512:(k + 1) * 512]) for k in range(KC)]
        xcv = [Buf(BIG[:, 4096 + i * 512:4096 + (i + 1) * 512]) for i in range(4)]
        xqb = [Buf(BIG[:, 6144 + i * 520:6144 + i * 520 + 515]) for i in range(2)]
        rb = [Buf(BIG[:, 7200 + i * 512:7200 + (i + 1) * 512]) for i in range(2)]
        ib = [Buf(BIG[:, 8224 + i * 512:8224 + (i + 1) * 512]) for i in range(2)]
        ab = [Buf(BIG[:, 9248 + i * 512:9248 + (i + 1) * 512]) for i in range(2)]
        hsb = [Buf(BIG[:, 10272:10784])]
        halo = Buf(fw_halo[:, 0:48])
        hstate = Buf(fw_hst[:, 0:16])
        c1 = Buf(fw_c1[:, 0:32])
        act(c1[:, 0:16], S("lam"), AF.Exp, [smb], [c1], scale=-1.0)
        act(c1[:, 0:16], c1[:, 0:16], AF.Ln, [c1], [c1], bias=1.0)
        ts("dve", c1[:, 16:32], c1[:, 0:16], LRU_C, None, ALU.mult, None, [c1], [c1])
        ts("dve", c1[:, 0:16], c1[:, 0:16], -LRU_C, None, ALU.mult, None, [c1], [c1])
        for t0 in range(0, NTOK, TT):
            b = t0 // T
            if t0 % T == 0:
                fw.op("dve", lambda e: e.memset(halo[:], 0.0), writes=[halo])
                fw.op("dve", lambda e: e.memset(hstate[:], 0.0), writes=[hstate])
            prologue(ls, t0, TT, b)
            def y_chunk(m):
                def cons(ps, m=m):
                    t1 = tmpb[0]
                    t2 = tmpb[1]
                    copy("act", t1[:], ps[:], [ps], [t1])
                    tt("pool", t2[:], t1[:], t1[:], ALU.mult, [t1], [t2])
                    ts("pool", t2[:], t2[:], 0.044715, 1.0, ALU.mult, ALU.add, [t2], [t2])
                    tt("pool", t2[:], t2[:], t1[:], ALU.mult, [t1, t2], [t2])
                    act(t2[:], t2[:], AF.Sigmoid, [t2], [t2], scale=1.5957691216057308)
                    tt("dve", yg[m][:], t1[:], t2[:], ALU.mult, [t1, t2], [yg[m]])
                proj_fm(odin_d[m], TT, cons, ("odin", m))

            def x_block(n):
                gsl = fws[n % 2]
                fw.dma("sp", "w", gsl[:, 0:1024], gatew_d[n], writes=[gsl])
                xcs = []
                for ic in range(2):
                    ci = n * 2 + ic
                    xq = xqb[ic]
                    xc = xcv[(n % 2) * 2 + ic]

                    def cons(ps, ci=ci, xq=xq, xc=xc):
                        copy("dve", xq[:, 0:3], halo[:, ci * 3:ci * 3 + 3], [halo], [xq])
                        copy("act", xq[:, 3:515], ps[:], [ps], [xq])
                        copy("dve", halo[:, ci * 3:ci * 3 + 3], xq[:, 512:515], [xq], [halo])
                        o = _SM["odconv"][0] + ci * 4
                        ob = _SM["odconvb"][0] + ci
                        ts("dve", xc[:], xq[:, 0:512], sm[:, o:o + 1], sm[:, ob:ob + 1], ALU.mult, ALU.add, [xq, smb], [xc])
                        for tap in range(1, 4):
                            stt("dve", xc[:], xq[:, tap:tap + 512], sm[:, o + tap:o + tap + 1], xc[:], ALU.mult, ALU.add, [xq, smb, xc], [xc])
                    proj_fm(odin_d[16 + ci], TT, cons, ("odin", 16 + ci))
                    xcs.append(xc)
                return gsl, xcs

            def gates(n, gsl, xcs):
                for jc in range(2):
                    cj = n * 2 + jc
                    pr = psb()
                    pi = psb()
                    for g, pp in ((0, pr), (1, pi)):
                        for ic in range(2):
                            o = ((g * 2 + jc) * 2 + ic) * 128
                            mm(pp, pp[:], gsl[:, o:o + 128], xcs[ic][:], ic == 0, ic == 1, [gsl, xcs[ic]])
                    r_ = rb[jc]
                    i_ = ib[jc]
                    a_ = ab[jc]
                    oga = _SM["gab"][0] + cj
                    ogx = _SM["gxb"][0] + cj
                    act(r_[:], pr[:], AF.Sigmoid, [pr, smb], [r_], bias=sm[:, oga:oga + 1])
                    act(i_[:], pi[:], AF.Sigmoid, [pi, smb], [i_], bias=sm[:, ogx:ogx + 1])
                    act(a_[:], r_[:], AF.Exp, [r_, c1], [a_], scale=c1[:, cj:cj + 1])
                    act(r_[:], r_[:], AF.Tanh, [r_, c1], [r_], scale=c1[:, 16 + cj:16 + cj + 1])
                    t2 = tmpb[2]
                    tt("dve", t2[:], a_[:], a_[:], ALU.mult, [a_], [t2])
                    stt("dve", t2[:], t2[:], 1.0, r_[:], ALU.add, ALU.mult, [t2, r_], [t2])
                    act(t2[:], t2[:], AF.Sqrt, [t2], [t2])
                    tt("pool", i_[:], i_[:], xcs[jc][:], ALU.mult, [i_, xcs[jc]], [i_])
                    tt("dve", t2[:], t2[:], i_[:], ALU.mult, [t2, i_], [t2])
                    hs = hsb[0]
                    fw.op("dve", lambda e, hs=hs, a_=a_, t2=t2, cj=cj: e.tensor_tensor_scan(hs[:], a_[:], t2[:], hstate[:, cj:cj + 1], ALU.mult, ALU.add),
                          reads=[a_, t2, hstate], writes=[hs])
                    copy("dve", hstate[:, cj:cj + 1], hs[:, 511:512], [hs], [hstate])
                    tt("dve", yg[cj][:], hs[:], yg[cj][:], ALU.mult, [hs, yg[cj]], [yg[cj]])

            pend = None
            for n in range(8):
                y_chunk(2 * n)
                y_chunk(2 * n + 1)
                cur = x_block(n)
                if pend is not None:
                    gates(*pend)
                pend = (n,) + cur
            gates(*pend)
            for m in range(KC):
                slot = wslot_h()
                wget(("odout", m), slot, 2048, odout_d[m])
                ps = psb()
                for k in range(KC):
                    mm(ps, ps[:], slot[:, k * 128:(k + 1) * 128], yg[k][:], k == 0, k == KC - 1, [slot, yg[k]])
                copy(ew2(), XY[m][:], ps[:], [ps], [XY[m]])
            epilogue(ls, t0, TT, b)
        _fence(fw)


    def gdn_sandwich(ls):
        TT = 256
        _, gdec = _consts()
        AX = mybir.AxisListType.X
        oall = [Buf(BIG[:, blk * 2048:(blk + 1) * 2048]) for blk in range(2)]
        xq = [Buf(BIG[:, 4096 + i * 264:4096 + i * 264 + 259]) for i in range(3)]
        qkvS = [[Buf(BIG[:, 4888 + s * 768 + i * 256:4888 + s * 768 + (i + 1) * 256]) for i in range(3)] for s in range(2)]
        ktokS = [Buf(BIG[:, 6424 + s * 768:6424 + s * 768 + 256]) for s in range(2)]
        vtokS = [Buf(BIG[:, 6680 + s * 768:6680 + s * 768 + 256]) for s in range(2)]
        ztokS = [Buf(BIG[:, 6936 + s * 768:6936 + s * 768 + 256]) for s in range(2)]
        G = [Buf(BIG[:, 8192 + i * 128:8192 + (i + 1) * 128]) for i in range(24)]
        G += [Buf(XYt[:, k, 256 + j * 128:256 + (j + 1) * 128]) for k in range(KC) for j in range(2)]

        class _R:
            pass
        roles = {}
        gi = 0
        for s in range(2):
            for blk in range(2):
                C = _R()
                (C.P0, C.N0, C.Pa, C.Na, C.Xa, C.Xb, C.QKT, C.kegT, C.qstT, C.kst) = G[gi:gi + 10]
                gi += 10
                roles[(s, blk)] = C
        hroles = []
        for s in range(2):
            Hh = _R()
            (Hh.Rb, Hh.vnew, Hh.osq, Hh.ob) = G[gi:gi + 4]
            gi += 4
            hroles.append(Hh)
        scT, qin0, qin1 = G[gi:gi + 3]
        osq, ob = hroles[0].osq, hroles[0].ob
        rq = [Buf(BIG[:, 4096 + i * 256:4096 + (i + 1) * 256]) for i in range(2)]
        rk = [Buf(BIG[:, 4608 + i * 256:4608 + (i + 1) * 256]) for i in range(2)]
        rqr = [Buf(BIG[:, 5120 + i * 256:5120 + (i + 1) * 256]) for i in range(2)]
        rkr = [Buf(BIG[:, 5632 + i * 256:5632 + (i + 1) * 256]) for i in range(2)]
        rtm = [Buf(BIG[:, 6144 + i * 256:6144 + (i + 1) * 256]) for i in range(2)]
        rktok = Buf(BIG[:, 6656:7168])
        rvtok = Buf(BIG[:, 7168:7680])
        rgtok = Buf(BIG[:, 7680:8192])
        gsm = fw.sbuf("gsm", [128, 8 * 16 + 8], F32)
        gsmb = Buf(gsm)
        Sg_t = fw.sbuf("Sg", [128, 1024], F32)
        Sg = [Buf(Sg_t[:, h * 128:(h + 1) * 128]) for h in range(8)]
        Sr_t = fw.sbuf("Sr", [128, 2048], F32)
        Sr = [Buf(Sr_t[:, h * 512:(h + 1) * 512]) for h in range(4)]
        cst = fw.sbuf("cst", [128, 2, 256], F32)
        cstb = Buf(cst)
        halo = Buf(fw_halo[:, 0:72])
        ssb_t = fw.sbuf("ssb", [128, 4], F32)
        ssb = Buf(ssb_t[:, 0:2])
        ssbS = [Buf(ssb_t[:, 2 + s:3 + s]) for s in range(2)]

        def gs(i, blk, lo=0, hi=8):
            return gsm[:, (i * 2 + blk) * 8 + lo:(i * 2 + blk) * 8 + hi]
        NEGA = gsm[:, 128:136]
        act(NEGA, S("alog"), AF.Exp, [smb], [gsmb])
        ts("dve", NEGA, NEGA, -1.0, None, ALU.mult, None, [gsmb], [gsmb])

        def rowsum_rstd(ps_ap, width, src):
            act(osq[:, :] if width <= 128 else rtm[0][:, :width], ps_ap, AF.Square, [src], [osq if width <= 128 else rtm[0]])
            sqap = osq[:, :] if width <= 128 else rtm[0][:, :width]
            sqb_ = osq if width <= 128 else rtm[0]
            fw.op("dve", lambda e: e.reduce_sum(ssb[:, 0:1], sqap, AX), reads=[sqb_], writes=[ssb])
            act(ssb[:, 0:1], ssb[:, 0:1], AF.Sqrt, [ssb], [ssb], bias=EPS, scale=1.0 / width)
            fw.op("dve", lambda e: e.reciprocal(ssb[:, 0:1], ssb[:, 0:1]), reads=[ssb], writes=[ssb])


        def run_gens(gens):
            active = list(gens)
            while active:
                for g in list(active):
                    try:
                        next(g)
                    except StopIteration:
                        active.remove(g)

        def stage_a_gen(s, h):
            qkv = qkvS[s]
            for idx, m in enumerate((h, 8 + h, 16 + h)):
                def cons(ps, idx=idx, m=m):
                    x_ = xq[idx]
                    copy("dve", x_[:, 0:3], halo[:, m * 3:m * 3 + 3], [halo], [x_])
                    copy("act", x_[:, 3:259], ps[:, :TT], [ps], [x_])
                    copy("dve", halo[:, m * 3:m * 3 + 3], x_[:, 256:259], [x_], [halo])
                    o = _SM["evconv"][0] + m * 4
                    tm = tmpb[idx]
                    ts("dve", tm[:, :TT], x_[:, 0:TT], sm[:, o:o + 1], None, ALU.mult, None, [x_, smb], [tm])
                    for tap in range(1, 4):
                        stt("dve", tm[:, :TT], x_[:, tap:tap + TT], sm[:, o + tap:o + tap + 1], tm[:, :TT], ALU.mult, ALU.add, [x_, smb, tm], [tm])
                    act(qkv[idx][:], tm[:, :TT], AF.Silu, [tm], [qkv[idx]])
                proj_fm(evin_d[m], TT, cons, ("evin", m))
                yield
            qT, kT, vT = qkv
            sumsq_rstd([(qT, qT[:])], TT, 1.0, EPS)
            stt("dve", qT[:], qT[:], 128.0 ** -0.5, rstd[:, :TT], ALU.mult, ALU.mult, [qT, rstd], [qT])
            yield
            sumsq_rstd([(kT, kT[:])], TT, 1.0, EPS)
            tt("dve", kT[:], kT[:], rstd[:, :TT], ALU.mult, [kT, rstd], [kT])
            yield

        def stage_b(s, h):
            qT, kT, vT = qkvS[s]
            zsl = wslot_h()
            wget(("evin", 24 + h), zsl, 2048, evin_d[24 + h])
            ktok, vtok, ztok = ktokS[s], vtokS[s], ztokS[s]
            for blk in range(2):
                ps = psq()
                for k in range(KC):
                    mm(ps, ps[:], hT[k][:, blk * 128:(blk + 1) * 128], zsl[:, k * 128:(k + 1) * 128], k == 0, k == KC - 1, [zsl, hT[k]])
                act(ztok[:, blk * 128:(blk + 1) * 128], ps[:], AF.Silu, [ps], [ztok])
                pk_ = psq()
                tr(pk_, pk_[:], kT[:, blk * 128:(blk + 1) * 128], [kT])
                copy("act", ktok[:, blk * 128:(blk + 1) * 128], pk_[:], [pk_], [ktok])
                pv_ = psq()
                tr(pv_, pv_[:], vT[:, blk * 128:(blk + 1) * 128], [vT])
                copy("dve", vtok[:, blk * 128:(blk + 1) * 128], pv_[:], [pv_], [vtok])

        def chain_gen(s, blk, h):
            C = roles[(s, blk)]
            qT, kT, vT = qkvS[s]
            ktok = ktokS[s]
            cs_ = slice(blk * 128, (blk + 1) * 128)
            bcol = gs(0, blk, h, h + 1)
            ldcol = gs(1, blk, h, h + 1)
            gcol = gs(2, blk, h, h + 1)
            eglcol = gs(5, blk, h, h + 1)
            ldb, egb, DT = C.Pa, C.Na, C.Xb
            P0, N0, QKT, kegT, qstT, kst = C.P0, C.N0, C.QKT, C.kegT, C.qstT, C.kst
            ts("dve", ldb[:], S("ones"), ldcol, None, ALU.mult, None, [smb, gsmb], [ldb])
            pg = psq()
            mm(pg, pg[:], ldb[:], S("tri"), True, True, [ldb, smb])
            act(egb[:], pg[:], AF.Exp, [pg], [egb])
            stt("dve", DT[:], pg[:], gcol, S("maskT"), ALU.subtract, ALU.add, [pg, gsmb, smb], [DT])
            yield
            act(DT[:], DT[:], AF.Exp, [DT], [DT])
            pkk = psq()
            mm(pkk, pkk[:], kT[:, cs_], kT[:, cs_], True, True, [kT])
            pkq = psq()
            mm(pkq, pkq[:], kT[:, cs_], qT[:, cs_], True, True, [kT, qT])
            tt("dve", kegT[:], kT[:, cs_], egb[:], ALU.mult, [kT, egb], [kegT])
            tt("dve", qstT[:], qT[:, cs_], egb[:], ALU.mult, [qT, egb], [qstT])
            act(kst[:], ktok[:, cs_], AF.Copy, [ktok, gsmb], [kst], scale=eglcol)
            stt("dve", P0[:], pkk[:], bcol, DT[:], ALU.mult, ALU.mult, [pkk, gsmb, DT], [P0])
            tt("dve", QKT[:], pkq[:], DT[:], ALU.mult, [pkq, DT], [QKT])
            yield
            tt("dve", P0[:], P0[:], S("smask"), ALU.mult, [P0, smb], [P0])
            pt = psq()
            tr(pt, pt[:], P0[:], [P0])
            copy("act", N0[:], pt[:], [pt], [N0])
            stt("dve", C.Xa[:], P0[:], -1.0, S("ident"), ALU.mult, ALU.add, [P0, smb], [C.Xa])
            yield
            X, Xo = C.Xa, C.Xb
            Pk, Nk = P0, N0
            Pn, Nn = C.Pa, C.Na
            for lvl in range(6):
                if lvl < 5:
                    pp = psq()
                    mm(pp, pp[:], Nk[:], Pk[:], True, True, [Nk, Pk])
                pn = psq()
                mm(pn, pn[:], Pk[:], Nk[:], True, True, [Nk, Pk])
                if lvl < 5:
                    copy("act", Pn[:], pp[:], [pp], [Pn])
                copy("dve", Nn[:], pn[:], [pn], [Nn])
                yield
                px = psq()
                mm(px, px[:], Nn[:], X[:], True, True, [Nn, X])
                tt("dve", Xo[:], X[:], px[:], ALU.add, [X, px], [Xo])
                yield
                X, Xo = Xo, X
                Pk, Nk, Pn, Nn = Pn, Nn, Pk, Nk
            C.X = X

        def sphase_gen(s, h):
            Hh = hroles[s]
            vtok, ztok = vtokS[s], ztokS[s]
            sb_ = ssbS[s]
            for blk in range(2):
                C = roles[(s, blk)]
                cs_ = slice(blk * 128, (blk + 1) * 128)
                bcol = gs(0, blk, h, h + 1)
                deccol = gs(6, blk, h, h + 1)
                pks = psq()
                mm(pks, pks[:], C.kegT[:], Sg[h][:], True, True, [C.kegT, Sg[h]])
                tt("dve", Hh.Rb[:], vtok[:, cs_], pks[:], ALU.subtract, [vtok, pks], [Hh.Rb])
                yield
                pv = psq()
                mm(pv, pv[:], C.X[:], Hh.Rb[:], True, True, [C.X, Hh.Rb])
                ts("dve", Hh.vnew[:], pv[:], bcol, None, ALU.mult, None, [pv, gsmb], [Hh.vnew])
                yield
                po = psq()
                mm(po, po[:], C.qstT[:], Sg[h][:], True, False, [C.qstT, Sg[h]])
                mm(po, po[:], C.QKT[:], Hh.vnew[:], False, True, [C.QKT, Hh.vnew])
                psn = psq()
                mm(psn, psn[:], C.kst[:], Hh.vnew[:], True, True, [C.kst, Hh.vnew])
                act(Hh.osq[:], po[:], AF.Square, [po], [Hh.osq])
                copy("dve", Hh.ob[:], po[:], [po], [Hh.ob])
                stt("dve", Sg[h][:], Sg[h][:], deccol, psn[:], ALU.mult, ALU.add, [Sg[h], gsmb, psn], [Sg[h]])
                yield
                fw.op("dve", lambda e, Hh=Hh, sb_=sb_: e.reduce_sum(sb_[:], Hh.osq[:], AX), reads=[Hh.osq], writes=[sb_])
                yield
                act(sb_[:], sb_[:], AF.Sqrt, [sb_], [sb_], bias=EPS, scale=1.0 / 128)
                yield
                fw.op("dve", lambda e, sb_=sb_: e.reciprocal(sb_[:], sb_[:]), reads=[sb_], writes=[sb_])
                yield
                stt("dve", Hh.ob[:], Hh.ob[:], sb_[:], S("onorm"), ALU.mult, ALU.mult, [Hh.ob, sb_, smb], [Hh.ob])
                yield
                tt("dve", oall[blk][:, h * 128:(h + 1) * 128], Hh.ob[:], ztok[:, cs_], ALU.mult, [Hh.ob, ztok], [oall[blk]])
                yield


        class _RS:
            pass
        RS = []
        for s_ in range(2):
            if s_ == 0:
                pcs = [Buf(BIG[:, 4096 + i * 256:4096 + (i + 1) * 256]) for i in range(16)]
            else:
                pcs = [Buf(XYt[:, k, 256:512]) for k in range(KC)]
            R = _RS()
            R.rq, R.rk, R.rqr, R.rkr, R.rtm = pcs[0:2], pcs[2:4], pcs[4:6], pcs[6:8], pcs[8:10]
            R.rktok, R.rvtok, R.rgtok = pcs[10:12], pcs[12:14], pcs[14:16]
            R.scT, R.qin0, R.qin1 = G[s_ * 3:s_ * 3 + 3]
            RS.append(R)

        def ret_gen(h, R, sb_):
            for i in range(2):
                proj_fm(evin_d[32 + 2 * h + i], TT, lambda ps, i=i: copy("act", R.rq[i][:], ps[:, :TT], [ps], [R.rq[i]]), ("evin", 32 + 2 * h + i))
                yield
            for i in range(2):
                proj_fm(evin_d[40 + 2 * h + i], TT, lambda ps, i=i: copy("dve", R.rk[i][:], ps[:, :TT], [ps], [R.rk[i]]), ("evin", 40 + 2 * h + i))
                yield
            for src, dst in ((R.rq, R.rqr), (R.rk, R.rkr)):
                x1, x2 = src
                t0_, t1_ = R.rtm
                tt("dve", t0_[:], x1[:], cst[:, 0, :], ALU.mult, [x1, cstb], [t0_])
                tt("dve", t1_[:], x2[:], cst[:, 1, :], ALU.mult, [x2, cstb], [t1_])
                tt("dve", dst[0][:], t0_[:], t1_[:], ALU.subtract, [t0_, t1_], [dst[0]])
                yield
                tt("dve", t0_[:], x2[:], cst[:, 0, :], ALU.mult, [x2, cstb], [t0_])
                tt("dve", t1_[:], x1[:], cst[:, 1, :], ALU.mult, [x1, cstb], [t1_])
                tt("dve", dst[1][:], t0_[:], t1_[:], ALU.add, [t0_, t1_], [dst[1]])
                yield
            for base, dstl, fn in ((48, R.rvtok, None), (56, R.rgtok, AF.Silu)):
                sl = [wslot_h(), wslot_h()]
                for i in range(2):
                    wget(("evin", base + 2 * h + i), sl[i], 2048, evin_d[base + 2 * h + i])
                for blk in range(2):
                    ps = psb()
                    for i in range(2):
                        for k in range(KC):
                            mm(ps, ps[:, i * 128:(i + 1) * 128], hT[k][:, blk * 128:(blk + 1) * 128], sl[i][:, k * 128:(k + 1) * 128], k == 0, k == KC - 1, [sl[i], hT[k]])
                    if fn is None:
                        copy("dve", dstl[blk][:], ps[:, 0:256], [ps], [dstl[blk]])
                    else:
                        act(dstl[blk][:], ps[:, 0:256], fn, [ps], [dstl[blk]])
                    yield
            og = _SM["gk"][0] + h
            for blk in range(2):
                for i in range(2):
                    pk_ = psq()
                    tr(pk_, pk_[:], R.rkr[i][:, blk * 128:(blk + 1) * 128], [R.rkr[i]])
                    ts("dve", R.rktok[blk][:, i * 128:(i + 1) * 128], pk_[:], sm[:, og:og + 1], None, ALU.mult, None, [pk_, smb], [R.rktok[blk]])
                yield
            for blk in range(2):
                cs_ = slice(blk * 128, (blk + 1) * 128)
                vb_ = R.rvtok[blk]
                psc = psq()
                mm(psc, psc[:], R.rkr[0][:, cs_], R.rqr[0][:, cs_], True, False, [R.rkr[0], R.rqr[0]])
                mm(psc, psc[:], R.rkr[1][:, cs_], R.rqr[1][:, cs_], False, True, [R.rkr[1], R.rqr[1]])
                tt("dve", R.scT[:], psc[:], S("decayT", h * 128, (h + 1) * 128), ALU.mult, [psc, smb], [R.scT])
                tt("dve", R.qin0[:], R.rqr[0][:, cs_], S("gq", h * 128, (h + 1) * 128), ALU.mult, [R.rqr[0], smb], [R.qin0])
                tt("dve", R.qin1[:], R.rqr[1][:, cs_], S("gq", h * 128, (h + 1) * 128), ALU.mult, [R.rqr[1], smb], [R.qin1])
                yield
                po = psb()
                mm(po, po[:, 0:256], R.scT[:], vb_[:], True, False, [R.scT, vb_])
                mm(po, po[:, 0:256], R.qin0[:], Sr[h][:, 0:256], False, False, [R.qin0, Sr[h]])
                mm(po, po[:, 0:256], R.qin1[:], Sr[h][:, 256:512], False, True, [R.qin1, Sr[h]])
                for i in range(2):
                    ps = psb()
                    mm(ps, ps[:, 0:256], R.rktok[blk][:, i * 128:(i + 1) * 128], vb_[:], True, True, [R.rktok[blk], vb_])
                    stt("dve", Sr[h][:, i * 256:(i + 1) * 256], Sr[h][:, i * 256:(i + 1) * 256], gdec[h], ps[:, 0:256], ALU.mult, ALU.add, [Sr[h], ps], [Sr[h]])
                act(R.rtm[0][:], po[:, 0:256], AF.Square, [po], [R.rtm[0]])
                copy("dve", R.rtm[1][:], po[:, 0:256], [po], [R.rtm[1]])
                yield
                fw.op("dve", lambda e, R=R, sb_=sb_: e.reduce_sum(sb_[:], R.rtm[0][:], AX), reads=[R.rtm[0]], writes=[sb_])
                yield
                act(sb_[:], sb_[:], AF.Sqrt, [sb_], [sb_], bias=EPS, scale=1.0 / 256)
                yield
                fw.op("dve", lambda e, sb_=sb_: e.reciprocal(sb_[:], sb_[:]), reads=[sb_], writes=[sb_])
                yield
                stt("dve", R.rtm[1][:], R.rtm[1][:], sb_[:], S("retnorm", h * 256, (h + 1) * 256), ALU.mult, ALU.mult, [R.rtm[1], sb_, smb], [R.rtm[1]])
                yield
                tt("dve", oall[blk][:, 1024 + h * 256:1024 + (h + 1) * 256], R.rtm[1][:], R.rgtok[blk][:], ALU.mult, [R.rtm[1], R.rgtok[blk]], [oall[blk]])
                yield

        for t0 in range(0, NTOK, TT):
            b = t0 // T
            tl = t0 % T
            if tl == 0:
                fw.op("dve", lambda e: e.memset(halo[:], 0.0), writes=[halo])
                fw.op("dve", lambda e: e.memset(Sg_t[:, :], 0.0), writes=Sg)
                fw.op("dve", lambda e: e.memset(Sr_t[:, :], 0.0), writes=Sr)
            prologue(ls, t0, TT, b)
            fw.dma("sp", "a", cst[:, :, :], cs_d[:, :, tl:tl + TT], writes=[cstb])
            tsl = wslot_h()
            wget(("evtail",), tsl, 256, evtail_d[:, :])
            for blk in range(2):
                ps = psq()
                for k in range(KC):
                    mm(ps, ps[:, 0:16], hT[k][:, blk * 128:(blk + 1) * 128], tsl[:, k * 16:(k + 1) * 16], k == 0, k == KC - 1, [tsl, hT[k]])
                act(gs(0, blk), ps[:, 0:8], AF.Sigmoid, [ps], [gsmb])
                tt("dve", gs(7, blk), ps[:, 8:16], S("dtb"), ALU.add, [ps, smb], [gsmb])
                act(gs(7, blk), gs(7, blk), AF.Exp, [gsmb], [gsmb])
                act(gs(7, blk), gs(7, blk), AF.Ln, [gsmb], [gsmb], bias=1.0)
                tt("dve", gs(1, blk), gs(7, blk), NEGA, ALU.mult, [gsmb], [gsmb])
                p2 = psq()
                mm(p2, p2[:, 0:8], S("tri"), gs(1, blk), True, True, [smb, gsmb])
                copy("dve", gs(2, blk), p2[:, 0:8], [p2], [gsmb])
                p3 = psq()
                mm(p3, p3[:, 0:8], S("ones"), gs(1, blk), True, True, [smb, gsmb])
                copy("dve", gs(3, blk), p3[:, 0:8], [p3], [gsmb])
                act(gs(4, blk), gs(2, blk), AF.Exp, [gsmb], [gsmb])
                tt("dve", gs(5, blk), gs(3, blk), gs(2, blk), ALU.subtract, [gsmb], [gsmb])
                act(gs(5, blk), gs(5, blk), AF.Exp, [gsmb], [gsmb])
                act(gs(6, blk), gs(3, blk), AF.Exp, [gsmb], [gsmb])
            run_gens([stage_a_gen(s, s) for s in range(2)])
            for hp in range(4):
                for s in range(2):
                    stage_b(s, 2 * hp + s)
                run_gens([chain_gen(s, blk, 2 * hp + s) for s in range(2) for blk in range(2)])
                gl = [sphase_gen(s, 2 * hp + s) for s in range(2)]
                if hp < 3:
                    gl += [stage_a_gen(s, 2 * (hp + 1) + s) for s in range(2)]
                run_gens(gl)
            _fence(fw)
            for hp in range(2):
                run_gens([ret_gen(2 * hp + s, RS[s], ssbS[s]) for s in range(2)])
            for blk in range(2):
                for k in range(KC):
                    ps = psq()
                    tr(ps, ps[:], oall[blk][:, k * 128:(k + 1) * 128], [oall[blk]])
                    copy(ew2(), hT[k][:, blk * 128:(blk + 1) * 128], ps[:], [ps], [hT[k]])
            for m in range(KC):
                proj_fm(evout_d[m], TT, lambda ps, m=m: copy(ew2(), XY[m][:, :TT], ps[:, :TT], [ps], [XY[m]]), ("evout", m))
            epilogue(ls, t0, TT, b)
            _fence(fw)
        _fence(fw)

    for item in plan:
        if item[0] == "ffn":
            ffn_sandwich(item[1], item[2], item[3])
        elif item[0] == "rglru":
            rglru_sandwich(item[1])
        elif item[0] == "gdn":
            gdn_sandwich(item[1])

    ot = [Buf(BIG[:, i * 2048:(i + 1) * 2048]) for i in range(4)]
    for t0 in range(0, NTOK, 512):
        fw.dma("sp", "a", XYt[:, :, :], xs_v[:, :, t0:t0 + 512], reads=[xsb[t0 // 256], xsb[t0 // 256 + 1]], writes=XY)
        for blk in range(4):
            for q in range(4):
                ps = psb()
                for kk in range(4):
                    k = q * 4 + kk
                    tr(ps, ps[:, kk * 128:(kk + 1) * 128], XY[k][:, blk * 128:(blk + 1) * 128], [XY[k]])
                copy(ew2(), ot[blk][:, q * 512:(q + 1) * 512], ps[:], [ps], [ot[blk]])
            fw.dma("sp", "a", out_d[t0 + blk * 128:t0 + (blk + 1) * 128, :], ot[blk][:], reads=[ot[blk]], writes=[outb])
    fw.final_wait("sp", [outb])
    fw.emit()
    fw.close()
    return nc


def _consts():
    f8 = np.float64
    i = np.arange(128)
    ident = np.eye(128)
    ones = np.ones((128, 128))
    tri = (i[:, None] <= i[None, :]).astype(f8)
    maskT = np.where(i[None, :] >= i[:, None], 0.0, -1e30)
    smask = (i[None, :] > i[:, None]).astype(f8)
    lg = np.log1p(-np.exp2(-5.0 - np.arange(4, dtype=np.float32))).astype(np.float32).astype(f8)
    decayT = np.zeros((128, 4, 128))
    gq = np.zeros((128, 4, 128))
    gk = np.zeros((128, 4))
    for h in range(4):
        dd = (i[None, :] - i[:, None]).astype(f8)
        decayT[:, h, :] = np.where(dd >= 0, np.exp(dd * lg[h]), 0.0) * (256.0 ** -0.5)
        gq[:, h, :] = np.exp((i[None, :] + 1.0) * lg[h])
        gk[:, h] = np.exp((127.0 - i) * lg[h]) * (256.0 ** -0.5)
    gdec = [float(np.exp(128.0 * lg[h])) for h in range(4)]
    return dict(ident=ident, ones=ones, tri=tri, maskT=maskT, smask=smask,
                decayT=decayT.reshape(128, 512), gq=gq.reshape(128, 512), gk=gk), gdec


def _rope_tables(T):
    half = 128
    inv_freq = (np.float32(10000.0) ** (-np.arange(half, dtype=np.float32) / np.float32(half))).astype(np.float32)
    pos = np.arange(T, dtype=np.float32)
    ang = (inv_freq[:, None] * pos[None, :]).astype(np.float32)
    cs = np.stack([np.cos(ang.astype(np.float64)), np.sin(ang.astype(np.float64))], axis=1)
    return np.ascontiguousarray(cs.astype(np.float32))


def _proj_layout(W):
    n = W.shape[1] // 128
    return np.ascontiguousarray(W.reshape(16, 128, n, 128).transpose(2, 1, 0, 3).reshape(n, 128, 2048))


def _pcol(v, n):
    return v.reshape(n, 128).T


def prep_shared(inp):
    ada_w = inp["ada_w"]
    sh = {}
    sh["adaw"] = np.ascontiguousarray(ada_w.reshape(2, 16, 128, 144, 128).transpose(0, 3, 2, 1, 4).reshape(288, 128, 2048))
    sh["w13r"] = np.ascontiguousarray(inp["ffn_w13"].reshape(2, 2, 16, 128, 2, 44, 128).transpose(0, 1, 5, 3, 4, 2, 6).reshape(176, 128, 4096))
    sh["w2r"] = np.ascontiguousarray(inp["ffn_w2"].reshape(2, 2, 2, 22, 128, 16, 128).transpose(0, 1, 5, 2, 4, 3, 6).reshape(128, 128, 2816))
    evw = inp["ev_w_in"][0]
    sh["evin"] = _proj_layout(np.concatenate([evw[:, :3072], evw[:, 3088:]], axis=1))
    sh["evtail"] = np.ascontiguousarray(evw[:, 3072:3088].reshape(16, 128, 16).transpose(1, 0, 2).reshape(128, 256))
    sh["evout"] = _proj_layout(inp["ev_w_out"][0])
    sh["odin"] = _proj_layout(inp["od_w_in"][0])
    sh["odout"] = _proj_layout(inp["od_w_out"][0])
    g = np.stack([inp["od_gate_a_w"][0], inp["od_gate_x_w"][0]], axis=0)
    g = g.reshape(2, 8, 2, 128, 2, 128)
    sh["gatew"] = np.ascontiguousarray(g.transpose(1, 3, 0, 4, 2, 5).reshape(8, 128, 1024))
    return sh


def prep_smalls(inp, core):
    consts, _ = _consts()
    sm = np.zeros((128, NS), np.float32)

    def put(name, arr):
        o, w = _SM[name]
        arr = np.asarray(arr, np.float32).reshape(128, w)
        sm[:, o:o + w] = arr
    c = inp["c"][core * NBC:(core + 1) * NBC]
    put("cT", c.reshape(NBC, 16, 128).transpose(2, 1, 0))
    put("adab", inp["ada_b"].reshape(2, 144, 128).transpose(2, 0, 1))
    put("gpre", inp["norm_pre"].reshape(2, 3, 16, 128).transpose(3, 0, 1, 2))
    put("gpost", inp["norm_post"].reshape(2, 3, 16, 128).transpose(3, 0, 1, 2))
    put("evconv", inp["ev_conv_w"][0].reshape(4, 24, 128).transpose(2, 1, 0))
    put("alog", np.broadcast_to(inp["ev_a_log"][0][None, :], (128, 8)))
    put("dtb", np.broadcast_to(inp["ev_dt_bias"][0][None, :], (128, 8)))
    put("onorm", np.broadcast_to(inp["ev_o_norm"][0][None, :], (128, 128)))
    put("retnorm", np.broadcast_to(inp["ev_ret_norm"][0][None, :], (128, 1024)))
    put("odconv", inp["od_conv_w"][0].reshape(4, 16, 128).transpose(2, 1, 0))
    put("odconvb", _pcol(inp["od_conv_b"][0], 16))
    put("gab", _pcol(inp["od_gate_a_b"][0], 16))
    put("gxb", _pcol(inp["od_gate_x_b"][0], 16))
    put("lam", _pcol(inp["od_lambda"][0], 16))
    for k, v in consts.items():
        put(k, v)
    return sm


def kernel(**inputs):
    inp = {k: np.asarray(v) for k, v in inputs.items()}
    T = inp["x"].shape[1]
    nc = build(FULL_PLAN, T)
    sh = prep_shared(inp)
    cs = _rope_tables(T)
    in_maps = []
    for core in range(NCORES):
        m = dict(sh)
        m["x"] = np.ascontiguousarray(inp["x"][core * NBC:(core + 1) * NBC].reshape(NBC * T, D))
        m["smalls"] = prep_smalls(inp, core)
        m["cs"] = cs
        in_maps.append(m)
    res = run_bass_kernel_spmd(nc, in_maps, core_ids=list(range(NCORES)))
    outs = [res.results[i]["out"].reshape(NBC, T, D) for i in range(NCORES)]
    return np.concatenate(outs, axis=0).astype(np.float32)
```

```python
import numpy as np
import concourse.bass as bass
import concourse.mybir as mybir
from concourse.bass_utils import run_bass_kernel_spmd


EPOCH = 24000


class _St:
    __slots__ = ("lw", "rd", "excl")

    def __init__(self, excl):
        self.lw = None
        self.rd = {}
        self.excl = excl


class Buf:
    __slots__ = ("ap", "st", "name")

    def __init__(self, ap, name="", excl=False, st=None):
        self.ap = ap
        self.st = st if st is not None else _St(excl)
        self.name = name

    def view(self, ap):
        return Buf(ap, self.name, st=self.st)

    @property
    def lw(self):
        return self.st.lw

    @lw.setter
    def lw(self, v):
        self.st.lw = v

    @property
    def rd(self):
        return self.st.rd

    @rd.setter
    def rd(self, v):
        self.st.rd = v

    @property
    def excl(self):
        return self.st.excl

    def __getitem__(self, k):
        return self.ap[k]


class Track:
    def __init__(self, fw, name, step):
        self.fw = fw
        self.name = name
        self.step = step
        self.epoch = 0
        self.idx = 0
        self.sems = [fw._new_sem(f"{name}_e0")]

    def next_token(self):
        if (self.idx + 1) * self.step > EPOCH:
            self.epoch += 1
            self.idx = 0
            self.sems.append(self.fw._new_sem(f"{self.name}_e{self.epoch}"))
        self.idx += 1
        return (self, self.epoch, self.idx)


class Engine:
    def __init__(self, fw, name, strict_self):
        self.fw = fw
        self.name = name
        self.track = Track(fw, name, 1)
        self.ops = []
        self.seen = {}
        self.strict_self = strict_self


class FW:
    def __init__(self, nc):
        self.nc = nc
        self._stack = []
        self._semcount = 0
        self.eng = {
            "pe": Engine(self, "pe", False),
            "act": Engine(self, "act", True),
            "dve": Engine(self, "dve", True),
            "pool": Engine(self, "pool", True),
            "sp": Engine(self, "sp", True),
        }
        self.lanes = {}
        self.n_inst = 0

    def _new_sem(self, name):
        cm = self.nc.semaphore(name)
        s = cm.__enter__()
        self._stack.append(cm)
        self._semcount += 1
        return s

    def sbuf(self, name, shape, dtype):
        cm = self.nc.sbuf_tensor(name, list(shape), dtype)
        t = cm.__enter__()
        self._stack.append(cm)
        return t

    def psum(self, name, shape, dtype):
        cm = self.nc.psum_tensor(name, list(shape), dtype)
        t = cm.__enter__()
        self._stack.append(cm)
        return t

    def lane_pool(self, name, n):
        self.lanes[name] = dict(tracks=[Track(self, f"{name}{i}", 16) for i in range(n)], rr=0)

    def _waits(self, engine, reads, writes):
        need = {}
        for b in reads:
            if b.lw is not None:
                k = (b.lw[0], b.lw[1])
                need[k] = max(need.get(k, 0), b.lw[2])
        for b in writes:
            if b.lw is not None:
                k = (b.lw[0], b.lw[1])
                need[k] = max(need.get(k, 0), b.lw[2])
            for k, i in b.rd.items():
                need[k] = max(need.get(k, 0), i)
        out = []
        for k, i in need.items():
            tr, ep = k
            if tr is engine.track and not engine.strict_self:
                continue
            if engine.seen.get(k, 0) >= i:
                continue
            engine.seen[k] = i
            out.append((tr.sems[ep], i * tr.step))
        return out

    def _commit(self, tok, reads, writes):
        k = (tok[0], tok[1])
        for b in reads:
            b.rd[k] = max(b.rd.get(k, 0), tok[2])
        for b in writes:
            b.lw = tok
            b.rd = {}

    def op(self, ename, fn, reads=(), writes=()):
        e = self.eng[ename]
        if any(b.excl for b in reads):
            writes = list(writes) + [b for b in reads if b.excl]
            reads = [b for b in reads if not b.excl]
        waits = self._waits(e, reads, writes)
        tok = e.track.next_token()
        sem = tok[0].sems[tok[1]]
        e.seen[(tok[0], tok[1])] = max(e.seen.get((tok[0], tok[1]), 0), 0)

        def run(eng, waits=waits, fn=fn, sem=sem):
            for s, v in waits:
                eng.wait_ge(s, v)
            fn(eng).then_inc(sem, 1)
        e.ops.append(run)
        self._commit(tok, reads, writes)
        self.n_inst += 1
        return tok

    def dma(self, ename, pool, out_ap, in_ap, reads=(), writes=(), **kw):
        e = self.eng[ename]
        lp = self.lanes[pool]
        tr = lp["tracks"][lp["rr"] % len(lp["tracks"])]
        lp["rr"] += 1
        waits = self._waits(e, reads, writes)
        if tr.idx > 0:
            k = (tr, tr.epoch)
            if e.seen.get(k, 0) < tr.idx:
                e.seen[k] = tr.idx
                waits.append((tr.sems[tr.epoch], tr.idx * 16))
        tok = tr.next_token()
        sem = tr.sems[tok[1]]

        def run(eng, waits=waits, sem=sem, out_ap=out_ap, in_ap=in_ap, kw=kw):
            for s, v in waits:
                eng.wait_ge(s, v)
            eng.dma_start(out=out_ap, in_=in_ap, **kw).then_inc(sem, 16)
        e.ops.append(run)
        self._commit(tok, reads, writes)
        self.n_inst += 1
        return tok

    def final_wait(self, ename, bufs):
        e = self.eng[ename]
        waits = self._waits(e, bufs, ())

        def run(eng, waits=waits):
            for s, v in waits:
                eng.wait_ge(s, v)
        e.ops.append(run)

    def emit(self):
        nc = self.nc
        with nc.Block() as block:
            @block.tensor
            def _(eng):
                for f in self.eng["pe"].ops:
                    f(eng)

            @block.scalar
            def _(eng):
                for f in self.eng["act"].ops:
                    f(eng)

            @block.vector
            def _(eng):
                for f in self.eng["dve"].ops:
                    f(eng)

            @block.gpsimd
            def _(eng):
                for f in self.eng["pool"].ops:
                    f(eng)

            @block.sync
            def _(eng):
                for f in self.eng["sp"].ops:
                    f(eng)

    def close(self):
        while self._stack:
            self._stack.pop().__exit__(None, None, None)


def _fence(fw):
    tracks = [e.track for e in fw.eng.values()]
    for lp in fw.lanes.values():
        tracks += lp["tracks"]
    for e in fw.eng.values():
        waits = []
        for tr in tracks:
            if tr is e.track or tr.idx == 0:
                continue
            k = (tr, tr.epoch)
            if e.seen.get(k, 0) >= tr.idx:
                continue
            e.seen[k] = tr.idx
            waits.append((tr.sems[tr.epoch], tr.idx * tr.step))

        def run(eng, waits=waits):
            for s, v in waits:
                eng.wait_ge(s, v)
        e.ops.append(run)


F32 = mybir.dt.float32
BF16 = mybir.dt.bfloat16
AF = mybir.ActivationFunctionType
ALU = mybir.AluOpType

D = 2048
KC = 16
DFF = 5632
FC = 44
NBC = 2
NCORES = 8
EPS = 1e-6
LRU_C = 8.0

_SM = {}
_off = 0
for _n, _w in [("cT", 32), ("adab", 288), ("gpre", 96), ("gpost", 96), ("evconv", 96),
               ("alog", 8), ("dtb", 8), ("onorm", 128), ("retnorm", 1024),
               ("odconv", 64), ("odconvb", 16), ("gab", 16), ("gxb", 16), ("lam", 16),
               ("ident", 128), ("ones", 128), ("tri", 128), ("maskT", 128), ("smask", 128),
               ("decayT", 512), ("gq", 512), ("gk", 4)]:
    _SM[_n] = (_off, _w)
    _off += _w
NS = _off


FULL_PLAN = [("ffn", 0, 0, 0), ("gdn", 1), ("ffn", 0, 1, 2), ("ffn", 1, 0, 3), ("rglru", 4), ("ffn", 1, 1, 5)]


def build(plan=FULL_PLAN, T=2048):
    NTOK = NBC * T
    nc = bass.Bass("TRN2", target_bir_lowering=False)

    def din(name, shape, dt=F32):
        return nc.dram_tensor(name, list(shape), dt, kind="ExternalInput").ap()

    x_in = din("x", [NTOK, D])
    smalls_d = din("smalls", [128, NS])
    cs_d = din("cs", [128, 2, T])
    adaw_d = din("adaw", [2 * 144, 128, 2048])
    w13_d = din("w13r", [2 * 2 * FC, 128, 4096])
    w2_d = din("w2r", [2 * 2 * 16 * 2, 128, 22 * 128])
    evin_d = din("evin", [64, 128, 2048])
    evtail_d = din("evtail", [128, 256])
    evout_d = din("evout", [16, 128, 2048])
    odin_d = din("odin", [32, 128, 2048])
    odout_d = din("odout", [16, 128, 2048])
    gatew_d = din("gatew", [8, 128, 1024])
    out_d = nc.dram_tensor("out", [NTOK, D], F32, kind="ExternalOutput").ap()
    xs_d = nc.dram_tensor("xs", [KC, 128, NTOK], F32).ap()
    xs_v = xs_d.rearrange("k p t -> p k t")
    wcs_ffn = [nc.dram_tensor(f"wcs{i}", [128, 44 * 4096 + 32 * 2816], BF16).ap() for i in range(4)]
    wcs_mix = nc.dram_tensor("wcsm", [128, 64 * 2048 + 256 + 16 * 2048 + 32 * 2048 + 16 * 2048], BF16).ap()

    fw = FW(nc)
    fw_halo = fw.sbuf("halo", [128, 96], F32)
    fw_hst = fw.sbuf("hst", [128, 16], F32)
    fw_c1 = fw.sbuf("c1", [128, 32], F32)
    fw.lane_pool("w", 6)
    fw.lane_pool("wb", 6)
    fw.lane_pool("ws", 4)
    fw.lane_pool("a", 6)

    sm = fw.sbuf("sm", [128, NS], F32)
    smb = Buf(sm)

    def S(name, lo=0, hi=None):
        o, w = _SM[name]
        hi = w if hi is None else hi
        return sm[:, o + lo:o + hi]

    XYt = fw.sbuf("XY", [128, KC, 512], F32)
    XY = [Buf(XYt[:, k, :]) for k in range(KC)]
    hTt = fw.sbuf("hT", [128, KC, 512], BF16)
    hT = [Buf(hTt[:, k, :]) for k in range(KC)]
    BIG = fw.sbuf("BIG", [128, 11264], F32)
    WS = [Buf(fw.sbuf(f"ws{i}", [128, 4096], BF16)) for i in range(5)]
    wsi = [0]

    def wslot():
        b = WS[wsi[0] % len(WS)]
        wsi[0] += 1
        return b
    WSH = [Buf(WS[i][:, j * 2048:(j + 1) * 2048]) for i in range(len(WS)) for j in range(2)]
    wshi = [0]

    def wslot_h():
        b = WSH[wshi[0] % len(WSH)]
        wshi[0] += 1
        return b
    xch = [Buf(fw.sbuf(f"xch{i}", [128, 512], F32)) for i in range(2)]
    sqb = [Buf(fw.sbuf(f"sq{i}", [128, 512], F32)) for i in range(2)]
    tmpb = [Buf(fw.sbuf(f"tmp{i}", [128, 512], F32)) for i in range(3)]
    rstd = Buf(fw.sbuf("rstd", [128, 512], F32))
    modT = fw.sbuf("modT", [128, 2 * 144 * 2], F32)
    modTb = Buf(modT)
    Ab = fw.sbuf("Ab", [128, 6 * KC * 2], F32)
    Bb = fw.sbuf("Bb", [128, 6 * KC * 2], F32)
    Cb = fw.sbuf("Cb", [128, 6 * KC * 2], F32)
    ABCb = Buf(Ab)
    silc = Buf(fw.sbuf("silc", [128, 32], BF16))
    fws = [Buf(fw.sbuf(f"fws{i}", [128, 1024], F32)) for i in range(2)]
    gxb_ = [Buf(fw.sbuf(f"gx{i}", [128, 512], F32)) for i in range(4)]

    pst = [fw.psum(f"ps{i}", [128, 512], F32) for i in range(8)]
    BANK = [Buf(pst[i], excl=True) for i in range(8)]
    PSB = BANK[0:4]
    PSQ = [BANK[2 + i].view(pst[2 + i][:, 0:128]) for i in range(6)]
    pbi = [0]
    pqi = [0]

    def psb():
        b = PSB[pbi[0] % 4]
        pbi[0] += 1
        return b

    def psq():
        b = PSQ[pqi[0] % 6]
        pqi[0] += 1
        return b

    xsb = [Buf(None, f"xs{i}") for i in range(NTOK // 256)]
    outb = Buf(None, "out")

    rr = [0]

    def ew2():
        rr[0] += 1
        return "dve" if rr[0] % 2 else "act"

    def mm(ps, ps_ap, lhsT, rhs, start, stop, reads):
        fw.op("pe", lambda e: e.matmul(ps_ap, lhsT, rhs, start=start, stop=stop), reads=reads, writes=[ps])

    def tr(ps, ps_ap, in_ap, reads):
        fw.op("pe", lambda e: e.transpose(ps_ap, in_ap, S("ident")), reads=reads + [smb], writes=[ps])

    def act(out, in_, func, reads, writes, bias=None, scale=None):
        kw = {}
        if bias is not None:
            kw["bias"] = bias
        if scale is not None:
            kw["scale"] = scale
        fw.op("act", lambda e: e.activation(out=out, in_=in_, func=func, **kw), reads=reads, writes=writes)

    def copy(eng, out, in_, reads, writes):
        if eng == "act":
            act(out, in_, AF.Copy, reads, writes)
        else:
            fw.op(eng, lambda e: e.tensor_copy(out, in_), reads=reads, writes=writes)

    def ts(eng, out, in0, s1, s2, op0, op1, reads, writes):
        if op1 is None:
            fw.op(eng, lambda e: e.tensor_scalar(out, in0, s1, None, op0), reads=reads, writes=writes)
        else:
            fw.op(eng, lambda e: e.tensor_scalar(out, in0, s1, s2, op0, op1), reads=reads, writes=writes)

    def stt(eng, out, in0, sc, in1, op0, op1, reads, writes):
        fw.op(eng, lambda e: e.scalar_tensor_tensor(out, in0, sc, in1, op0, op1), reads=reads, writes=writes)

    def tt(eng, out, in0, in1, op, reads, writes):
        fw.op(eng, lambda e: e.tensor_tensor(out, in0, in1, op), reads=reads, writes=writes)

    def wload(slot, dst_ap, src_ap):
        fw.dma("pool", "w", dst_ap, src_ap, writes=[slot])

    wcache = {}
    wc_off = {}

    def wget(key, slot, ncols, src_ap):
        dst = slot[:, 0:ncols]
        if key in wcache:
            dap, dbuf = wcache[key]
            fw.dma("sp", "wb", dst, dap, reads=[dbuf], writes=[slot])
        else:
            wload(slot, dst, src_ap)
            if key[0] == "w13":
                gidx = key[1] * 2 + key[2]
            elif key[0] == "w2":
                gidx = key[1] // 32
            else:
                gidx = 4
            wt = wcs_ffn[gidx] if gidx < 4 else wcs_mix
            o = wc_off.get(gidx, 0)
            wc_off[gidx] = o + ncols
            dap = wt[:, o:o + ncols]
            dbuf = Buf(None, "wc")
            fw.dma("sp", "ws", dap, dst, reads=[slot], writes=[dbuf])
            wcache[key] = (dap, dbuf)

    fw.dma("sp", "a", sm[:, :], smalls_d[:, :], writes=[smb])
    act(silc[:], S("cT"), AF.Silu, [smb], [silc])
    for l in range(2):
        for q in range(144):
            slot = wslot()
            wload(slot, slot[:, 0:2048], adaw_d[l * 144 + q])
            ps = psq()
            for k in range(KC):
                mm(ps, ps[:, 0:2], slot[:, k * 128:(k + 1) * 128], silc[:, k * 2:k * 2 + 2], k == 0, k == KC - 1, [slot, silc])
            o = (l * 144 + q) * 2
            oa = _SM["adab"][0] + l * 144 + q
            ts("dve", modT[:, o:o + 2], ps[:, 0:2], sm[:, oa:oa + 1], None, ALU.add, None, [ps, smb], [modTb])
    mv = modT[:, :].rearrange("p (l q b) -> p l q b", l=2, q=144)
    Av = Ab[:, :].rearrange("p (s k b) -> p s k b", s=6, k=KC)
    Bv = Bb[:, :].rearrange("p (s k b) -> p s k b", s=6, k=KC)
    Cv = Cb[:, :].rearrange("p (s k b) -> p s k b", s=6, k=KC)
    for l in range(2):
        for s in range(3):
            ls = l * 3 + s
            resw = 1.0 if s == 1 else 0.5
            gp = S("gpre", ls * KC, (ls + 1) * KC)
            gq_ = S("gpost", ls * KC, (ls + 1) * KC)
            for b in range(NBC):
                sh = mv[:, l, (s * 3 + 0) * KC:(s * 3 + 1) * KC, b]
                sc = mv[:, l, (s * 3 + 1) * KC:(s * 3 + 2) * KC, b]
                ga = mv[:, l, (s * 3 + 2) * KC:(s * 3 + 3) * KC, b]
                stt("dve", Av[:, ls, :, b], sc, 1.0, gp, ALU.add, ALU.mult, [modTb, smb], [ABCb])
                copy("dve", Bv[:, ls, :, b], sh, [modTb], [ABCb])
                stt("dve", Cv[:, ls, :, b], ga, 1.0, gq_, ALU.add, ALU.mult, [modTb, smb], [ABCb])
                ts("dve", Cv[:, ls, :, b], Cv[:, ls, :, b], resw, None, ALU.mult, None, [ABCb], [ABCb])

    def Asc(tab, ls, k, b):
        return tab[:, ls, k, b:b + 1]

    def sumsq_rstd(srcs, TT, scale, eps):
        ps = psb()
        n = len(srcs)
        for k, (b, ap) in enumerate(srcs):
            sq = sqb[k % 2]
            act(sq[:, :TT], ap, AF.Square, [b], [sq])
            mm(ps, ps[:, :TT], S("ones"), sq[:, :TT], k == 0, k == n - 1, [sq, smb])
        act(rstd[:, :TT], ps[:, :TT], AF.Sqrt, [ps], [rstd], bias=eps, scale=scale)
        fw.op("dve", lambda e: e.reciprocal(rstd[:, :TT], rstd[:, :TT]), reads=[rstd], writes=[rstd])

    def prologue(ls, t0, TT, b):
        nb = TT // 256
        xbufs = [xsb[t0 // 256 + i] for i in range(nb)]
        fw.dma("sp", "a", XYt[:, :, :TT], xs_v[:, :, t0:t0 + TT], reads=xbufs, writes=XY)
        sumsq_rstd([(XY[k], XY[k][:, :TT]) for k in range(KC)], TT, 1.0 / D, EPS)
        for k in range(KC):
            tm = tmpb[k % 3]
            stt("dve", tm[:, :TT], XY[k][:, :TT], Asc(Av, ls, k, b), rstd[:, :TT], ALU.mult, ALU.mult, [XY[k], rstd, ABCb], [tm])
            act(hT[k][:, :TT], tm[:, :TT], AF.Identity, [tm, ABCb], [hT[k]], bias=Asc(Bv, ls, k, b))

    def epilogue(ls, t0, TT, b):
        nb = TT // 256
        xbufs = [xsb[t0 // 256 + i] for i in range(nb)]
        sumsq_rstd([(XY[k], XY[k][:, :TT]) for k in range(KC)], TT, 1.0 / D, EPS)
        for k in range(KC):
            xc = xch[k % 2]
            fw.dma("sp", "a", xc[:, :TT], xs_d[k, :, t0:t0 + TT], reads=xbufs, writes=[xc])
            tm = tmpb[k % 3]
            stt("dve", tm[:, :TT], XY[k][:, :TT], Asc(Cv, ls, k, b), rstd[:, :TT], ALU.mult, ALU.mult, [XY[k], rstd, ABCb], [tm])
            tt("dve", XY[k][:, :TT], tm[:, :TT], xc[:, :TT], ALU.add, [tm, xc], [XY[k]])
        fw.dma("sp", "a", xs_v[:, :, t0:t0 + TT], XYt[:, :, :TT], reads=XY, writes=xbufs)

    def proj_fm(wd_piece, TT, consume, key):
        slot = wslot_h()
        wget(key, slot, 2048, wd_piece)
        ps = psb()
        for k in range(KC):
            mm(ps, ps[:, :TT], slot[:, k * 128:(k + 1) * 128], hT[k][:, :TT], k == 0, k == KC - 1, [slot, hT[k]])
        consume(ps)

    xin = [Buf(BIG[:, i * 2048:(i + 1) * 2048]) for i in range(4)]
    for t0 in range(0, NTOK, 512):
        for blk in range(4):
            fw.dma("sp", "a", xin[blk][:], x_in[t0 + blk * 128:t0 + (blk + 1) * 128, :], writes=[xin[blk]])
        for k in range(KC):
            ps = psb()
            for blk in range(4):
                tr(ps, ps[:, blk * 128:(blk + 1) * 128], xin[blk][:, k * 128:(k + 1) * 128], [xin[blk]])
            copy(ew2(), XY[k][:], ps[:], [ps], [XY[k]])
        fw.dma("sp", "a", xs_v[:, :, t0:t0 + 512], XYt[:, :, :], reads=XY, writes=[xsb[t0 // 256], xsb[t0 // 256 + 1]])
    _fence(fw)

    def ffn_sandwich(l, f, ls):
        actT = BIG[:, :].bitcast(BF16)
        actb = [Buf(actT[:, j * 512:(j + 1) * 512]) for j in range(FC)]
        TT = 512
        xcb = xch
        sq_, acc_ = sqb
        rstdP = rstd
        tmP, tmE0, tmE1, rstdE = gxb_
        tmE = [tmE0, tmE1]

        def sumsq_acc(k, src_b, src_ap):
            act(sq_[:], src_ap, AF.Square, [src_b], [sq_])
            if k == 0:
                copy("dve", acc_[:], sq_[:], [sq_], [acc_])
            else:
                tt("dve", acc_[:], acc_[:], sq_[:], ALU.add, [acc_, sq_], [acc_])

        def finish_rstd(dst):
            ps = psb()
            mm(ps, ps[:], S("ones"), acc_[:], True, True, [acc_, smb])
            act(dst[:], ps[:], AF.Sqrt, [ps], [dst], bias=EPS, scale=1.0 / D)
            fw.op("dve", lambda e: e.reciprocal(dst[:], dst[:]), reads=[dst], writes=[dst])

        def pro_gen(t0):
            b = t0 // T
            xbufs = [xsb[t0 // 256], xsb[t0 // 256 + 1]]

            def ld(k):
                xc = xcb[k % 2]
                fw.dma("pool", "a", xc[:], xs_d[k, :, t0:t0 + TT], reads=xbufs, writes=[xc])
            ld(0)
            yield
            for k in range(KC):
                if k + 1 < KC:
                    ld(k + 1)
                sumsq_acc(k, xcb[k % 2], xcb[k % 2][:])
                yield
            ld(0)
            finish_rstd(rstdP)
            yield
            for k in range(KC):
                if k + 1 < KC:
                    ld(k + 1)
                xc = xcb[k % 2]
                stt("dve", tmP[:], xc[:], Asc(Av, ls, k, b), rstdP[:], ALU.mult, ALU.mult, [xc, rstdP, ABCb], [tmP])
                act(hT[k][:], tmP[:], AF.Identity, [tmP, ABCb], [hT[k]], bias=Asc(Bv, ls, k, b))
                yield

        def epi_gen(t0):
            b = t0 // T
            xbufs = [xsb[t0 // 256], xsb[t0 // 256 + 1]]

            def ld(k):
                xc = xcb[k % 2]
                fw.dma("pool", "a", xc[:], xs_d[k, :, t0:t0 + TT], reads=xbufs, writes=[xc])
            for k in range(KC):
                sumsq_acc(k, XY[k], XY[k][:])
                yield
            ld(0)
            finish_rstd(rstdE)
            yield
            for k in range(KC):
                if k + 1 < KC:
                    ld(k + 1)
                xc = xcb[k % 2]
                te = tmE[k % 2]
                stt("dve", te[:], XY[k][:], Asc(Cv, ls, k, b), rstdE[:], ALU.mult, ALU.mult, [XY[k], rstdE, ABCb], [te])
                tt("dve", te[:], te[:], xc[:], ALU.add, [te, xc], [te])
                fw.dma("pool", "a", xs_d[k, :, t0:t0 + TT], te[:], reads=[te], writes=xbufs)
                yield

        def pull(g, n):
            if g is None:
                return None
            for _ in range(n):
                try:
                    next(g)
                except StopIteration:
                    return None
            return g

        def drain(g):
            while g is not None:
                g = pull(g, 1)

        tiles = list(range(0, NTOK, TT))
        drain(pro_gen(tiles[0]))
        eg = None
        for ti, t0 in enumerate(tiles):
            for j in range(FC):
                slot = wslot()
                wget(("w13", l, f, j), slot, 4096, w13_d[(l * 2 + f) * FC + j])
                pg = psb()
                pu = psb()
                for k in range(KC):
                    mm(pg, pg[:], slot[:, k * 128:(k + 1) * 128], hT[k][:], k == 0, k == KC - 1, [slot, hT[k]])
                for k in range(KC):
                    mm(pu, pu[:], slot[:, 2048 + k * 128:2048 + (k + 1) * 128], hT[k][:], k == 0, k == KC - 1, [slot, hT[k]])
                tm = tmpb[j % 3]
                act(tm[:], pg[:], AF.Silu, [pg], [tm])
                tt("dve", actb[j][:], tm[:], pu[:], ALU.mult, [tm, pu], [actb[j]])
                eg = pull(eg, 1)
            drain(eg)
            pgn = pro_gen(tiles[ti + 1]) if ti + 1 < len(tiles) else None
            for m in range(KC):
                s0 = wslot()
                s1 = wslot()
                base = (((l * 2 + f) * 16) + m) * 2
                wget(("w2", base), s0, 2816, w2_d[base])
                wget(("w2", base + 1), s1, 2816, w2_d[base + 1])
                ps = psb()
                for c in range(FC):
                    sl = s0 if c < 22 else s1
                    cc = c % 22
                    mm(ps, ps[:], sl[:, cc * 128:(cc + 1) * 128], actb[c][:], c == 0, c == FC - 1, [sl, actb[c]])
                copy(ew2(), XY[m][:], ps[:], [ps], [XY[m]])
                pgn = pull(pgn, 3)
            drain(pgn)
            eg = epi_gen(t0)
        drain(eg)
        _fence(fw)

    def rglru_sandwich(ls):
        TT = 512
        ygt = BIG[:, 0:4096].bitcast(BF16)
        yg = [Buf(ygt[:, k * 512:(k + 1) * 512]) for k in range(KC)]
        xcv = [Buf(BIG[:, 4096 + i * 512:4096 + (i + 1) * 512]) for i in range(4)]
        xqb = [Buf(BIG[:, 6144 + i * 520:6144 + i * 520 + 515]) for i in range(2)]
        rb = [Buf(BIG[:, 7200 + i * 512:7200 + (i + 1) * 512]) for i in range(2)]
        ib = [Buf(BIG[:, 8224 + i * 512:8224 + (i + 1) * 512]) for i in range(2)]
        ab = [Buf(BIG[:, 9248 + i * 512:9248 + (i + 1) * 512]) for i in range(2)]
        hsb = [Buf(BIG[:, 10272:10784])]
        halo = Buf(fw_halo[:, 0:48])
        hstate = Buf(fw_hst[:, 0:16])
        c1 = Buf(fw_c1[:, 0:32])
        act(c1[:, 0:16], S("lam"), AF.Exp, [smb], [c1], scale=-1.0)
        act(c1[:, 0:16], c1[:, 0:16], AF.Ln, [c1], [c1], bias=1.0)
        ts("dve", c1[:, 16:32], c1[:, 0:16], LRU_C, None, ALU.mult, None, [c1], [c1])
        ts("dve", c1[:, 0:16], c1[:, 0:16], -LRU_C, None, ALU.mult, None, [c1], [c1])
        for t0 in range(0, NTOK, TT):
            b = t0 // T
            if t0 % T == 0:
                fw.op("dve", lambda e: e.memset(halo[:], 0.0), writes=[halo])
                fw.op("dve", lambda e: e.memset(hstate[:], 0.0), writes=[hstate])
            prologue(ls, t0, TT, b)
            def y_chunk(m):
                def cons(ps, m=m):
                    t1 = tmpb[0]
                    t2 = tmpb[1]
                    copy("act", t1[:], ps[:], [ps], [t1])
                    tt("pool", t2[:], t1[:], t1[:], ALU.mult, [t1], [t2])
                    ts("pool", t2[:], t2[:], 0.044715, 1.0, ALU.mult, ALU.add, [t2], [t2])
                    tt("pool", t2[:], t2[:], t1[:], ALU.mult, [t1, t2], [t2])
                    act(t2[:], t2[:], AF.Sigmoid, [t2], [t2], scale=1.5957691216057308)
                    tt("dve", yg[m][:], t1[:], t2[:], ALU.mult, [t1, t2], [yg[m]])
                proj_fm(odin_d[m], TT, cons, ("odin", m))

            def x_block(n):
                gsl = fws[n % 2]
                fw.dma("sp", "w", gsl[:, 0:1024], gatew_d[n], writes=[gsl])
                xcs = []
                for ic in range(2):
                    ci = n * 2 + ic
                    xq = xqb[ic]
                    xc = xcv[(n % 2) * 2 + ic]

                    def cons(ps, ci=ci, xq=xq, xc=xc):
                        copy("dve", xq[:, 0:3], halo[:, ci * 3:ci * 3 + 3], [halo], [xq])
                        copy("act", xq[:, 3:515], ps[:], [ps], [xq])
                        copy("dve", halo[:, ci * 3:ci * 3 + 3], xq[:, 512:515], [xq], [halo])
                        o = _SM["odconv"][0] + ci * 4
                        ob = _SM["odconvb"][0] + ci
                        ts("dve", xc[:], xq[:, 0:512], sm[:, o:o + 1], sm[:, ob:ob + 1], ALU.mult, ALU.add, [xq, smb], [xc])
                        for tap in range(1, 4):
                            stt("dve", xc[:], xq[:, tap:tap + 512], sm[:, o + tap:o + tap + 1], xc[:], ALU.mult, ALU.add, [xq, smb, xc], [xc])
                    proj_fm(odin_d[16 + ci], TT, cons, ("odin", 16 + ci))
                    xcs.append(xc)
                return gsl, xcs

            def gates(n, gsl, xcs):
                for jc in range(2):
                    cj = n * 2 + jc
                    pr = psb()
                    pi = psb()
                    for g, pp in ((0, pr), (1, pi)):
                        for ic in range(2):
                            o = ((g * 2 + jc) * 2 + ic) * 128
                            mm(pp, pp[:], gsl[:, o:o + 128], xcs[ic][:], ic == 0, ic == 1, [gsl, xcs[ic]])
                    r_ = rb[jc]
                    i_ = ib[jc]
                    a_ = ab[jc]
                    oga = _SM["gab"][0] + cj
                    ogx = _SM["gxb"][0] + cj
                    act(r_[:], pr[:], AF.Sigmoid, [pr, smb], [r_], bias=sm[:, oga:oga + 1])
                    act(i_[:], pi[:], AF.Sigmoid, [pi, smb], [i_], bias=sm[:, ogx:ogx + 1])
                    act(a_[:], r_[:], AF.Exp, [r_, c1], [a_], scale=c1[:, cj:cj + 1])
                    act(r_[:], r_[:], AF.Tanh, [r_, c1], [r_], scale=c1[:, 16 + cj:16 + cj + 1])
                    t2 = tmpb[2]
                    tt("dve", t2[:], a_[:], a_[:], ALU.mult, [a_], [t2])
                    stt("dve", t2[:], t2[:], 1.0, r_[:], ALU.add, ALU.mult, [t2, r_], [t2])
                    act(t2[:], t2[:], AF.Sqrt, [t2], [t2])
                    tt("pool", i_[:], i_[:], xcs[jc][:], ALU.mult, [i_, xcs[jc]], [i_])
                    tt("dve", t2[:], t2[:], i_[:], ALU.mult, [t2, i_], [t2])
                    hs = hsb[0]
                    fw.op("dve", lambda e, hs=hs, a_=a_, t2=t2, cj=cj: e.tensor_tensor_scan(hs[:], a_[:], t2[:], hstate[:, cj:cj + 1], ALU.mult, ALU.add),
                          reads=[a_, t2, hstate], writes=[hs])
                    copy("dve", hstate[:, cj:cj + 1], hs[:, 511:512], [hs], [hstate])
                    tt("dve", yg[cj][:], hs[:], yg[cj][:], ALU.mult, [hs, yg[cj]], [yg[cj]])

            pend = None
            for n in range(8):
                y_chunk(2 * n)
                y_chunk(2 * n + 1)
                cur = x_block(n)
                if pend is not None:
                    gates(*pend)
                pend = (n,) + cur
            gates(*pend)
            for m in range(KC):
                slot = wslot_h()
                wget(("odout", m), slot, 2048, odout_d[m])
                ps = psb()
                for k in range(KC):
                    mm(ps, ps[:], slot[:, k * 128:(k + 1) * 128], yg[k][:], k == 0, k == KC - 1, [slot, yg[k]])
                copy(ew2(), XY[m][:], ps[:], [ps], [XY[m]])
            epilogue(ls, t0, TT, b)
        _fence(fw)


    def gdn_sandwich(ls):
        TT = 256
        _, gdec = _consts()
        AX = mybir.AxisListType.X
        oall = [Buf(BIG[:, blk * 2048:(blk + 1) * 2048]) for blk in range(2)]
        xq = [Buf(BIG[:, 4096 + i * 264:4096 + i * 264 + 259]) for i in range(3)]
        qkvS = [[Buf(BIG[:, 4888 + s * 768 + i * 256:4888 + s * 768 + (i + 1) * 256]) for i in range(3)] for s in range(2)]
        ktokS = [Buf(BIG[:, 6424 + s * 768:6424 + s * 768 + 256]) for s in range(2)]
        vtokS = [Buf(BIG[:, 6680 + s * 768:6680 + s * 768 + 256]) for s in range(2)]
        ztokS = [Buf(BIG[:, 6936 + s * 768:6936 + s * 768 + 256]) for s in range(2)]
        G = [Buf(BIG[:, 8192 + i * 128:8192 + (i + 1) * 128]) for i in range(24)]
        G += [Buf(XYt[:, k, 256 + j * 128:256 + (j + 1) * 128]) for k in range(KC) for j in range(2)]

        class _R:
            pass
        roles = {}
        gi = 0
        for s in range(2):
            for blk in range(2):
                C = _R()
                (C.P0, C.N0, C.Pa, C.Na, C.Xa, C.Xb, C.QKT, C.kegT, C.qstT, C.kst) = G[gi:gi + 10]
                gi += 10
                roles[(s, blk)] = C
        hroles = []
        for s in range(2):
            Hh = _R()
            (Hh.Rb, Hh.vnew, Hh.osq, Hh.ob) = G[gi:gi + 4]
            gi += 4
            hroles.append(Hh)
        scT, qin0, qin1 = G[gi:gi + 3]
        osq, ob = hroles[0].osq, hroles[0].ob
        rq = [Buf(BIG[:, 4096 + i * 256:4096 + (i + 1) * 256]) for i in range(2)]
        rk = [Buf(BIG[:, 4608 + i * 256:4608 + (i + 1) * 256]) for i in range(2)]
        rqr = [Buf(BIG[:, 5120 + i * 256:5120 + (i + 1) * 256]) for i in range(2)]
        rkr = [Buf(BIG[:, 5632 + i * 256:5632 + (i + 1) * 256]) for i in range(2)]
        rtm = [Buf(BIG[:, 6144 + i * 256:6144 + (i + 1) * 256]) for i in range(2)]
        rktok = Buf(BIG[:, 6656:7168])
        rvtok = Buf(BIG[:, 7168:7680])
        rgtok = Buf(BIG[:, 7680:8192])
        gsm = fw.sbuf("gsm", [128, 8 * 16 + 8], F32)
        gsmb = Buf(gsm)
        Sg_t = fw.sbuf("Sg", [128, 1024], F32)
        Sg = [Buf(Sg_t[:, h * 128:(h + 1) * 128]) for h in range(8)]
        Sr_t = fw.sbuf("Sr", [128, 2048], F32)
        Sr = [Buf(Sr_t[:, h * 512:(h + 1) * 512]) for h in range(4)]
        cst = fw.sbuf("cst", [128, 2, 256], F32)
        cstb = Buf(cst)
        halo = Buf(fw_halo[:, 0:72])
        ssb_t = fw.sbuf("ssb", [128, 4], F32)
        ssb = Buf(ssb_t[:, 0:2])
        ssbS = [Buf(ssb_t[:, 2 + s:3 + s]) for s in range(2)]

        def gs(i, blk, lo=0, hi=8):
            return gsm[:, (i * 2 + blk) * 8 + lo:(i * 2 + blk) * 8 + hi]
        NEGA = gsm[:, 128:136]
        act(NEGA, S("alog"), AF.Exp, [smb], [gsmb])
        ts("dve", NEGA, NEGA, -1.0, None, ALU.mult, None, [gsmb], [gsmb])

        def rowsum_rstd(ps_ap, width, src):
            act(osq[:, :] if width <= 128 else rtm[0][:, :width], ps_ap, AF.Square, [src], [osq if width <= 128 else rtm[0]])
            sqap = osq[:, :] if width <= 128 else rtm[0][:, :width]
            sqb_ = osq if width <= 128 else rtm[0]
            fw.op("dve", lambda e: e.reduce_sum(ssb[:, 0:1], sqap, AX), reads=[sqb_], writes=[ssb])
            act(ssb[:, 0:1], ssb[:, 0:1], AF.Sqrt, [ssb], [ssb], bias=EPS, scale=1.0 / width)
            fw.op("dve", lambda e: e.reciprocal(ssb[:, 0:1], ssb[:, 0:1]), reads=[ssb], writes=[ssb])


        def run_gens(gens):
            active = list(gens)
            while active:
                for g in list(active):
                    try:
                        next(g)
                    except StopIteration:
                        active.remove(g)

        def stage_a_gen(s, h):
            qkv = qkvS[s]
            for idx, m in enumerate((h, 8 + h, 16 + h)):
                def cons(ps, idx=idx, m=m):
                    x_ = xq[idx]
                    copy("dve", x_[:, 0:3], halo[:, m * 3:m * 3 + 3], [halo], [x_])
                    copy("act", x_[:, 3:259], ps[:, :TT], [ps], [x_])
                    copy("dve", halo[:, m * 3:m * 3 + 3], x_[:, 256:259], [x_], [halo])
                    o = _SM["evconv"][0] + m * 4
                    tm = tmpb[idx]
                    ts("dve", tm[:, :TT], x_[:, 0:TT], sm[:, o:o + 1], None, ALU.mult, None, [x_, smb], [tm])
                    for tap in range(1, 4):
                        stt("dve", tm[:, :TT], x_[:, tap:tap + TT], sm[:, o + tap:o + tap + 1], tm[:, :TT], ALU.mult, ALU.add, [x_, smb, tm], [tm])
                    act(qkv[idx][:], tm[:, :TT], AF.Silu, [tm], [qkv[idx]])
                proj_fm(evin_d[m], TT, cons, ("evin", m))
                yield
            qT, kT, vT = qkv
            sumsq_rstd([(qT, qT[:])], TT, 1.0, EPS)
            stt("dve", qT[:], qT[:], 128.0 ** -0.5, rstd[:, :TT], ALU.mult, ALU.mult, [qT, rstd], [qT])
            yield
            sumsq_rstd([(kT, kT[:])], TT, 1.0, EPS)
            tt("dve", kT[:], kT[:], rstd[:, :TT], ALU.mult, [kT, rstd], [kT])
            yield

        def stage_b(s, h):
            qT, kT, vT = qkvS[s]
            zsl = wslot_h()
            wget(("evin", 24 + h), zsl, 2048, evin_d[24 + h])
            ktok, vtok, ztok = ktokS[s], vtokS[s], ztokS[s]
            for blk in range(2):
                ps = psq()
                for k in range(KC):
                    mm(ps, ps[:], hT[k][:, blk * 128:(blk + 1) * 128], zsl[:, k * 128:(k + 1) * 128], k == 0, k == KC - 1, [zsl, hT[k]])
                act(ztok[:, blk * 128:(blk + 1) * 128], ps[:], AF.Silu, [ps], [ztok])
                pk_ = psq()
                tr(pk_, pk_[:], kT[:, blk * 128:(blk + 1) * 128], [kT])
                copy("act", ktok[:, blk * 128:(blk + 1) * 128], pk_[:], [pk_], [ktok])
                pv_ = psq()
                tr(pv_, pv_[:], vT[:, blk * 128:(blk + 1) * 128], [vT])
                copy("dve", vtok[:, blk * 128:(blk + 1) * 128], pv_[:], [pv_], [vtok])

        def chain_gen(s, blk, h):
            C = roles[(s, blk)]
            qT, kT, vT = qkvS[s]
            ktok = ktokS[s]
            cs_ = slice(blk * 128, (blk + 1) * 128)
            bcol = gs(0, blk, h, h + 1)
            ldcol = gs(1, blk, h, h + 1)
            gcol = gs(2, blk, h, h + 1)
            eglcol = gs(5, blk, h, h + 1)
            ldb, egb, DT = C.Pa, C.Na, C.Xb
            P0, N0, QKT, kegT, qstT, kst = C.P0, C.N0, C.QKT, C.kegT, C.qstT, C.kst
            ts("dve", ldb[:], S("ones"), ldcol, None, ALU.mult, None, [smb, gsmb], [ldb])
            pg = psq()
            mm(pg, pg[:], ldb[:], S("tri"), True, True, [ldb, smb])
            act(egb[:], pg[:], AF.Exp, [pg], [egb])
            stt("dve", DT[:], pg[:], gcol, S("maskT"), ALU.subtract, ALU.add, [pg, gsmb, smb], [DT])
            yield
            act(DT[:], DT[:], AF.Exp, [DT], [DT])
            pkk = psq()
            mm(pkk, pkk[:], kT[:, cs_], kT[:, cs_], True, True, [kT])
            pkq = psq()
            mm(pkq, pkq[:], kT[:, cs_], qT[:, cs_], True, True, [kT, qT])
            tt("dve", kegT[:], kT[:, cs_], egb[:], ALU.mult, [kT, egb], [kegT])
            tt("dve", qstT[:], qT[:, cs_], egb[:], ALU.mult, [qT, egb], [qstT])
            act(kst[:], ktok[:, cs_], AF.Copy, [ktok, gsmb], [kst], scale=eglcol)
            stt("dve", P0[:], pkk[:], bcol, DT[:], ALU.mult, ALU.mult, [pkk, gsmb, DT], [P0])
            tt("dve", QKT[:], pkq[:], DT[:], ALU.mult, [pkq, DT], [QKT])
            yield
            tt("dve", P0[:], P0[:], S("smask"), ALU.mult, [P0, smb], [P0])
            pt = psq()
            tr(pt, pt[:], P0[:], [P0])
            copy("act", N0[:], pt[:], [pt], [N0])
            stt("dve", C.Xa[:], P0[:], -1.0, S("ident"), ALU.mult, ALU.add, [P0, smb], [C.Xa])
            yield
            X, Xo = C.Xa, C.Xb
            Pk, Nk = P0, N0
            Pn, Nn = C.Pa, C.Na
            for lvl in range(6):
                if lvl < 5:
                    pp = psq()
                    mm(pp, pp[:], Nk[:], Pk[:], True, True, [Nk, Pk])
                pn = psq()
                mm(pn, pn[:], Pk[:], Nk[:], True, True, [Nk, Pk])
                if lvl < 5:
                    copy("act", Pn[:], pp[:], [pp], [Pn])
                copy("dve", Nn[:], pn[:], [pn], [Nn])
                yield
                px = psq()
                mm(px, px[:], Nn[:], X[:], True, True, [Nn, X])
                tt("dve", Xo[:], X[:], px[:], ALU.add, [X, px], [Xo])
                yield
                X, Xo = Xo, X
                Pk, Nk, Pn, Nn = Pn, Nn, Pk, Nk
            C.X = X

        def sphase_gen(s, h):
            Hh = hroles[s]
            vtok, ztok = vtokS[s], ztokS[s]
            sb_ = ssbS[s]
            for blk in range(2):
                C = roles[(s, blk)]
                cs_ = slice(blk * 128, (blk + 1) * 128)
                bcol = gs(0, blk, h, h + 1)
                deccol = gs(6, blk, h, h + 1)
                pks = psq()
                mm(pks, pks[:], C.kegT[:], Sg[h][:], True, True, [C.kegT, Sg[h]])
                tt("dve", Hh.Rb[:], vtok[:, cs_], pks[:], ALU.subtract, [vtok, pks], [Hh.Rb])
                yield
                pv = psq()
                mm(pv, pv[:], C.X[:], Hh.Rb[:], True, True, [C.X, Hh.Rb])
                ts("dve", Hh.vnew[:], pv[:], bcol, None, ALU.mult, None, [pv, gsmb], [Hh.vnew])
                yield
                po = psq()
                mm(po, po[:], C.qstT[:], Sg[h][:], True, False, [C.qstT, Sg[h]])
                mm(po, po[:], C.QKT[:], Hh.vnew[:], False, True, [C.QKT, Hh.vnew])
                psn = psq()
                mm(psn, psn[:], C.kst[:], Hh.vnew[:], True, True, [C.kst, Hh.vnew])
                act(Hh.osq[:], po[:], AF.Square, [po], [Hh.osq])
                copy("dve", Hh.ob[:], po[:], [po], [Hh.ob])
                stt("dve", Sg[h][:], Sg[h][:], deccol, psn[:], ALU.mult, ALU.add, [Sg[h], gsmb, psn], [Sg[h]])
                yield
                fw.op("dve", lambda e, Hh=Hh, sb_=sb_: e.reduce_sum(sb_[:], Hh.osq[:], AX), reads=[Hh.osq], writes=[sb_])
                yield
                act(sb_[:], sb_[:], AF.Sqrt, [sb_], [sb_], bias=EPS, scale=1.0 / 128)
                yield
                fw.op("dve", lambda e, sb_=sb_: e.reciprocal(sb_[:], sb_[:]), reads=[sb_], writes=[sb_])
                yield
                stt("dve", Hh.ob[:], Hh.ob[:], sb_[:], S("onorm"), ALU.mult, ALU.mult, [Hh.ob, sb_, smb], [Hh.ob])
                yield
                tt("dve", oall[blk][:, h * 128:(h + 1) * 128], Hh.ob[:], ztok[:, cs_], ALU.mult, [Hh.ob, ztok], [oall[blk]])
                yield


        class _RS:
            pass
        RS = []
        for s_ in range(2):
            if s_ == 0:
                pcs = [Buf(BIG[:, 4096 + i * 256:4096 + (i + 1) * 256]) for i in range(16)]
            else:
                pcs = [Buf(XYt[:, k, 256:512]) for k in range(KC)]
            R = _RS()
            R.rq, R.rk, R.rqr, R.rkr, R.rtm = pcs[0:2], pcs[2:4], pcs[4:6], pcs[6:8], pcs[8:10]
            R.rktok, R.rvtok, R.rgtok = pcs[10:12], pcs[12:14], pcs[14:16]
            R.scT, R.qin0, R.qin1 = G[s_ * 3:s_ * 3 + 3]
            RS.append(R)

        def ret_gen(h, R, sb_):
            for i in range(2):
                proj_fm(evin_d[32 + 2 * h + i], TT, lambda ps, i=i: copy("act", R.rq[i][:], ps[:, :TT], [ps], [R.rq[i]]), ("evin", 32 + 2 * h + i))
                yield
            for i in range(2):
                proj_fm(evin_d[40 + 2 * h + i], TT, lambda ps, i=i: copy("dve", R.rk[i][:], ps[:, :TT], [ps], [R.rk[i]]), ("evin", 40 + 2 * h + i))
                yield
            for src, dst in ((R.rq, R.rqr), (R.rk, R.rkr)):
                x1, x2 = src
                t0_, t1_ = R.rtm
                tt("dve", t0_[:], x1[:], cst[:, 0, :], ALU.mult, [x1, cstb], [t0_])
                tt("dve", t1_[:], x2[:], cst[:, 1, :], ALU.mult, [x2, cstb], [t1_])
                tt("dve", dst[0][:], t0_[:], t1_[:], ALU.subtract, [t0_, t1_], [dst[0]])
                yield
                tt("dve", t0_[:], x2[:], cst[:, 0, :], ALU.mult, [x2, cstb], [t0_])
                tt("dve", t1_[:], x1[:], cst[:, 1, :], ALU.mult, [x1, cstb], [t1_])
                tt("dve", dst[1][:], t0_[:], t1_[:], ALU.add, [t0_, t1_], [dst[1]])
                yield
            for base, dstl, fn in ((48, R.rvtok, None), (56, R.rgtok, AF.Silu)):
                sl = [wslot_h(), wslot_h()]
                for i in range(2):
                    wget(("evin", base + 2 * h + i), sl[i], 2048, evin_d[base + 2 * h + i])
                for blk in range(2):
                    ps = psb()
                    for i in range(2):
                        for k in range(KC):
                            mm(ps, ps[:, i * 128:(i + 1) * 128], hT[k][:, blk * 128:(blk + 1) * 128], sl[i][:, k * 128:(k + 1) * 128], k == 0, k == KC - 1, [sl[i], hT[k]])
                    if fn is None:
                        copy("dve", dstl[blk][:], ps[:, 0:256], [ps], [dstl[blk]])
                    else:
                        act(dstl[blk][:], ps[:, 0:256], fn, [ps], [dstl[blk]])
                    yield
            og = _SM["gk"][0] + h
            for blk in range(2):
                for i in range(2):
                    pk_ = psq()
                    tr(pk_, pk_[:], R.rkr[i][:, blk * 128:(blk + 1) * 128], [R.rkr[i]])
                    ts("dve", R.rktok[blk][:, i * 128:(i + 1) * 128], pk_[:], sm[:, og:og + 1], None, ALU.mult, None, [pk_, smb], [R.rktok[blk]])
                yield
            for blk in range(2):
                cs_ = slice(blk * 128, (blk + 1) * 128)
                vb_ = R.rvtok[blk]
                psc = psq()
                mm(psc, psc[:], R.rkr[0][:, cs_], R.rqr[0][:, cs_], True, False, [R.rkr[0], R.rqr[0]])
                mm(psc, psc[:], R.rkr[1][:, cs_], R.rqr[1][:, cs_], False, True, [R.rkr[1], R.rqr[1]])
                tt("dve", R.scT[:], psc[:], S("decayT", h * 128, (h + 1) * 128), ALU.mult, [psc, smb], [R.scT])
                tt("dve", R.qin0[:], R.rqr[0][:, cs_], S("gq", h * 128, (h + 1) * 128), ALU.mult, [R.rqr[0], smb], [R.qin0])
                tt("dve", R.qin1[:], R.rqr[1][:, cs_], S("gq", h * 128, (h + 1) * 128), ALU.mult, [R.rqr[1], smb], [R.qin1])
                yield
                po = psb()
                mm(po, po[:, 0:256], R.scT[:], vb_[:], True, False, [R.scT, vb_])
                mm(po, po[:, 0:256], R.qin0[:], Sr[h][:, 0:256], False, False, [R.qin0, Sr[h]])
                mm(po, po[:, 0:256], R.qin1[:], Sr[h][:, 256:512], False, True, [R.qin1, Sr[h]])
                for i in range(2):
                    ps = psb()
                    mm(ps, ps[:, 0:256], R.rktok[blk][:, i * 128:(i + 1) * 128], vb_[:], True, True, [R.rktok[blk], vb_])
                    stt("dve", Sr[h][:, i * 256:(i + 1) * 256], Sr[h][:, i * 256:(i + 1) * 256], gdec[h], ps[:, 0:256], ALU.mult, ALU.add, [Sr[h], ps], [Sr[h]])
                act(R.rtm[0][:], po[:, 0:256], AF.Square, [po], [R.rtm[0]])
                copy("dve", R.rtm[1][:], po[:, 0:256], [po], [R.rtm[1]])
                yield
                fw.op("dve", lambda e, R=R, sb_=sb_: e.reduce_sum(sb_[:], R.rtm[0][:], AX), reads=[R.rtm[0]], writes=[sb_])
                yield
                act(sb_[:], sb_[:], AF.Sqrt, [sb_], [sb_], bias=EPS, scale=1.0 / 256)
                yield
                fw.op("dve", lambda e, sb_=sb_: e.reciprocal(sb_[:], sb_[:]), reads=[sb_], writes=[sb_])
                yield
                stt("dve", R.rtm[1][:], R.rtm[1][:], sb_[:], S("retnorm", h * 256, (h + 1) * 256), ALU.mult, ALU.mult, [R.rtm[1], sb_, smb], [R.rtm[1]])
                yield
                tt("dve", oall[blk][:, 1024 + h * 256:1024 + (h + 1) * 256], R.rtm[1][:], R.rgtok[blk][:], ALU.mult, [R.rtm[1], R.rgtok[blk]], [oall[blk]])
                yield

        for t0 in range(0, NTOK, TT):
            b = t0 // T
            tl = t0 % T
            if tl == 0:
                fw.op("dve", lambda e: e.memset(halo[:], 0.0), writes=[halo])
                fw.op("dve", lambda e: e.memset(Sg_t[:, :], 0.0), writes=Sg)
                fw.op("dve", lambda e: e.memset(Sr_t[:, :], 0.0), writes=Sr)
            prologue(ls, t0, TT, b)
            fw.dma("sp", "a", cst[:, :, :], cs_d[:, :, tl:tl + TT], writes=[cstb])
            tsl = wslot_h()
            wget(("evtail",), tsl, 256, evtail_d[:, :])
            for blk in range(2):
                ps = psq()
                for k in range(KC):
                    mm(ps, ps[:, 0:16], hT[k][:, blk * 128:(blk + 1) * 128], tsl[:, k * 16:(k + 1) * 16], k == 0, k == KC - 1, [tsl, hT[k]])
                act(gs(0, blk), ps[:, 0:8], AF.Sigmoid, [ps], [gsmb])
                tt("dve", gs(7, blk), ps[:, 8:16], S("dtb"), ALU.add, [ps, smb], [gsmb])
                act(gs(7, blk), gs(7, blk), AF.Exp, [gsmb], [gsmb])
                act(gs(7, blk), gs(7, blk), AF.Ln, [gsmb], [gsmb], bias=1.0)
                tt("dve", gs(1, blk), gs(7, blk), NEGA, ALU.mult, [gsmb], [gsmb])
                p2 = psq()
                mm(p2, p2[:, 0:8], S("tri"), gs(1, blk), True, True, [smb, gsmb])
                copy("dve", gs(2, blk), p2[:, 0:8], [p2], [gsmb])
                p3 = psq()
                mm(p3, p3[:, 0:8], S("ones"), gs(1, blk), True, True, [smb, gsmb])
                copy("dve", gs(3, blk), p3[:, 0:8], [p3], [gsmb])
                act(gs(4, blk), gs(2, blk), AF.Exp, [gsmb], [gsmb])
                tt("dve", gs(5, blk), gs(3, blk), gs(2, blk), ALU.subtract, [gsmb], [gsmb])
                act(gs(5, blk), gs(5, blk), AF.Exp, [gsmb], [gsmb])
                act(gs(6, blk), gs(3, blk), AF.Exp, [gsmb], [gsmb])
            run_gens([stage_a_gen(s, s) for s in range(2)])
            for hp in range(4):
                for s in range(2):
                    stage_b(s, 2 * hp + s)
                run_gens([chain_gen(s, blk, 2 * hp + s) for s in range(2) for blk in range(2)])
                gl = [sphase_gen(s, 2 * hp + s) for s in range(2)]
                if hp < 3:
                    gl += [stage_a_gen(s, 2 * (hp + 1) + s) for s in range(2)]
                run_gens(gl)
            _fence(fw)
            for hp in range(2):
                run_gens([ret_gen(2 * hp + s, RS[s], ssbS[s]) for s in range(2)])
            for blk in range(2):
                for k in range(KC):
                    ps = psq()
                    tr(ps, ps[:], oall[blk][:, k * 128:(k + 1) * 128], [oall[blk]])
                    copy(ew2(), hT[k][:, blk * 128:(blk + 1) * 128], ps[:], [ps], [hT[k]])
            for m in range(KC):
                proj_fm(evout_d[m], TT, lambda ps, m=m: copy(ew2(), XY[m][:, :TT], ps[:, :TT], [ps], [XY[m]]), ("evout", m))
            epilogue(ls, t0, TT, b)
            _fence(fw)
        _fence(fw)

    for item in plan:
        if item[0] == "ffn":
            ffn_sandwich(item[1], item[2], item[3])
        elif item[0] == "rglru":
            rglru_sandwich(item[1])
        elif item[0] == "gdn":
            gdn_sandwich(item[1])

    ot = [Buf(BIG[:, i * 2048:(i + 1) * 2048]) for i in range(4)]
    for t0 in range(0, NTOK, 512):
        fw.dma("sp", "a", XYt[:, :, :], xs_v[:, :, t0:t0 + 512], reads=[xsb[t0 // 256], xsb[t0 // 256 + 1]], writes=XY)
        for blk in range(4):
            for q in range(4):
                ps = psb()
                for kk in range(4):
                    k = q * 4 + kk
                    tr(ps, ps[:, kk * 128:(kk + 1) * 128], XY[k][:, blk * 128:(blk + 1) * 128], [XY[k]])
                copy(ew2(), ot[blk][:, q * 512:(q + 1) * 512], ps[:], [ps], [ot[blk]])
            fw.dma("sp", "a", out_d[t0 + blk * 128:t0 + (blk + 1) * 128, :], ot[blk][:], reads=[ot[blk]], writes=[outb])
    fw.final_wait("sp", [outb])
    fw.emit()
    fw.close()
    return nc


def _consts():
    f8 = np.float64
    i = np.arange(128)
    ident = np.eye(128)
    ones = np.ones((128, 128))
    tri = (i[:, None] <= i[None, :]).astype(f8)
    maskT = np.where(i[None, :] >= i[:, None], 0.0, -1e30)
    smask = (i[None, :] > i[:, None]).astype(f8)
    lg = np.log1p(-np.exp2(-5.0 - np.arange(4, dtype=np.float32))).astype(np.float32).astype(f8)
    decayT = np.zeros((128, 4, 128))
    gq = np.zeros((128, 4, 128))
    gk = np.zeros((128, 4))
    for h in range(4):
        dd = (i[None, :] - i[:, None]).astype(f8)
        decayT[:, h, :] = np.where(dd >= 0, np.exp(dd * lg[h]), 0.0) * (256.0 ** -0.5)
        gq[:, h, :] = np.exp((i[None, :] + 1.0) * lg[h])
        gk[:, h] = np.exp((127.0 - i) * lg[h]) * (256.0 ** -0.5)
    gdec = [float(np.exp(128.0 * lg[h])) for h in range(4)]
    return dict(ident=ident, ones=ones, tri=tri, maskT=maskT, smask=smask,
                decayT=decayT.reshape(128, 512), gq=gq.reshape(128, 512), gk=gk), gdec


def _rope_tables(T):
    half = 128
    inv_freq = (np.float32(10000.0) ** (-np.arange(half, dtype=np.float32) / np.float32(half))).astype(np.float32)
    pos = np.arange(T, dtype=np.float32)
    ang = (inv_freq[:, None] * pos[None, :]).astype(np.float32)
    cs = np.stack([np.cos(ang.astype(np.float64)), np.sin(ang.astype(np.float64))], axis=1)
    return np.ascontiguousarray(cs.astype(np.float32))


def _proj_layout(W):
    n = W.shape[1] // 128
    return np.ascontiguousarray(W.reshape(16, 128, n, 128).transpose(2, 1, 0, 3).reshape(n, 128, 2048))


def _pcol(v, n):
    return v.reshape(n, 128).T


def prep_shared(inp):
    ada_w = inp["ada_w"]
    sh = {}
    sh["adaw"] = np.ascontiguousarray(ada_w.reshape(2, 16, 128, 144, 128).transpose(0, 3, 2, 1, 4).reshape(288, 128, 2048))
    sh["w13r"] = np.ascontiguousarray(inp["ffn_w13"].reshape(2, 2, 16, 128, 2, 44, 128).transpose(0, 1, 5, 3, 4, 2, 6).reshape(176, 128, 4096))
    sh["w2r"] = np.ascontiguousarray(inp["ffn_w2"].reshape(2, 2, 2, 22, 128, 16, 128).transpose(0, 1, 5, 2, 4, 3, 6).reshape(128, 128, 2816))
    evw = inp["ev_w_in"][0]
    sh["evin"] = _proj_layout(np.concatenate([evw[:, :3072], evw[:, 3088:]], axis=1))
    sh["evtail"] = np.ascontiguousarray(evw[:, 3072:3088].reshape(16, 128, 16).transpose(1, 0, 2).reshape(128, 256))
    sh["evout"] = _proj_layout(inp["ev_w_out"][0])
    sh["odin"] = _proj_layout(inp["od_w_in"][0])
    sh["odout"] = _proj_layout(inp["od_w_out"][0])
    g = np.stack([inp["od_gate_a_w"][0], inp["od_gate_x_w"][0]], axis=0)
    g = g.reshape(2, 8, 2, 128, 2, 128)
    sh["gatew"] = np.ascontiguousarray(g.transpose(1, 3, 0, 4, 2, 5).reshape(8, 128, 1024))
    return sh


def prep_smalls(inp, core):
    consts, _ = _consts()
    sm = np.zeros((128, NS), np.float32)

    def put(name, arr):
        o, w = _SM[name]
        arr = np.asarray(arr, np.float32).reshape(128, w)
        sm[:, o:o + w] = arr
    c = inp["c"][core * NBC:(core + 1) * NBC]
    put("cT", c.reshape(NBC, 16, 128).transpose(2, 1, 0))
    put("adab", inp["ada_b"].reshape(2, 144, 128).transpose(2, 0, 1))
    put("gpre", inp["norm_pre"].reshape(2, 3, 16, 128).transpose(3, 0, 1, 2))
    put("gpost", inp["norm_post"].reshape(2, 3, 16, 128).transpose(3, 0, 1, 2))
    put("evconv", inp["ev_conv_w"][0].reshape(4, 24, 128).transpose(2, 1, 0))
    put("alog", np.broadcast_to(inp["ev_a_log"][0][None, :], (128, 8)))
    put("dtb", np.broadcast_to(inp["ev_dt_bias"][0][None, :], (128, 8)))
    put("onorm", np.broadcast_to(inp["ev_o_norm"][0][None, :], (128, 128)))
    put("retnorm", np.broadcast_to(inp["ev_ret_norm"][0][None, :], (128, 1024)))
    put("odconv", inp["od_conv_w"][0].reshape(4, 16, 128).transpose(2, 1, 0))
    put("odconvb", _pcol(inp["od_conv_b"][0], 16))
    put("gab", _pcol(inp["od_gate_a_b"][0], 16))
    put("gxb", _pcol(inp["od_gate_x_b"][0], 16))
    put("lam", _pcol(inp["od_lambda"][0], 16))
    for k, v in consts.items():
        put(k, v)
    return sm


def kernel(**inputs):
    inp = {k: np.asarray(v) for k, v in inputs.items()}
    T = inp["x"].shape[1]
    nc = build(FULL_PLAN, T)
    sh = prep_shared(inp)
    cs = _rope_tables(T)
    in_maps = []
    for core in range(NCORES):
        m = dict(sh)
        m["x"] = np.ascontiguousarray(inp["x"][core * NBC:(core + 1) * NBC].reshape(NBC * T, D))
        m["smalls"] = prep_smalls(inp, core)
        m["cs"] = cs
        in_maps.append(m)
    res = run_bass_kernel_spmd(nc, in_maps, core_ids=list(range(NCORES)))
    outs = [res.results[i]["out"].reshape(NBC, T, D) for i in range(NCORES)]
    return np.concatenate(outs, axis=0).astype(np.float32)
```

```python
import numpy as np
import concourse.bass as bass
import concourse.mybir as mybir
from concourse.bass_utils import run_bass_kernel_spmd


EPOCH = 24000


class _St:
    __slots__ = ("lw", "rd", "excl")

    def __init__(self, excl):
        self.lw = None
        self.rd = {}
        self.excl = excl


class Buf:
    __slots__ = ("ap", "st", "name")

    def __init__(self, ap, name="", excl=False, st=None):
        self.ap = ap
        self.st = st if st is not None else _St(excl)
        self.name = name

    def view(self, ap):
        return Buf(ap, self.name, st=self.st)

    @property
    def lw(self):
        return self.st.lw

    @lw.setter
    def lw(self, v):
        self.st.lw = v

    @property
    def rd(self):
        return self.st.rd

    @rd.setter
    def rd(self, v):
        self.st.rd = v

    @property
    def excl(self):
        return self.st.excl

    def __getitem__(self, k):
        return self.ap[k]


class Track:
    def __init__(self, fw, name, step):
        self.fw = fw
        self.name = name
        self.step = step
        self.epoch = 0
        self.idx = 0
        self.sems = [fw._new_sem(f"{name}_e0")]

    def next_token(self):
        if (self.idx + 1) * self.step > EPOCH:
            self.epoch += 1
            self.idx = 0
            self.sems.append(self.fw._new_sem(f"{self.name}_e{self.epoch}"))
        self.idx += 1
        return (self, self.epoch, self.idx)


class Engine:
    def __init__(self, fw, name, strict_self):
        self.fw = fw
        self.name = name
        self.track = Track(fw, name, 1)
        self.ops = []
        self.seen = {}
        self.strict_self = strict_self


class FW:
    def __init__(self, nc):
        self.nc = nc
        self._stack = []
        self._semcount = 0
        self.eng = {
            "pe": Engine(self, "pe", False),
            "act": Engine(self, "act", True),
            "dve": Engine(self, "dve", True),
            "pool": Engine(self, "pool", True),
            "sp": Engine(self, "sp", True),
        }
        self.lanes = {}
        self.n_inst = 0

    def _new_sem(self, name):
        cm = self.nc.semaphore(name)
        s = cm.__enter__()
        self._stack.append(cm)
        self._semcount += 1
        return s

    def sbuf(self, name, shape, dtype):
        cm = self.nc.sbuf_tensor(name, list(shape), dtype)
        t = cm.__enter__()
        self._stack.append(cm)
        return t

    def psum(self, name, shape, dtype):
        cm = self.nc.psum_tensor(name, list(shape), dtype)
        t = cm.__enter__()
        self._stack.append(cm)
        return t

    def lane_pool(self, name, n):
        self.lanes[name] = dict(tracks=[Track(self, f"{name}{i}", 16) for i in range(n)], rr=0)

    def _waits(self, engine, reads, writes):
        need = {}
        for b in reads:
            if b.lw is not None:
                k = (b.lw[0], b.lw[1])
                need[k] = max(need.get(k, 0), b.lw[2])
        for b in writes:
            if b.lw is not None:
                k = (b.lw[0], b.lw[1])
                need[k] = max(need.get(k, 0), b.lw[2])
            for k, i in b.rd.items():
                need[k] = max(need.get(k, 0), i)
        out = []
        for k, i in need.items():
            tr, ep = k
            if tr is engine.track and not engine.strict_self:
                continue
            if engine.seen.get(k, 0) >= i:
                continue
            engine.seen[k] = i
            out.append((tr.sems[ep], i * tr.step))
        return out

    def _commit(self, tok, reads, writes):
        k = (tok[0], tok[1])
        for b in reads:
            b.rd[k] = max(b.rd.get(k, 0), tok[2])
        for b in writes:
            b.lw = tok
            b.rd = {}

    def op(self, ename, fn, reads=(), writes=()):
        e = self.eng[ename]
        if any(b.excl for b in reads):
            writes = list(writes) + [b for b in reads if b.excl]
            reads = [b for b in reads if not b.excl]
        waits = self._waits(e, reads, writes)
        tok = e.track.next_token()
        sem = tok[0].sems[tok[1]]
        e.seen[(tok[0], tok[1])] = max(e.seen.get((tok[0], tok[1]), 0), 0)

        def run(eng, waits=waits, fn=fn, sem=sem):
            for s, v in waits:
                eng.wait_ge(s, v)
            fn(eng).then_inc(sem, 1)
        e.ops.append(run)
        self._commit(tok, reads, writes)
        self.n_inst += 1
        return tok

    def dma(self, ename, pool, out_ap, in_ap, reads=(), writes=(), **kw):
        e = self.eng[ename]
        lp = self.lanes[pool]
        tr = lp["tracks"][lp["rr"] % len(lp["tracks"])]
        lp["rr"] += 1
        waits = self._waits(e, reads, writes)
        if tr.idx > 0:
            k = (tr, tr.epoch)
            if e.seen.get(k, 0) < tr.idx:
                e.seen[k] = tr.idx
                waits.append((tr.sems[tr.epoch], tr.idx * 16))
        tok = tr.next_token()
        sem = tr.sems[tok[1]]

        def run(eng, waits=waits, sem=sem, out_ap=out_ap, in_ap=in_ap, kw=kw):
            for s, v in waits:
                eng.wait_ge(s, v)
            eng.dma_start(out=out_ap, in_=in_ap, **kw).then_inc(sem, 16)
        e.ops.append(run)
        self._commit(tok, reads, writes)
        self.n_inst += 1
        return tok

    def final_wait(self, ename, bufs):
        e = self.eng[ename]
        waits = self._waits(e, bufs, ())

        def run(eng, waits=waits):
            for s, v in waits:
                eng.wait_ge(s, v)
        e.ops.append(run)

    def emit(self):
        nc = self.nc
        with nc.Block() as block:
            @block.tensor
            def _(eng):
                for f in self.eng["pe"].ops:
                    f(eng)

            @block.scalar
            def _(eng):
                for f in self.eng["act"].ops:
                    f(eng)

            @block.vector
            def _(eng):
                for f in self.eng["dve"].ops:
                    f(eng)

            @block.gpsimd
            def _(eng):
                for f in self.eng["pool"].ops:
                    f(eng)

            @block.sync
            def _(eng):
                for f in self.eng["sp"].ops:
                    f(eng)

    def close(self):
        while self._stack:
            self._stack.pop().__exit__(None, None, None)


def _fence(fw):
    tracks = [e.track for e in fw.eng.values()]
    for lp in fw.lanes.values():
        tracks += lp["tracks"]
    for e in fw.eng.values():
        waits = []
        for tr in tracks:
            if tr is e.track or tr.idx == 0:
                continue
            k = (tr, tr.epoch)
            if e.seen.get(k, 0) >= tr.idx:
                continue
            e.seen[k] = tr.idx
            waits.append((tr.sems[tr.epoch], tr.idx * tr.step))

        def run(eng, waits=waits):
            for s, v in waits:
                eng.wait_ge(s, v)
        e.ops.append(run)


F32 = mybir.dt.float32
BF16 = mybir.dt.bfloat16
AF = mybir.ActivationFunctionType
ALU = mybir.AluOpType

D = 2048
KC = 16
DFF = 5632
FC = 44
NBC = 2
NCORES = 8
EPS = 1e-6
LRU_C = 8.0

_SM = {}
_off = 0
for _n, _w in [("cT", 32), ("adab", 288), ("gpre", 96), ("gpost", 96), ("evconv", 96),
               ("alog", 8), ("dtb", 8), ("onorm", 128), ("retnorm", 1024),
               ("odconv", 64), ("odconvb", 16), ("gab", 16), ("gxb", 16), ("lam", 16),
               ("ident", 128), ("ones", 128), ("tri", 128), ("maskT", 128), ("smask", 128),
               ("decayT", 512), ("gq", 512), ("gk", 4)]:
    _SM[_n] = (_off, _w)
    _off += _w
NS = _off


FULL_PLAN = [("ffn", 0, 0, 0), ("gdn", 1), ("ffn", 0, 1, 2), ("ffn", 1, 0, 3), ("rglru", 4), ("ffn", 1, 1, 5)]


def build(plan=FULL_PLAN, T=2048):
    NTOK = NBC * T
    nc = bass.Bass("TRN2", target_bir_lowering=False)

    def din(name, shape, dt=F32):
        return nc.dram_tensor(name, list(shape), dt, kind="ExternalInput").ap()

    x_in = din("x", [NTOK, D])
    smalls_d = din("smalls", [128, NS])
    cs_d = din("cs", [128, 2, T])
    adaw_d = din("adaw", [2 * 144, 128, 2048])
    w13_d = din("w13r", [2 * 2 * FC, 128, 4096])
    w2_d = din("w2r", [2 * 2 * 16 * 2, 128, 22 * 128])
    evin_d = din("evin", [64, 128, 2048])
    evtail_d = din("evtail", [128, 256])
    evout_d = din("evout", [16, 128, 2048])
    odin_d = din("odin", [32, 128, 2048])
    odout_d = din("odout", [16, 128, 2048])
    gatew_d = din("gatew", [8, 128, 1024])
    out_d = nc.dram_tensor("out", [NTOK, D], F32, kind="ExternalOutput").ap()
    xs_d = nc.dram_tensor("xs", [KC, 128, NTOK], F32).ap()
    xs_v = xs_d.rearrange("k p t -> p k t")
    wcs_ffn = [nc.dram_tensor(f"wcs{i}", [128, 44 * 4096 + 32 * 2816], BF16).ap() for i in range(4)]
    wcs_mix = nc.dram_tensor("wcsm", [128, 64 * 2048 + 256 + 16 * 2048 + 32 * 2048 + 16 * 2048], BF16).ap()

    fw = FW(nc)
    fw_halo = fw.sbuf("halo", [128, 96], F32)
    fw_hst = fw.sbuf("hst", [128, 16], F32)
    fw_c1 = fw.sbuf("c1", [128, 32], F32)
    fw.lane_pool("w", 6)
    fw.lane_pool("wb", 6)
    fw.lane_pool("ws", 4)
    fw.lane_pool("pc", 6)
    fw.lane_pool("a", 6)

    sm = fw.sbuf("sm", [128, NS], F32)
    smb = Buf(sm)

    def S(name, lo=0, hi=None):
        o, w = _SM[name]
        hi = w if hi is None else hi
        return sm[:, o + lo:o + hi]

    XYt = fw.sbuf("XY", [128, KC, 512], F32)
    XY = [Buf(XYt[:, k, :]) for k in range(KC)]
    hTt = fw.sbuf("hT", [128, KC, 512], BF16)
    hT = [Buf(hTt[:, k, :]) for k in range(KC)]
    BIG = fw.sbuf("BIG", [128, 11264], F32)
    WS = [Buf(fw.sbuf(f"ws{i}", [128, 4096], BF16)) for i in range(5)]
    wsi = [0]

    def wslot():
        b = WS[wsi[0] % len(WS)]
        wsi[0] += 1
        return b
    WSH = [Buf(WS[i][:, j * 2048:(j + 1) * 2048]) for i in range(len(WS)) for j in range(2)]
    wshi = [0]

    def wslot_h():
        b = WSH[wshi[0] % len(WSH)]
        wshi[0] += 1
        return b
    xch = [Buf(fw.sbuf(f"xch{i}", [128, 512], F32)) for i in range(2)]
    sqb = [Buf(fw.sbuf(f"sq{i}", [128, 512], F32)) for i in range(2)]
    tmpb = [Buf(fw.sbuf(f"tmp{i}", [128, 512], F32)) for i in range(3)]
    rstd = Buf(fw.sbuf("rstd", [128, 512], F32))
    modT = fw.sbuf("modT", [128, 2 * 144 * 2], F32)
    modTb = Buf(modT)
    Ab = fw.sbuf("Ab", [128, 6 * KC * 2], F32)
    Bb = fw.sbuf("Bb", [128, 6 * KC * 2], F32)
    Cb = fw.sbuf("Cb", [128, 6 * KC * 2], F32)
    ABCb = Buf(Ab)
    silc = Buf(fw.sbuf("silc", [128, 32], BF16))
    fws = [Buf(fw.sbuf(f"fws{i}", [128, 1024], F32)) for i in range(2)]
    gxb_ = [Buf(fw.sbuf(f"gx{i}", [128, 512], F32)) for i in range(4)]

    pst = [fw.psum(f"ps{i}", [128, 512], F32) for i in range(8)]
    BANK = [Buf(pst[i], excl=True) for i in range(8)]
    PSB = BANK[0:4]
    PSQ = [BANK[2 + i].view(pst[2 + i][:, 0:128]) for i in range(6)]
    pbi = [0]
    pqi = [0]

    def psb():
        b = PSB[pbi[0] % 4]
        pbi[0] += 1
        return b

    def psq():
        b = PSQ[pqi[0] % 6]
        pqi[0] += 1
        return b

    xsb = [Buf(None, f"xs{i}") for i in range(NTOK // 256)]
    outb = Buf(None, "out")

    rr = [0]

    def ew2():
        rr[0] += 1
        return "dve" if rr[0] % 2 else "act"

    def mm(ps, ps_ap, lhsT, rhs, start, stop, reads):
        fw.op("pe", lambda e: e.matmul(ps_ap, lhsT, rhs, start=start, stop=stop), reads=reads, writes=[ps])

    def tr(ps, ps_ap, in_ap, reads):
        fw.op("pe", lambda e: e.transpose(ps_ap, in_ap, S("ident")), reads=reads + [smb], writes=[ps])

    def act(out, in_, func, reads, writes, bias=None, scale=None):
        kw = {}
        if bias is not None:
            kw["bias"] = bias
        if scale is not None:
            kw["scale"] = scale
        fw.op("act", lambda e: e.activation(out=out, in_=in_, func=func, **kw), reads=reads, writes=writes)

    def copy(eng, out, in_, reads, writes):
        if eng == "act":
            act(out, in_, AF.Copy, reads, writes)
        else:
            fw.op(eng, lambda e: e.tensor_copy(out, in_), reads=reads, writes=writes)

    def ts(eng, out, in0, s1, s2, op0, op1, reads, writes):
        if op1 is None:
            fw.op(eng, lambda e: e.tensor_scalar(out, in0, s1, None, op0), reads=reads, writes=writes)
        else:
            fw.op(eng, lambda e: e.tensor_scalar(out, in0, s1, s2, op0, op1), reads=reads, writes=writes)

    def stt(eng, out, in0, sc, in1, op0, op1, reads, writes):
        fw.op(eng, lambda e: e.scalar_tensor_tensor(out, in0, sc, in1, op0, op1), reads=reads, writes=writes)

    def tt(eng, out, in0, in1, op, reads, writes):
        fw.op(eng, lambda e: e.tensor_tensor(out, in0, in1, op), reads=reads, writes=writes)

    def wload(slot, dst_ap, src_ap):
        fw.dma("pool", "w", dst_ap, src_ap, writes=[slot])

    wcache = {}
    wc_off = {}

    def _cache_ap(key, ncols):
        if key[0] == "w13":
            gidx = key[1] * 2 + key[2]
        elif key[0] == "w2":
            gidx = key[1] // 32
        else:
            gidx = 4
        wt = wcs_ffn[gidx] if gidx < 4 else wcs_mix
        o = wc_off.get(gidx, 0)
        wc_off[gidx] = o + ncols
        return wt[:, o:o + ncols]

    def wget(key, slot, ncols, src_ap):
        dst = slot[:, 0:ncols]
        if key in wcache:
            dap, dbuf = wcache[key]
            fw.dma("sp", "wb", dst, dap, reads=[dbuf], writes=[slot])
        else:
            wload(slot, dst, src_ap)
            dap = _cache_ap(key, ncols)
            dbuf = Buf(None, "wc")
            fw.dma("sp", "ws", dap, dst, reads=[slot], writes=[dbuf])
            wcache[key] = (dap, dbuf)

    pc_queue = []

    def pc_add(key, ncols, src_ap):
        pc_queue.append((key, ncols, src_ap))

    def pc_pull(n):
        while n > 0 and pc_queue:
            key, ncols, src_ap = pc_queue.pop(0)
            if key in wcache:
                continue
            dap = _cache_ap(key, ncols)
            dbuf = Buf(None, "wc")
            fw.dma("pool", "pc", dap, src_ap, writes=[dbuf])
            wcache[key] = (dap, dbuf)
            n -= 1

    def pc_add_ffn(l, f):
        for j in range(FC):
            pc_add(("w13", l, f, j), 4096, w13_d[(l * 2 + f) * FC + j])
        for m_ in range(KC):
            base = (((l * 2 + f) * 16) + m_) * 2
            pc_add(("w2", base), 2816, w2_d[base])
            pc_add(("w2", base + 1), 2816, w2_d[base + 1])

    def pc_add_gdn():
        for h in range(8):
            for m_ in (h, 8 + h, 16 + h, 24 + h):
                pc_add(("evin", m_), 2048, evin_d[m_])
            if h == 0:
                pc_add(("evtail",), 256, evtail_d[:, :])
        for m_ in range(32, 64):
            pc_add(("evin", m_), 2048, evin_d[m_])
        for m_ in range(KC):
            pc_add(("evout", m_), 2048, evout_d[m_])

    def pc_add_rglru():
        for n in range(8):
            pc_add(("odin", 2 * n), 2048, odin_d[2 * n])
            pc_add(("odin", 2 * n + 1), 2048, odin_d[2 * n + 1])
            pc_add(("odin", 16 + 2 * n), 2048, odin_d[16 + 2 * n])
            pc_add(("odin", 16 + 2 * n + 1), 2048, odin_d[16 + 2 * n + 1])
        for m_ in range(KC):
            pc_add(("odout", m_), 2048, odout_d[m_])

    fw.dma("sp", "a", sm[:, :], smalls_d[:, :], writes=[smb])
    act(silc[:], S("cT"), AF.Silu, [smb], [silc])
    for l in range(2):
        for q in range(144):
            slot = wslot()
            wload(slot, slot[:, 0:2048], adaw_d[l * 144 + q])
            ps = psq()
            for k in range(KC):
                mm(ps, ps[:, 0:2], slot[:, k * 128:(k + 1) * 128], silc[:, k * 2:k * 2 + 2], k == 0, k == KC - 1, [slot, silc])
            o = (l * 144 + q) * 2
            oa = _SM["adab"][0] + l * 144 + q
            ts("dve", modT[:, o:o + 2], ps[:, 0:2], sm[:, oa:oa + 1], None, ALU.add, None, [ps, smb], [modTb])
    mv = modT[:, :].rearrange("p (l q b) -> p l q b", l=2, q=144)
    Av = Ab[:, :].rearrange("p (s k b) -> p s k b", s=6, k=KC)
    Bv = Bb[:, :].rearrange("p (s k b) -> p s k b", s=6, k=KC)
    Cv = Cb[:, :].rearrange("p (s k b) -> p s k b", s=6, k=KC)
    for l in range(2):
        for s in range(3):
            ls = l * 3 + s
            resw = 1.0 if s == 1 else 0.5
            gp = S("gpre", ls * KC, (ls + 1) * KC)
            gq_ = S("gpost", ls * KC, (ls + 1) * KC)
            for b in range(NBC):
                sh = mv[:, l, (s * 3 + 0) * KC:(s * 3 + 1) * KC, b]
                sc = mv[:, l, (s * 3 + 1) * KC:(s * 3 + 2) * KC, b]
                ga = mv[:, l, (s * 3 + 2) * KC:(s * 3 + 3) * KC, b]
                stt("dve", Av[:, ls, :, b], sc, 1.0, gp, ALU.add, ALU.mult, [modTb, smb], [ABCb])
                copy("dve", Bv[:, ls, :, b], sh, [modTb], [ABCb])
                stt("dve", Cv[:, ls, :, b], ga, 1.0, gq_, ALU.add, ALU.mult, [modTb, smb], [ABCb])
                ts("dve", Cv[:, ls, :, b], Cv[:, ls, :, b], resw, None, ALU.mult, None, [ABCb], [ABCb])

    def Asc(tab, ls, k, b):
        return tab[:, ls, k, b:b + 1]

    def sumsq_rstd(srcs, TT, scale, eps):
        ps = psb()
        n = len(srcs)
        for k, (b, ap) in enumerate(srcs):
            sq = sqb[k % 2]
            act(sq[:, :TT], ap, AF.Square, [b], [sq])
            mm(ps, ps[:, :TT], S("ones"), sq[:, :TT], k == 0, k == n - 1, [sq, smb])
        act(rstd[:, :TT], ps[:, :TT], AF.Sqrt, [ps], [rstd], bias=eps, scale=scale)
        fw.op("dve", lambda e: e.reciprocal(rstd[:, :TT], rstd[:, :TT]), reads=[rstd], writes=[rstd])

    def prologue(ls, t0, TT, b):
        nb = TT // 256
        xbufs = [xsb[t0 // 256 + i] for i in range(nb)]
        fw.dma("sp", "a", XYt[:, :, :TT], xs_v[:, :, t0:t0 + TT], reads=xbufs, writes=XY)
        sumsq_rstd([(XY[k], XY[k][:, :TT]) for k in range(KC)], TT, 1.0 / D, EPS)
        for k in range(KC):
            tm = tmpb[k % 3]
            stt("dve", tm[:, :TT], XY[k][:, :TT], Asc(Av, ls, k, b), rstd[:, :TT], ALU.mult, ALU.mult, [XY[k], rstd, ABCb], [tm])
            act(hT[k][:, :TT], tm[:, :TT], AF.Identity, [tm, ABCb], [hT[k]], bias=Asc(Bv, ls, k, b))

    def epilogue(ls, t0, TT, b):
        nb = TT // 256
        xbufs = [xsb[t0 // 256 + i] for i in range(nb)]
        sumsq_rstd([(XY[k], XY[k][:, :TT]) for k in range(KC)], TT, 1.0 / D, EPS)
        for k in range(KC):
            xc = xch[k % 2]
            fw.dma("sp", "a", xc[:, :TT], xs_d[k, :, t0:t0 + TT], reads=xbufs, writes=[xc])
            tm = tmpb[k % 3]
            stt("dve", tm[:, :TT], XY[k][:, :TT], Asc(Cv, ls, k, b), rstd[:, :TT], ALU.mult, ALU.mult, [XY[k], rstd, ABCb], [tm])
            tt("dve", XY[k][:, :TT], tm[:, :TT], xc[:, :TT], ALU.add, [tm, xc], [XY[k]])
        fw.dma("sp", "a", xs_v[:, :, t0:t0 + TT], XYt[:, :, :TT], reads=XY, writes=xbufs)

    def proj_fm(wd_piece, TT, consume, key):
        slot = wslot_h()
        wget(key, slot, 2048, wd_piece)
        ps = psb()
        for k in range(KC):
            mm(ps, ps[:, :TT], slot[:, k * 128:(k + 1) * 128], hT[k][:, :TT], k == 0, k == KC - 1, [slot, hT[k]])
        consume(ps)

    xin = [Buf(BIG[:, i * 2048:(i + 1) * 2048]) for i in range(4)]
    for t0 in range(0, NTOK, 512):
        for blk in range(4):
            fw.dma("sp", "a", xin[blk][:], x_in[t0 + blk * 128:t0 + (blk + 1) * 128, :], writes=[xin[blk]])
        for k in range(KC):
            ps = psb()
            for blk in range(4):
                tr(ps, ps[:, blk * 128:(blk + 1) * 128], xin[blk][:, k * 128:(k + 1) * 128], [xin[blk]])
            copy(ew2(), XY[k][:], ps[:], [ps], [XY[k]])
        fw.dma("sp", "a", xs_v[:, :, t0:t0 + 512], XYt[:, :, :], reads=XY, writes=[xsb[t0 // 256], xsb[t0 // 256 + 1]])
    _fence(fw)

    def ffn_sandwich(l, f, ls):
        actT = BIG[:, :].bitcast(BF16)
        actb = [Buf(actT[:, j * 512:(j + 1) * 512]) for j in range(FC)]
        TT = 512
        xcb = xch
        sq_, acc_ = sqb
        rstdP = rstd
        tmP, tmE0, tmE1, rstdE = gxb_
        tmE = [tmE0, tmE1]

        def sumsq_acc(k, src_b, src_ap):
            act(sq_[:], src_ap, AF.Square, [src_b], [sq_])
            if k == 0:
                copy("dve", acc_[:], sq_[:], [sq_], [acc_])
            else:
                tt("dve", acc_[:], acc_[:], sq_[:], ALU.add, [acc_, sq_], [acc_])

        def finish_rstd(dst):
            ps = psb()
            mm(ps, ps[:], S("ones"), acc_[:], True, True, [acc_, smb])
            act(dst[:], ps[:], AF.Sqrt, [ps], [dst], bias=EPS, scale=1.0 / D)
            fw.op("dve", lambda e: e.reciprocal(dst[:], dst[:]), reads=[dst], writes=[dst])

        def pro_gen(t0, q="pool"):
            b = t0 // T
            xbufs = [xsb[t0 // 256], xsb[t0 // 256 + 1]]

            def ld(k):
                xc = xcb[k % 2]
                fw.dma(q, "a", xc[:], xs_d[k, :, t0:t0 + TT], reads=xbufs, writes=[xc])
            ld(0)
            yield
            for k in range(KC):
                if k + 1 < KC:
                    ld(k + 1)
                sumsq_acc(k, xcb[k % 2], xcb[k % 2][:])
                yield
            ld(0)
            finish_rstd(rstdP)
            yield
            for k in range(KC):
                if k + 1 < KC:
                    ld(k + 1)
                xc = xcb[k % 2]
                stt("dve", tmP[:], xc[:], Asc(Av, ls, k, b), rstdP[:], ALU.mult, ALU.mult, [xc, rstdP, ABCb], [tmP])
                act(hT[k][:], tmP[:], AF.Identity, [tmP, ABCb], [hT[k]], bias=Asc(Bv, ls, k, b))
                yield

        def epi_gen(t0):
            b = t0 // T
            xbufs = [xsb[t0 // 256], xsb[t0 // 256 + 1]]

            def ld(k):
                xc = xcb[k % 2]
                fw.dma("pool", "a", xc[:], xs_d[k, :, t0:t0 + TT], reads=xbufs, writes=[xc])
            for k in range(KC):
                sumsq_acc(k, XY[k], XY[k][:])
                yield
            ld(0)
            finish_rstd(rstdE)
            yield
            for k in range(KC):
                if k + 1 < KC:
                    ld(k + 1)
                xc = xcb[k % 2]
                te = tmE[k % 2]
                stt("dve", te[:], XY[k][:], Asc(Cv, ls, k, b), rstdE[:], ALU.mult, ALU.mult, [XY[k], rstdE, ABCb], [te])
                tt("dve", te[:], te[:], xc[:], ALU.add, [te, xc], [te])
                fw.dma("pool", "a", xs_d[k, :, t0:t0 + TT], te[:], reads=[te], writes=xbufs)
                yield

        def pull(g, n):
            if g is None:
                return None
            for _ in range(n):
                try:
                    next(g)
                except StopIteration:
                    return None
            return g

        def drain(g):
            while g is not None:
                g = pull(g, 1)

        tiles = list(range(0, NTOK, TT))
        drain(pro_gen(tiles[0]))
        eg = None
        for ti, t0 in enumerate(tiles):
            for j in range(FC):
                slot = wslot()
                wget(("w13", l, f, j), slot, 4096, w13_d[(l * 2 + f) * FC + j])
                pg = psb()
                pu = psb()
                for k in range(KC):
                    mm(pg, pg[:], slot[:, k * 128:(k + 1) * 128], hT[k][:], k == 0, k == KC - 1, [slot, hT[k]])
                for k in range(KC):
                    mm(pu, pu[:], slot[:, 2048 + k * 128:2048 + (k + 1) * 128], hT[k][:], k == 0, k == KC - 1, [slot, hT[k]])
                tm = tmpb[j % 3]
                act(tm[:], pg[:], AF.Silu, [pg], [tm])
                tt("dve", actb[j][:], tm[:], pu[:], ALU.mult, [tm, pu], [actb[j]])
                eg = pull(eg, 1)
                if ti >= 1 and j % 3 == 0:
                    pc_pull(1)
            drain(eg)
            pgn = pro_gen(tiles[ti + 1]) if ti + 1 < len(tiles) else None
            for m in range(KC):
                s0 = wslot()
                s1 = wslot()
                base = (((l * 2 + f) * 16) + m) * 2
                wget(("w2", base), s0, 2816, w2_d[base])
                wget(("w2", base + 1), s1, 2816, w2_d[base + 1])
                ps = psb()
                for c in range(FC):
                    sl = s0 if c < 22 else s1
                    cc = c % 22
                    mm(ps, ps[:], sl[:, cc * 128:(cc + 1) * 128], actb[c][:], c == 0, c == FC - 1, [sl, actb[c]])
                copy(ew2(), XY[m][:], ps[:], [ps], [XY[m]])
                pgn = pull(pgn, 3)
            drain(pgn)
            eg = epi_gen(t0)
        drain(eg)
        _fence(fw)

    def rglru_sandwich(ls):
        TT = 512
        ygt = BIG[:, 0:4096].bitcast(BF16)
        yg = [Buf(ygt[:, k * 512:(k + 1) * 512]) for k in range(KC)]
        xcv = [Buf(BIG[:, 4096 + i * 512:4096 + (i + 1) * 512]) for i in range(4)]
        xqb = [Buf(BIG[:, 6144 + i * 520:6144 + i * 520 + 515]) for i in range(2)]
        rb = [Buf(BIG[:, 7200 + i * 512:7200 + (i + 1) * 512]) for i in range(2)]
        ib = [Buf(BIG[:, 8224 + i * 512:8224 + (i + 1) * 512]) for i in range(2)]
        ab = [Buf(BIG[:, 9248 + i * 512:9248 + (i + 1) * 512]) for i in range(2)]
        hsb = [Buf(BIG[:, 10272:10784])]
        halo = Buf(fw_halo[:, 0:48])
        hstate = Buf(fw_hst[:, 0:16])
        c1 = Buf(fw_c1[:, 0:32])
        act(c1[:, 0:16], S("lam"), AF.Exp, [smb], [c1], scale=-1.0)
        act(c1[:, 0:16], c1[:, 0:16], AF.Ln, [c1], [c1], bias=1.0)
        ts("dve", c1[:, 16:32], c1[:, 0:16], LRU_C, None, ALU.mult, None, [c1], [c1])
        ts("dve", c1[:, 0:16], c1[:, 0:16], -LRU_C, None, ALU.mult, None, [c1], [c1])
        for t0 in range(0, NTOK, TT):
            b = t0 // T
            if t0 % T == 0:
                fw.op("dve", lambda e: e.memset(halo[:], 0.0), writes=[halo])
                fw.op("dve", lambda e: e.memset(hstate[:], 0.0), writes=[hstate])
            prologue(ls, t0, TT, b)
            def y_chunk(m):
                def cons(ps, m=m):
                    t1 = tmpb[0]
                    t2 = tmpb[1]
                    copy("act", t1[:], ps[:], [ps], [t1])
                    tt("pool", t2[:], t1[:], t1[:], ALU.mult, [t1], [t2])
                    ts("pool", t2[:], t2[:], 0.044715, 1.0, ALU.mult, ALU.add, [t2], [t2])
                    tt("pool", t2[:], t2[:], t1[:], ALU.mult, [t1, t2], [t2])
                    act(t2[:], t2[:], AF.Sigmoid, [t2], [t2], scale=1.5957691216057308)
                    tt("dve", yg[m][:], t1[:], t2[:], ALU.mult, [t1, t2], [yg[m]])
                proj_fm(odin_d[m], TT, cons, ("odin", m))

            def x_block(n):
                gsl = fws[n % 2]
                fw.dma("sp", "w", gsl[:, 0:1024], gatew_d[n], writes=[gsl])
                xcs = []
                for ic in range(2):
                    ci = n * 2 + ic
                    xq = xqb[ic]
                    xc = xcv[(n % 2) * 2 + ic]

                    def cons(ps, ci=ci, xq=xq, xc=xc):
                        copy("dve", xq[:, 0:3], halo[:, ci * 3:ci * 3 + 3], [halo], [xq])
                        copy("act", xq[:, 3:515], ps[:], [ps], [xq])
                        copy("dve", halo[:, ci * 3:ci * 3 + 3], xq[:, 512:515], [xq], [halo])
                        o = _SM["odconv"][0] + ci * 4
                        ob = _SM["odconvb"][0] + ci
                        ts("dve", xc[:], xq[:, 0:512], sm[:, o:o + 1], sm[:, ob:ob + 1], ALU.mult, ALU.add, [xq, smb], [xc])
                        for tap in range(1, 4):
                            stt("dve", xc[:], xq[:, tap:tap + 512], sm[:, o + tap:o + tap + 1], xc[:], ALU.mult, ALU.add, [xq, smb, xc], [xc])
                    proj_fm(odin_d[16 + ci], TT, cons, ("odin", 16 + ci))
                    xcs.append(xc)
                return gsl, xcs

            def gates(n, gsl, xcs):
                for jc in range(2):
                    cj = n * 2 + jc
                    pr = psb()
                    pi = psb()
                    for g, pp in ((0, pr), (1, pi)):
                        for ic in range(2):
                            o = ((g * 2 + jc) * 2 + ic) * 128
                            mm(pp, pp[:], gsl[:, o:o + 128], xcs[ic][:], ic == 0, ic == 1, [gsl, xcs[ic]])
                    r_ = rb[jc]
                    i_ = ib[jc]
                    a_ = ab[jc]
                    oga = _SM["gab"][0] + cj
                    ogx = _SM["gxb"][0] + cj
                    act(r_[:], pr[:], AF.Sigmoid, [pr, smb], [r_], bias=sm[:, oga:oga + 1])
                    act(i_[:], pi[:], AF.Sigmoid, [pi, smb], [i_], bias=sm[:, ogx:ogx + 1])
                    act(a_[:], r_[:], AF.Exp, [r_, c1], [a_], scale=c1[:, cj:cj + 1])
                    act(r_[:], r_[:], AF.Tanh, [r_, c1], [r_], scale=c1[:, 16 + cj:16 + cj + 1])
                    t2 = tmpb[2]
                    tt("dve", t2[:], a_[:], a_[:], ALU.mult, [a_], [t2])
                    stt("dve", t2[:], t2[:], 1.0, r_[:], ALU.add, ALU.mult, [t2, r_], [t2])
                    act(t2[:], t2[:], AF.Sqrt, [t2], [t2])
                    tt("pool", i_[:], i_[:], xcs[jc][:], ALU.mult, [i_, xcs[jc]], [i_])
                    tt("dve", t2[:], t2[:], i_[:], ALU.mult, [t2, i_], [t2])
                    hs = hsb[0]
                    fw.op("dve", lambda e, hs=hs, a_=a_, t2=t2, cj=cj: e.tensor_tensor_scan(hs[:], a_[:], t2[:], hstate[:, cj:cj + 1], ALU.mult, ALU.add),
                          reads=[a_, t2, hstate], writes=[hs])
                    copy("dve", hstate[:, cj:cj + 1], hs[:, 511:512], [hs], [hstate])
                    tt("dve", yg[cj][:], hs[:], yg[cj][:], ALU.mult, [hs, yg[cj]], [yg[cj]])

            pend = None
            for n in range(8):
                y_chunk(2 * n)
                y_chunk(2 * n + 1)
                cur = x_block(n)
                if pend is not None:
                    gates(*pend)
                pend = (n,) + cur
            gates(*pend)
            for m in range(KC):
                slot = wslot_h()
                wget(("odout", m), slot, 2048, odout_d[m])
                ps = psb()
                for k in range(KC):
                    mm(ps, ps[:], slot[:, k * 128:(k + 1) * 128], yg[k][:], k == 0, k == KC - 1, [slot, yg[k]])
                copy(ew2(), XY[m][:], ps[:], [ps], [XY[m]])
            epilogue(ls, t0, TT, b)
        _fence(fw)


    def gdn_sandwich(ls):
        TT = 256
        _, gdec = _consts()
        AX = mybir.AxisListType.X
        oall = [Buf(BIG[:, blk * 2048:(blk + 1) * 2048]) for blk in range(2)]
        xq = [Buf(BIG[:, 4096 + i * 264:4096 + i * 264 + 259]) for i in range(3)]
        qkvS = [[Buf(BIG[:, 4888 + s * 768 + i * 256:4888 + s * 768 + (i + 1) * 256]) for i in range(3)] for s in range(2)]
        ktokS = [Buf(BIG[:, 6424 + s * 768:6424 + s * 768 + 256]) for s in range(2)]
        vtokS = [Buf(BIG[:, 6680 + s * 768:6680 + s * 768 + 256]) for s in range(2)]
        ztokS = [Buf(BIG[:, 6936 + s * 768:6936 + s * 768 + 256]) for s in range(2)]
        G = [Buf(BIG[:, 8192 + i * 128:8192 + (i + 1) * 128]) for i in range(24)]
        G += [Buf(XYt[:, k, 256 + j * 128:256 + (j + 1) * 128]) for k in range(KC) for j in range(2)]

        class _R:
            pass
        roles = {}
        gi = 0
        for s in range(2):
            for blk in range(2):
                C = _R()
                (C.P0, C.N0, C.Pa, C.Na, C.Xa, C.Xb, C.QKT, C.kegT, C.qstT, C.kst) = G[gi:gi + 10]
                gi += 10
                roles[(s, blk)] = C
        hroles = []
        for s in range(2):
            Hh = _R()
            (Hh.Rb, Hh.vnew, Hh.osq, Hh.ob) = G[gi:gi + 4]
            gi += 4
            hroles.append(Hh)
        scT, qin0, qin1 = G[gi:gi + 3]
        osq, ob = hroles[0].osq, hroles[0].ob
        rq = [Buf(BIG[:, 4096 + i * 256:4096 + (i + 1) * 256]) for i in range(2)]
        rk = [Buf(BIG[:, 4608 + i * 256:4608 + (i + 1) * 256]) for i in range(2)]
        rqr = [Buf(BIG[:, 5120 + i * 256:5120 + (i + 1) * 256]) for i in range(2)]
        rkr = [Buf(BIG[:, 5632 + i * 256:5632 + (i + 1) * 256]) for i in range(2)]
        rtm = [Buf(BIG[:, 6144 + i * 256:6144 + (i + 1) * 256]) for i in range(2)]
        rktok = Buf(BIG[:, 6656:7168])
        rvtok = Buf(BIG[:, 7168:7680])
        rgtok = Buf(BIG[:, 7680:8192])
        gsm = fw.sbuf("gsm", [128, 8 * 16 + 8], F32)
        gsmb = Buf(gsm)
        Sg_t = fw.sbuf("Sg", [128, 1024], F32)
        Sg = [Buf(Sg_t[:, h * 128:(h + 1) * 128]) for h in range(8)]
        Sr_t = fw.sbuf("Sr", [128, 2048], F32)
        Sr = [Buf(Sr_t[:, h * 512:(h + 1) * 512]) for h in range(4)]
        cst = fw.sbuf("cst", [128, 2, 256], F32)
        cstb = Buf(cst)
        halo = Buf(fw_halo[:, 0:72])
        ssb_t = fw.sbuf("ssb", [128, 4], F32)
        ssb = Buf(ssb_t[:, 0:2])
        ssbS = [Buf(ssb_t[:, 2 + s:3 + s]) for s in range(2)]

        def gs(i, blk, lo=0, hi=8):
            return gsm[:, (i * 2 + blk) * 8 + lo:(i * 2 + blk) * 8 + hi]
        NEGA = gsm[:, 128:136]
        act(NEGA, S("alog"), AF.Exp, [smb], [gsmb])
        ts("dve", NEGA, NEGA, -1.0, None, ALU.mult, None, [gsmb], [gsmb])

        def rowsum_rstd(ps_ap, width, src):
            act(osq[:, :] if width <= 128 else rtm[0][:, :width], ps_ap, AF.Square, [src], [osq if width <= 128 else rtm[0]])
            sqap = osq[:, :] if width <= 128 else rtm[0][:, :width]
            sqb_ = osq if width <= 128 else rtm[0]
            fw.op("dve", lambda e: e.reduce_sum(ssb[:, 0:1], sqap, AX), reads=[sqb_], writes=[ssb])
            act(ssb[:, 0:1], ssb[:, 0:1], AF.Sqrt, [ssb], [ssb], bias=EPS, scale=1.0 / width)
            fw.op("dve", lambda e: e.reciprocal(ssb[:, 0:1], ssb[:, 0:1]), reads=[ssb], writes=[ssb])


        def run_gens(gens):
            active = list(gens)
            while active:
                for g in list(active):
                    try:
                        next(g)
                    except StopIteration:
                        active.remove(g)

        def stage_a_gen(s, h):
            qkv = qkvS[s]
            for idx, m in enumerate((h, 8 + h, 16 + h)):
                def cons(ps, idx=idx, m=m):
                    x_ = xq[idx]
                    copy("dve", x_[:, 0:3], halo[:, m * 3:m * 3 + 3], [halo], [x_])
                    copy("act", x_[:, 3:259], ps[:, :TT], [ps], [x_])
                    copy("dve", halo[:, m * 3:m * 3 + 3], x_[:, 256:259], [x_], [halo])
                    o = _SM["evconv"][0] + m * 4
                    tm = tmpb[idx]
                    ts("dve", tm[:, :TT], x_[:, 0:TT], sm[:, o:o + 1], None, ALU.mult, None, [x_, smb], [tm])
                    for tap in range(1, 4):
                        stt("dve", tm[:, :TT], x_[:, tap:tap + TT], sm[:, o + tap:o + tap + 1], tm[:, :TT], ALU.mult, ALU.add, [x_, smb, tm], [tm])
                    act(qkv[idx][:], tm[:, :TT], AF.Silu, [tm], [qkv[idx]])
                proj_fm(evin_d[m], TT, cons, ("evin", m))
                yield
            qT, kT, vT = qkv
            sumsq_rstd([(qT, qT[:])], TT, 1.0, EPS)
            stt("dve", qT[:], qT[:], 128.0 ** -0.5, rstd[:, :TT], ALU.mult, ALU.mult, [qT, rstd], [qT])
            yield
            sumsq_rstd([(kT, kT[:])], TT, 1.0, EPS)
            tt("dve", kT[:], kT[:], rstd[:, :TT], ALU.mult, [kT, rstd], [kT])
            yield

        def stage_b(s, h):
            qT, kT, vT = qkvS[s]
            zsl = wslot_h()
            wget(("evin", 24 + h), zsl, 2048, evin_d[24 + h])
            ktok, vtok, ztok = ktokS[s], vtokS[s], ztokS[s]
            for blk in range(2):
                ps = psq()
                for k in range(KC):
                    mm(ps, ps[:], hT[k][:, blk * 128:(blk + 1) * 128], zsl[:, k * 128:(k + 1) * 128], k == 0, k == KC - 1, [zsl, hT[k]])
                act(ztok[:, blk * 128:(blk + 1) * 128], ps[:], AF.Silu, [ps], [ztok])
                pk_ = psq()
                tr(pk_, pk_[:], kT[:, blk * 128:(blk + 1) * 128], [kT])
                copy("act", ktok[:, blk * 128:(blk + 1) * 128], pk_[:], [pk_], [ktok])
                pv_ = psq()
                tr(pv_, pv_[:], vT[:, blk * 128:(blk + 1) * 128], [vT])
                copy("dve", vtok[:, blk * 128:(blk + 1) * 128], pv_[:], [pv_], [vtok])

        def chain_gen(s, blk, h):
            C = roles[(s, blk)]
            qT, kT, vT = qkvS[s]
            ktok = ktokS[s]
            cs_ = slice(blk * 128, (blk + 1) * 128)
            bcol = gs(0, blk, h, h + 1)
            ldcol = gs(1, blk, h, h + 1)
            gcol = gs(2, blk, h, h + 1)
            eglcol = gs(5, blk, h, h + 1)
            ldb, egb, DT = C.Pa, C.Na, C.Xb
            P0, N0, QKT, kegT, qstT, kst = C.P0, C.N0, C.QKT, C.kegT, C.qstT, C.kst
            ts("dve", ldb[:], S("ones"), ldcol, None, ALU.mult, None, [smb, gsmb], [ldb])
            pg = psq()
            mm(pg, pg[:], ldb[:], S("tri"), True, True, [ldb, smb])
            act(egb[:], pg[:], AF.Exp, [pg], [egb])
            stt("dve", DT[:], pg[:], gcol, S("maskT"), ALU.subtract, ALU.add, [pg, gsmb, smb], [DT])
            yield
            act(DT[:], DT[:], AF.Exp, [DT], [DT])
            pkk = psq()
            mm(pkk, pkk[:], kT[:, cs_], kT[:, cs_], True, True, [kT])
            pkq = psq()
            mm(pkq, pkq[:], kT[:, cs_], qT[:, cs_], True, True, [kT, qT])
            tt("dve", kegT[:], kT[:, cs_], egb[:], ALU.mult, [kT, egb], [kegT])
            tt("dve", qstT[:], qT[:, cs_], egb[:], ALU.mult, [qT, egb], [qstT])
            act(kst[:], ktok[:, cs_], AF.Copy, [ktok, gsmb], [kst], scale=eglcol)
            stt("dve", P0[:], pkk[:], bcol, DT[:], ALU.mult, ALU.mult, [pkk, gsmb, DT], [P0])
            tt("dve", QKT[:], pkq[:], DT[:], ALU.mult, [pkq, DT], [QKT])
            yield
            tt("dve", P0[:], P0[:], S("smask"), ALU.mult, [P0, smb], [P0])
            pt = psq()
            tr(pt, pt[:], P0[:], [P0])
            copy("act", N0[:], pt[:], [pt], [N0])
            stt("dve", C.Xa[:], P0[:], -1.0, S("ident"), ALU.mult, ALU.add, [P0, smb], [C.Xa])
            yield
            X, Xo = C.Xa, C.Xb
            Pk, Nk = P0, N0
            Pn, Nn = C.Pa, C.Na
            for lvl in range(6):
                if lvl < 5:
                    pp = psq()
                    mm(pp, pp[:], Nk[:], Pk[:], True, True, [Nk, Pk])
                pn = psq()
                mm(pn, pn[:], Pk[:], Nk[:], True, True, [Nk, Pk])
                if lvl < 5:
                    copy("act", Pn[:], pp[:], [pp], [Pn])
                copy("dve", Nn[:], pn[:], [pn], [Nn])
                yield
                px = psq()
                mm(px, px[:], Nn[:], X[:], True, True, [Nn, X])
                tt("dve", Xo[:], X[:], px[:], ALU.add, [X, px], [Xo])
                yield
                X, Xo = Xo, X
                Pk, Nk, Pn, Nn = Pn, Nn, Pk, Nk
            C.X = X

        def sphase_gen(s, h):
            Hh = hroles[s]
            vtok, ztok = vtokS[s], ztokS[s]
            sb_ = ssbS[s]
            for blk in range(2):
                C = roles[(s, blk)]
                cs_ = slice(blk * 128, (blk + 1) * 128)
                bcol = gs(0, blk, h, h + 1)
                deccol = gs(6, blk, h, h + 1)
                pks = psq()
                mm(pks, pks[:], C.kegT[:], Sg[h][:], True, True, [C.kegT, Sg[h]])
                tt("dve", Hh.Rb[:], vtok[:, cs_], pks[:], ALU.subtract, [vtok, pks], [Hh.Rb])
                yield
                pv = psq()
                mm(pv, pv[:], C.X[:], Hh.Rb[:], True, True, [C.X, Hh.Rb])
                ts("dve", Hh.vnew[:], pv[:], bcol, None, ALU.mult, None, [pv, gsmb], [Hh.vnew])
                yield
                po = psq()
                mm(po, po[:], C.qstT[:], Sg[h][:], True, False, [C.qstT, Sg[h]])
                mm(po, po[:], C.QKT[:], Hh.vnew[:], False, True, [C.QKT, Hh.vnew])
                psn = psq()
                mm(psn, psn[:], C.kst[:], Hh.vnew[:], True, True, [C.kst, Hh.vnew])
                act(Hh.osq[:], po[:], AF.Square, [po], [Hh.osq])
                copy("dve", Hh.ob[:], po[:], [po], [Hh.ob])
                stt("dve", Sg[h][:], Sg[h][:], deccol, psn[:], ALU.mult, ALU.add, [Sg[h], gsmb, psn], [Sg[h]])
                yield
                fw.op("dve", lambda e, Hh=Hh, sb_=sb_: e.reduce_sum(sb_[:], Hh.osq[:], AX), reads=[Hh.osq], writes=[sb_])
                yield
                act(sb_[:], sb_[:], AF.Sqrt, [sb_], [sb_], bias=EPS, scale=1.0 / 128)
                yield
                fw.op("dve", lambda e, sb_=sb_: e.reciprocal(sb_[:], sb_[:]), reads=[sb_], writes=[sb_])
                yield
                stt("dve", Hh.ob[:], Hh.ob[:], sb_[:], S("onorm"), ALU.mult, ALU.mult, [Hh.ob, sb_, smb], [Hh.ob])
                yield
                tt("dve", oall[blk][:, h * 128:(h + 1) * 128], Hh.ob[:], ztok[:, cs_], ALU.mult, [Hh.ob, ztok], [oall[blk]])
                yield


        class _RS:
            pass
        RS = []
        for s_ in range(2):
            if s_ == 0:
                pcs = [Buf(BIG[:, 4096 + i * 256:4096 + (i + 1) * 256]) for i in range(16)]
            else:
                pcs = [Buf(XYt[:, k, 256:512]) for k in range(KC)]
            R = _RS()
            R.rq, R.rk, R.rqr, R.rkr, R.rtm = pcs[0:2], pcs[2:4], pcs[4:6], pcs[6:8], pcs[8:10]
            R.rktok, R.rvtok, R.rgtok = pcs[10:12], pcs[12:14], pcs[14:16]
            R.scT, R.qin0, R.qin1 = G[s_ * 3:s_ * 3 + 3]
            RS.append(R)

        def ret_gen(h, R, sb_):
            for i in range(2):
                proj_fm(evin_d[32 + 2 * h + i], TT, lambda ps, i=i: copy("act", R.rq[i][:], ps[:, :TT], [ps], [R.rq[i]]), ("evin", 32 + 2 * h + i))
                yield
            for i in range(2):
                proj_fm(evin_d[40 + 2 * h + i], TT, lambda ps, i=i: copy("dve", R.rk[i][:], ps[:, :TT], [ps], [R.rk[i]]), ("evin", 40 + 2 * h + i))
                yield
            for src, dst in ((R.rq, R.rqr), (R.rk, R.rkr)):
                x1, x2 = src
                t0_, t1_ = R.rtm
                tt("dve", t0_[:], x1[:], cst[:, 0, :], ALU.mult, [x1, cstb], [t0_])
                tt("dve", t1_[:], x2[:], cst[:, 1, :], ALU.mult, [x2, cstb], [t1_])
                tt("dve", dst[0][:], t0_[:], t1_[:], ALU.subtract, [t0_, t1_], [dst[0]])
                yield
                tt("dve", t0_[:], x2[:], cst[:, 0, :], ALU.mult, [x2, cstb], [t0_])
                tt("dve", t1_[:], x1[:], cst[:, 1, :], ALU.mult, [x1, cstb], [t1_])
                tt("dve", dst[1][:], t0_[:], t1_[:], ALU.add, [t0_, t1_], [dst[1]])
                yield
            for base, dstl, fn in ((48, R.rvtok, None), (56, R.rgtok, AF.Silu)):
                sl = [wslot_h(), wslot_h()]
                for i in range(2):
                    wget(("evin", base + 2 * h + i), sl[i], 2048, evin_d[base + 2 * h + i])
                for blk in range(2):
                    ps = psb()
                    for i in range(2):
                        for k in range(KC):
                            mm(ps, ps[:, i * 128:(i + 1) * 128], hT[k][:, blk * 128:(blk + 1) * 128], sl[i][:, k * 128:(k + 1) * 128], k == 0, k == KC - 1, [sl[i], hT[k]])
                    if fn is None:
                        copy("dve", dstl[blk][:], ps[:, 0:256], [ps], [dstl[blk]])
                    else:
                        act(dstl[blk][:], ps[:, 0:256], fn, [ps], [dstl[blk]])
                    yield
            og = _SM["gk"][0] + h
            for blk in range(2):
                for i in range(2):
                    pk_ = psq()
                    tr(pk_, pk_[:], R.rkr[i][:, blk * 128:(blk + 1) * 128], [R.rkr[i]])
                    ts("dve", R.rktok[blk][:, i * 128:(i + 1) * 128], pk_[:], sm[:, og:og + 1], None, ALU.mult, None, [pk_, smb], [R.rktok[blk]])
                yield
            for blk in range(2):
                cs_ = slice(blk * 128, (blk + 1) * 128)
                vb_ = R.rvtok[blk]
                psc = psq()
                mm(psc, psc[:], R.rkr[0][:, cs_], R.rqr[0][:, cs_], True, False, [R.rkr[0], R.rqr[0]])
                mm(psc, psc[:], R.rkr[1][:, cs_], R.rqr[1][:, cs_], False, True, [R.rkr[1], R.rqr[1]])
                tt("dve", R.scT[:], psc[:], S("decayT", h * 128, (h + 1) * 128), ALU.mult, [psc, smb], [R.scT])
                tt("dve", R.qin0[:], R.rqr[0][:, cs_], S("gq", h * 128, (h + 1) * 128), ALU.mult, [R.rqr[0], smb], [R.qin0])
                tt("dve", R.qin1[:], R.rqr[1][:, cs_], S("gq", h * 128, (h + 1) * 128), ALU.mult, [R.rqr[1], smb], [R.qin1])
                yield
                po = psb()
                mm(po, po[:, 0:256], R.scT[:], vb_[:], True, False, [R.scT, vb_])
                mm(po, po[:, 0:256], R.qin0[:], Sr[h][:, 0:256], False, False, [R.qin0, Sr[h]])
                mm(po, po[:, 0:256], R.qin1[:], Sr[h][:, 256:512], False, True, [R.qin1, Sr[h]])
                for i in range(2):
                    ps = psb()
                    mm(ps, ps[:, 0:256], R.rktok[blk][:, i * 128:(i + 1) * 128], vb_[:], True, True, [R.rktok[blk], vb_])
                    stt("dve", Sr[h][:, i * 256:(i + 1) * 256], Sr[h][:, i * 256:(i + 1) * 256], gdec[h], ps[:, 0:256], ALU.mult, ALU.add, [Sr[h], ps], [Sr[h]])
                act(R.rtm[0][:], po[:, 0:256], AF.Square, [po], [R.rtm[0]])
                copy("dve", R.rtm[1][:], po[:, 0:256], [po], [R.rtm[1]])
                yield
                fw.op("dve", lambda e, R=R, sb_=sb_: e.reduce_sum(sb_[:], R.rtm[0][:], AX), reads=[R.rtm[0]], writes=[sb_])
                yield
                act(sb_[:], sb_[:], AF.Sqrt, [sb_], [sb_], bias=EPS, scale=1.0 / 256)
                yield
                fw.op("dve", lambda e, sb_=sb_: e.reciprocal(sb_[:], sb_[:]), reads=[sb_], writes=[sb_])
                yield
                stt("dve", R.rtm[1][:], R.rtm[1][:], sb_[:], S("retnorm", h * 256, (h + 1) * 256), ALU.mult, ALU.mult, [R.rtm[1], sb_, smb], [R.rtm[1]])
                yield
                tt("dve", oall[blk][:, 1024 + h * 256:1024 + (h + 1) * 256], R.rtm[1][:], R.rgtok[blk][:], ALU.mult, [R.rtm[1], R.rgtok[blk]], [oall[blk]])
                yield

        for t0 in range(0, NTOK, TT):
            b = t0 // T
            tl = t0 % T
            if tl == 0:
                fw.op("dve", lambda e: e.memset(halo[:], 0.0), writes=[halo])
                fw.op("dve", lambda e: e.memset(Sg_t[:, :], 0.0), writes=Sg)
                fw.op("dve", lambda e: e.memset(Sr_t[:, :], 0.0), writes=Sr)
            prologue(ls, t0, TT, b)
            fw.dma("sp", "a", cst[:, :, :], cs_d[:, :, tl:tl + TT], writes=[cstb])
            tsl = wslot_h()
            wget(("evtail",), tsl, 256, evtail_d[:, :])
            for blk in range(2):
                ps = psq()
                for k in range(KC):
                    mm(ps, ps[:, 0:16], hT[k][:, blk * 128:(blk + 1) * 128], tsl[:, k * 16:(k + 1) * 16], k == 0, k == KC - 1, [tsl, hT[k]])
                act(gs(0, blk), ps[:, 0:8], AF.Sigmoid, [ps], [gsmb])
                tt("dve", gs(7, blk), ps[:, 8:16], S("dtb"), ALU.add, [ps, smb], [gsmb])
                act(gs(7, blk), gs(7, blk), AF.Exp, [gsmb], [gsmb])
                act(gs(7, blk), gs(7, blk), AF.Ln, [gsmb], [gsmb], bias=1.0)
                tt("dve", gs(1, blk), gs(7, blk), NEGA, ALU.mult, [gsmb], [gsmb])
                p2 = psq()
                mm(p2, p2[:, 0:8], S("tri"), gs(1, blk), True, True, [smb, gsmb])
                copy("dve", gs(2, blk), p2[:, 0:8], [p2], [gsmb])
                p3 = psq()
                mm(p3, p3[:, 0:8], S("ones"), gs(1, blk), True, True, [smb, gsmb])
                copy("dve", gs(3, blk), p3[:, 0:8], [p3], [gsmb])
                act(gs(4, blk), gs(2, blk), AF.Exp, [gsmb], [gsmb])
                tt("dve", gs(5, blk), gs(3, blk), gs(2, blk), ALU.subtract, [gsmb], [gsmb])
                act(gs(5, blk), gs(5, blk), AF.Exp, [gsmb], [gsmb])
                act(gs(6, blk), gs(3, blk), AF.Exp, [gsmb], [gsmb])
            run_gens([stage_a_gen(s, s) for s in range(2)])
            for hp in range(4):
                if t0 > 0:
                    pc_pull(5)
                for s in range(2):
                    stage_b(s, 2 * hp + s)
                run_gens([chain_gen(s, blk, 2 * hp + s) for s in range(2) for blk in range(2)])
                gl = [sphase_gen(s, 2 * hp + s) for s in range(2)]
                if hp < 3:
                    gl += [stage_a_gen(s, 2 * (hp + 1) + s) for s in range(2)]
                run_gens(gl)
            _fence(fw)
            for hp in range(2):
                run_gens([ret_gen(2 * hp + s, RS[s], ssbS[s]) for s in range(2)])
            for blk in range(2):
                for k in range(KC):
                    ps = psq()
                    tr(ps, ps[:], oall[blk][:, k * 128:(k + 1) * 128], [oall[blk]])
                    copy(ew2(), hT[k][:, blk * 128:(blk + 1) * 128], ps[:], [ps], [hT[k]])
            for m in range(KC):
                proj_fm(evout_d[m], TT, lambda ps, m=m: copy(ew2(), XY[m][:, :TT], ps[:, :TT], [ps], [XY[m]]), ("evout", m))
            epilogue(ls, t0, TT, b)
            _fence(fw)
        _fence(fw)

    for pi_, item in enumerate(plan):
        if pi_ == 0:
            for nxt in plan[1:]:
                if nxt[0] == "ffn":
                    pc_add_ffn(nxt[1], nxt[2])
                elif nxt[0] == "gdn":
                    pc_add_gdn()
                elif nxt[0] == "rglru":
                    pc_add_rglru()
        if item[0] == "ffn":
            ffn_sandwich(item[1], item[2], item[3])
        elif item[0] == "rglru":
            rglru_sandwich(item[1])
        elif item[0] == "gdn":
            gdn_sandwich(item[1])

    ot = [Buf(BIG[:, i * 2048:(i + 1) * 2048]) for i in range(4)]
    for t0 in range(0, NTOK, 512):
        fw.dma("sp", "a", XYt[:, :, :], xs_v[:, :, t0:t0 + 512], reads=[xsb[t0 // 256], xsb[t0 // 256 + 1]], writes=XY)
        for blk in range(4):
            for q in range(4):
                ps = psb()
                for kk in range(4):
                    k = q * 4 + kk
                    tr(ps, ps[:, kk * 128:(kk + 1) * 128], XY[k][:, blk * 128:(blk + 1) * 128], [XY[k]])
                copy(ew2(), ot[blk][:, q * 512:(q + 1) * 512], ps[:], [ps], [ot[blk]])
            fw.dma("sp", "a", out_d[t0 + blk * 128:t0 + (blk + 1) * 128, :], ot[blk][:], reads=[ot[blk]], writes=[outb])
    fw.final_wait("sp", [outb])
    fw.emit()
    fw.close()
    return nc


def _consts():
    f8 = np.float64
    i = np.arange(128)
    ident = np.eye(128)
    ones = np.ones((128, 128))
    tri = (i[:, None] <= i[None, :]).astype(f8)
    maskT = np.where(i[None, :] >= i[:, None], 0.0, -1e30)
    smask = (i[None, :] > i[:, None]).astype(f8)
    lg = np.log1p(-np.exp2(-5.0 - np.arange(4, dtype=np.float32))).astype(np.float32).astype(f8)
    decayT = np.zeros((128, 4, 128))
    gq = np.zeros((128, 4, 128))
    gk = np.zeros((128, 4))
    for h in range(4):
        dd = (i[None, :] - i[:, None]).astype(f8)
        decayT[:, h, :] = np.where(dd >= 0, np.exp(dd * lg[h]), 0.0) * (256.0 ** -0.5)
        gq[:, h, :] = np.exp((i[None, :] + 1.0) * lg[h])
        gk[:, h] = np.exp((127.0 - i) * lg[h]) * (256.0 ** -0.5)
    gdec = [float(np.exp(128.0 * lg[h])) for h in range(4)]
    return dict(ident=ident, ones=ones, tri=tri, maskT=maskT, smask=smask,
                decayT=decayT.reshape(128, 512), gq=gq.reshape(128, 512), gk=gk), gdec


def _rope_tables(T):
    half = 128
    inv_freq = (np.float32(10000.0) ** (-np.arange(half, dtype=np.float32) / np.float32(half))).astype(np.float32)
    pos = np.arange(T, dtype=np.float32)
    ang = (inv_freq[:, None] * pos[None, :]).astype(np.float32)
    cs = np.stack([np.cos(ang.astype(np.float64)), np.sin(ang.astype(np.float64))], axis=1)
    return np.ascontiguousarray(cs.astype(np.float32))


def _proj_layout(W):
    n = W.shape[1] // 128
    return np.ascontiguousarray(W.reshape(16, 128, n, 128).transpose(2, 1, 0, 3).reshape(n, 128, 2048))


def _pcol(v, n):
    return v.reshape(n, 128).T


def prep_shared(inp):
    ada_w = inp["ada_w"]
    sh = {}
    sh["adaw"] = np.ascontiguousarray(ada_w.reshape(2, 16, 128, 144, 128).transpose(0, 3, 2, 1, 4).reshape(288, 128, 2048))
    sh["w13r"] = np.ascontiguousarray(inp["ffn_w13"].reshape(2, 2, 16, 128, 2, 44, 128).transpose(0, 1, 5, 3, 4, 2, 6).reshape(176, 128, 4096))
    sh["w2r"] = np.ascontiguousarray(inp["ffn_w2"].reshape(2, 2, 2, 22, 128, 16, 128).transpose(0, 1, 5, 2, 4, 3, 6).reshape(128, 128, 2816))
    evw = inp["ev_w_in"][0]
    sh["evin"] = _proj_layout(np.concatenate([evw[:, :3072], evw[:, 3088:]], axis=1))
    sh["evtail"] = np.ascontiguousarray(evw[:, 3072:3088].reshape(16, 128, 16).transpose(1, 0, 2).reshape(128, 256))
    sh["evout"] = _proj_layout(inp["ev_w_out"][0])
    sh["odin"] = _proj_layout(inp["od_w_in"][0])
    sh["odout"] = _proj_layout(inp["od_w_out"][0])
    g = np.stack([inp["od_gate_a_w"][0], inp["od_gate_x_w"][0]], axis=0)
    g = g.reshape(2, 8, 2, 128, 2, 128)
    sh["gatew"] = np.ascontiguousarray(g.transpose(1, 3, 0, 4, 2, 5).reshape(8, 128, 1024))
    return sh


def prep_smalls(inp, core):
    consts, _ = _consts()
    sm = np.zeros((128, NS), np.float32)

    def put(name, arr):
        o, w = _SM[name]
        arr = np.asarray(arr, np.float32).reshape(128, w)
        sm[:, o:o + w] = arr
    c = inp["c"][core * NBC:(core + 1) * NBC]
    put("cT", c.reshape(NBC, 16, 128).transpose(2, 1, 0))
    put("adab", inp["ada_b"].reshape(2, 144, 128).transpose(2, 0, 1))
    put("gpre", inp["norm_pre"].reshape(2, 3, 16, 128).transpose(3, 0, 1, 2))
    put("gpost", inp["norm_post"].reshape(2, 3, 16, 128).transpose(3, 0, 1, 2))
    put("evconv", inp["ev_conv_w"][0].reshape(4, 24, 128).transpose(2, 1, 0))
    put("alog", np.broadcast_to(inp["ev_a_log"][0][None, :], (128, 8)))
    put("dtb", np.broadcast_to(inp["ev_dt_bias"][0][None, :], (128, 8)))
    put("onorm", np.broadcast_to(inp["ev_o_norm"][0][None, :], (128, 128)))
    put("retnorm", np.broadcast_to(inp["ev_ret_norm"][0][None, :], (128, 1024)))
    put("odconv", inp["od_conv_w"][0].reshape(4, 16, 128).transpose(2, 1, 0))
    put("odconvb", _pcol(inp["od_conv_b"][0], 16))
    put("gab", _pcol(inp["od_gate_a_b"][0], 16))
    put("gxb", _pcol(inp["od_gate_x_b"][0], 16))
    put("lam", _pcol(inp["od_lambda"][0], 16))
    for k, v in consts.items():
        put(k, v)
    return sm


def kernel(**inputs):
    inp = {k: np.asarray(v) for k, v in inputs.items()}
    T = inp["x"].shape[1]
    nc = build(FULL_PLAN, T)
    sh = prep_shared(inp)
    cs = _rope_tables(T)
    in_maps = []
    for core in range(NCORES):
        m = dict(sh)
        m["x"] = np.ascontiguousarray(inp["x"][core * NBC:(core + 1) * NBC].reshape(NBC * T, D))
        m["smalls"] = prep_smalls(inp, core)
        m["cs"] = cs
        in_maps.append(m)
    res = run_bass_kernel_spmd(nc, in_maps, core_ids=list(range(NCORES)))
    outs = [res.results[i]["out"].reshape(NBC, T, D) for i in range(NCORES)]
    return np.concatenate(outs, axis=0).astype(np.float32)
```

```python
import numpy as np
import concourse.bass as bass
import concourse.mybir as mybir
from concourse.bass_utils import run_bass_kernel_spmd


EPOCH = 24000


class _St:
    __slots__ = ("lw", "rd", "excl")

    def __init__(self, excl):
        self.lw = None
        self.rd = {}
        self.excl = excl


class Buf:
    __slots__ = ("ap", "st", "name")

    def __init__(self, ap, name="", excl=False, st=None):
        self.ap = ap
        self.st = st if st is not None else _St(excl)
        self.name = name

    def view(self, ap):
        return Buf(ap, self.name, st=self.st)

    @property
    def lw(self):
        return self.st.lw

    @lw.setter
    def lw(self, v):
        self.st.lw = v

    @property
    def rd(self):
        return self.st.rd

    @rd.setter
    def rd(self, v):
        self.st.rd = v

    @property
    def excl(self):
        return self.st.excl

    def __getitem__(self, k):
        return self.ap[k]


class Track:
    def __init__(self, fw, name, step):
        self.fw = fw
        self.name = name
        self.step = step
        self.epoch = 0
        self.idx = 0
        self.sems = [fw._new_sem(f"{name}_e0")]

    def next_token(self):
        if (self.idx + 1) * self.step > EPOCH:
            self.epoch += 1
            self.idx = 0
            self.sems.append(self.fw._new_sem(f"{self.name}_e{self.epoch}"))
        self.idx += 1
        return (self, self.epoch, self.idx)


class Engine:
    def __init__(self, fw, name, strict_self):
        self.fw = fw
        self.name = name
        self.track = Track(fw, name, 1)
        self.ops = []
        self.seen = {}
        self.strict_self = strict_self


class FW:
    def __init__(self, nc):
        self.nc = nc
        self._stack = []
        self._semcount = 0
        self.eng = {
            "pe": Engine(self, "pe", False),
            "act": Engine(self, "act", True),
            "dve": Engine(self, "dve", True),
            "pool": Engine(self, "pool", True),
            "sp": Engine(self, "sp", True),
        }
        self.lanes = {}
        self.n_inst = 0

    def _new_sem(self, name):
        cm = self.nc.semaphore(name)
        s = cm.__enter__()
        self._stack.append(cm)
        self._semcount += 1
        return s

    def sbuf(self, name, shape, dtype):
        cm = self.nc.sbuf_tensor(name, list(shape), dtype)
        t = cm.__enter__()
        self._stack.append(cm)
        return t

    def psum(self, name, shape, dtype):
        cm = self.nc.psum_tensor(name, list(shape), dtype)
        t = cm.__enter__()
        self._stack.append(cm)
        return t

    def lane_pool(self, name, n):
        self.lanes[name] = dict(tracks=[Track(self, f"{name}{i}", 16) for i in range(n)], rr=0)

    def _waits(self, engine, reads, writes):
        need = {}
        for b in reads:
            if b.lw is not None:
                k = (b.lw[0], b.lw[1])
                need[k] = max(need.get(k, 0), b.lw[2])
        for b in writes:
            if b.lw is not None:
                k = (b.lw[0], b.lw[1])
                need[k] = max(need.get(k, 0), b.lw[2])
            for k, i in b.rd.items():
                need[k] = max(need.get(k, 0), i)
        out = []
        for k, i in need.items():
            tr, ep = k
            if tr is engine.track and not engine.strict_self:
                continue
            if engine.seen.get(k, 0) >= i:
                continue
            engine.seen[k] = i
            out.append((tr.sems[ep], i * tr.step))
        return out

    def _commit(self, tok, reads, writes):
        k = (tok[0], tok[1])
        for b in reads:
            b.rd[k] = max(b.rd.get(k, 0), tok[2])
        for b in writes:
            b.lw = tok
            b.rd = {}

    def op(self, ename, fn, reads=(), writes=()):
        e = self.eng[ename]
        if any(b.excl for b in reads):
            writes = list(writes) + [b for b in reads if b.excl]
            reads = [b for b in reads if not b.excl]
        waits = self._waits(e, reads, writes)
        tok = e.track.next_token()
        sem = tok[0].sems[tok[1]]
        e.seen[(tok[0], tok[1])] = max(e.seen.get((tok[0], tok[1]), 0), 0)

        def run(eng, waits=waits, fn=fn, sem=sem):
            for s, v in waits:
                eng.wait_ge(s, v)
            fn(eng).then_inc(sem, 1)
        e.ops.append(run)
        self._commit(tok, reads, writes)
        self.n_inst += 1
        return tok

    def dma(self, ename, pool, out_ap, in_ap, reads=(), writes=(), **kw):
        e = self.eng[ename]
        lp = self.lanes[pool]
        tr = lp["tracks"][lp["rr"] % len(lp["tracks"])]
        lp["rr"] += 1
        waits = self._waits(e, reads, writes)
        if tr.idx > 0:
            k = (tr, tr.epoch)
            if e.seen.get(k, 0) < tr.idx:
                e.seen[k] = tr.idx
                waits.append((tr.sems[tr.epoch], tr.idx * 16))
        tok = tr.next_token()
        sem = tr.sems[tok[1]]

        def run(eng, waits=waits, sem=sem, out_ap=out_ap, in_ap=in_ap, kw=kw):
            for s, v in waits:
                eng.wait_ge(s, v)
            eng.dma_start(out=out_ap, in_=in_ap, **kw).then_inc(sem, 16)
        e.ops.append(run)
        self._commit(tok, reads, writes)
        self.n_inst += 1
        return tok

    def final_wait(self, ename, bufs):
        e = self.eng[ename]
        waits = self._waits(e, bufs, ())

        def run(eng, waits=waits):
            for s, v in waits:
                eng.wait_ge(s, v)
        e.ops.append(run)

    def emit(self):
        nc = self.nc
        with nc.Block() as block:
            @block.tensor
            def _(eng):
                for f in self.eng["pe"].ops:
                    f(eng)

            @block.scalar
            def _(eng):
                for f in self.eng["act"].ops:
                    f(eng)

            @block.vector
            def _(eng):
                for f in self.eng["dve"].ops:
                    f(eng)

            @block.gpsimd
            def _(eng):
                for f in self.eng["pool"].ops:
                    f(eng)

            @block.sync
            def _(eng):
                for f in self.eng["sp"].ops:
                    f(eng)

    def close(self):
        while self._stack:
            self._stack.pop().__exit__(None, None, None)


def _fence(fw):
    tracks = [e.track for e in fw.eng.values()]
    for lp in fw.lanes.values():
        tracks += lp["tracks"]
    for e in fw.eng.values():
        waits = []
        for tr in tracks:
            if tr is e.track or tr.idx == 0:
                continue
            k = (tr, tr.epoch)
            if e.seen.get(k, 0) >= tr.idx:
                continue
            e.seen[k] = tr.idx
            waits.append((tr.sems[tr.epoch], tr.idx * tr.step))

        def run(eng, waits=waits):
            for s, v in waits:
                eng.wait_ge(s, v)
        e.ops.append(run)


F32 = mybir.dt.float32
BF16 = mybir.dt.bfloat16
AF = mybir.ActivationFunctionType
ALU = mybir.AluOpType

D = 2048
KC = 16
DFF = 5632
FC = 44
NBC = 2
NCORES = 8
EPS = 1e-6
LRU_C = 8.0

_SM = {}
_off = 0
for _n, _w in [("cT", 32), ("adab", 288), ("gpre", 96), ("gpost", 96), ("evconv", 96),
               ("alog", 8), ("dtb", 8), ("onorm", 128), ("retnorm", 1024),
               ("odconv", 64), ("odconvb", 16), ("gab", 16), ("gxb", 16), ("lam", 16),
               ("ident", 128), ("ones", 128), ("tri", 128), ("maskT", 128), ("smask", 128),
               ("decayT", 512), ("gq", 512), ("gk", 4)]:
    _SM[_n] = (_off, _w)
    _off += _w
NS = _off


FULL_PLAN = [("ffn", 0, 0, 0), ("gdn", 1), ("ffn", 0, 1, 2), ("ffn", 1, 0, 3), ("rglru", 4), ("ffn", 1, 1, 5)]


def build(plan=FULL_PLAN, T=2048):
    NTOK = NBC * T
    nc = bass.Bass("TRN2", target_bir_lowering=False)

    def din(name, shape, dt=F32):
        return nc.dram_tensor(name, list(shape), dt, kind="ExternalInput").ap()

    x_in = din("x", [NTOK, D])
    smalls_d = din("smalls", [128, NS])
    cs_d = din("cs", [128, 2, T])
    adaw_d = din("adaw", [2 * 144, 128, 2048])
    w13_d = din("w13r", [2 * 2 * FC, 128, 4096])
    w2_d = din("w2r", [2 * 2 * 16 * 2, 128, 22 * 128])
    evin_d = din("evin", [64, 128, 2048])
    evtail_d = din("evtail", [128, 256])
    evout_d = din("evout", [16, 128, 2048])
    odin_d = din("odin", [32, 128, 2048])
    odout_d = din("odout", [16, 128, 2048])
    gatew_d = din("gatew", [8, 128, 1024])
    out_d = nc.dram_tensor("out", [NTOK, D], F32, kind="ExternalOutput").ap()
    xs_d = nc.dram_tensor("xs", [KC, 128, NTOK], F32).ap()
    xs_v = xs_d.rearrange("k p t -> p k t")
    wcs_ffn = [nc.dram_tensor(f"wcs{i}", [128, 44 * 4096 + 32 * 2816], BF16).ap() for i in range(4)]
    wcs_mix = nc.dram_tensor("wcsm", [128, 64 * 2048 + 256 + 16 * 2048 + 32 * 2048 + 16 * 2048], BF16).ap()

    fw = FW(nc)
    fw_halo = fw.sbuf("halo", [128, 96], F32)
    fw_hst = fw.sbuf("hst", [128, 16], F32)
    fw_c1 = fw.sbuf("c1", [128, 32], F32)
    fw.lane_pool("w", 6)
    fw.lane_pool("wb", 6)
    fw.lane_pool("ws", 4)
    fw.lane_pool("a", 6)

    sm = fw.sbuf("sm", [128, NS], F32)
    smb = Buf(sm)

    def S(name, lo=0, hi=None):
        o, w = _SM[name]
        hi = w if hi is None else hi
        return sm[:, o + lo:o + hi]

    XYt = fw.sbuf("XY", [128, KC, 512], F32)
    XY = [Buf(XYt[:, k, :]) for k in range(KC)]
    hTt = fw.sbuf("hT", [128, KC, 512], BF16)
    hT = [Buf(hTt[:, k, :]) for k in range(KC)]
    BIG = fw.sbuf("BIG", [128, 11264], F32)
    WS = [Buf(fw.sbuf(f"ws{i}", [128, 4096], BF16)) for i in range(5)]
    wsi = [0]

    def wslot():
        b = WS[wsi[0] % len(WS)]
        wsi[0] += 1
        return b
    WSH = [Buf(WS[i][:, j * 2048:(j + 1) * 2048]) for i in range(len(WS)) for j in range(2)]
    wshi = [0]

    def wslot_h():
        b = WSH[wshi[0] % len(WSH)]
        wshi[0] += 1
        return b
    xch = [Buf(fw.sbuf(f"xch{i}", [128, 512], F32)) for i in range(2)]
    sqb = [Buf(fw.sbuf(f"sq{i}", [128, 512], F32)) for i in range(2)]
    tmpb = [Buf(fw.sbuf(f"tmp{i}", [128, 512], F32)) for i in range(3)]
    rstd = Buf(fw.sbuf("rstd", [128, 512], F32))
    modT = fw.sbuf("modT", [128, 2 * 144 * 2], F32)
    modTb = Buf(modT)
    Ab = fw.sbuf("Ab", [128, 6 * KC * 2], F32)
    Bb = fw.sbuf("Bb", [128, 6 * KC * 2], F32)
    Cb = fw.sbuf("Cb", [128, 6 * KC * 2], F32)
    ABCb = Buf(Ab)
    silc = Buf(fw.sbuf("silc", [128, 32], BF16))
    fws = [Buf(fw.sbuf(f"fws{i}", [128, 1024], F32)) for i in range(2)]
    gxb_ = [Buf(fw.sbuf(f"gx{i}", [128, 512], F32)) for i in range(4)]

    pst = [fw.psum(f"ps{i}", [128, 512], F32) for i in range(8)]
    BANK = [Buf(pst[i], excl=True) for i in range(8)]
    PSB = BANK[0:4]
    PSQ = [BANK[2 + i].view(pst[2 + i][:, 0:128]) for i in range(6)]
    pbi = [0]
    pqi = [0]

    def psb():
        b = PSB[pbi[0] % 4]
        pbi[0] += 1
        return b

    def psq():
        b = PSQ[pqi[0] % 6]
        pqi[0] += 1
        return b

    xsb = [Buf(None, f"xs{i}") for i in range(NTOK // 256)]
    outb = Buf(None, "out")

    rr = [0]

    def ew2():
        rr[0] += 1
        return "dve" if rr[0] % 2 else "act"

    def mm(ps, ps_ap, lhsT, rhs, start, stop, reads):
        fw.op("pe", lambda e: e.matmul(ps_ap, lhsT, rhs, start=start, stop=stop), reads=reads, writes=[ps])

    def tr(ps, ps_ap, in_ap, reads):
        fw.op("pe", lambda e: e.transpose(ps_ap, in_ap, S("ident")), reads=reads + [smb], writes=[ps])

    def act(out, in_, func, reads, writes, bias=None, scale=None):
        kw = {}
        if bias is not None:
            kw["bias"] = bias
        if scale is not None:
            kw["scale"] = scale
        fw.op("act", lambda e: e.activation(out=out, in_=in_, func=func, **kw), reads=reads, writes=writes)

    def copy(eng, out, in_, reads, writes):
        if eng == "act":
            act(out, in_, AF.Copy, reads, writes)
        else:
            fw.op(eng, lambda e: e.tensor_copy(out, in_), reads=reads, writes=writes)

    def ts(eng, out, in0, s1, s2, op0, op1, reads, writes):
        if op1 is None:
            fw.op(eng, lambda e: e.tensor_scalar(out, in0, s1, None, op0), reads=reads, writes=writes)
        else:
            fw.op(eng, lambda e: e.tensor_scalar(out, in0, s1, s2, op0, op1), reads=reads, writes=writes)

    def stt(eng, out, in0, sc, in1, op0, op1, reads, writes):
        fw.op(eng, lambda e: e.scalar_tensor_tensor(out, in0, sc, in1, op0, op1), reads=reads, writes=writes)

    def tt(eng, out, in0, in1, op, reads, writes):
        fw.op(eng, lambda e: e.tensor_tensor(out, in0, in1, op), reads=reads, writes=writes)

    def wload(slot, dst_ap, src_ap):
        fw.dma("pool", "w", dst_ap, src_ap, writes=[slot])

    wcache = {}
    wc_off = {}

    def wget(key, slot, ncols, src_ap):
        dst = slot[:, 0:ncols]
        if key in wcache:
            dap, dbuf = wcache[key]
            fw.dma("sp", "wb", dst, dap, reads=[dbuf], writes=[slot])
        else:
            wload(slot, dst, src_ap)
            if key[0] == "w13":
                gidx = key[1] * 2 + key[2]
            elif key[0] == "w2":
                gidx = key[1] // 32
            else:
                gidx = 4
            wt = wcs_ffn[gidx] if gidx < 4 else wcs_mix
            o = wc_off.get(gidx, 0)
            wc_off[gidx] = o + ncols
            dap = wt[:, o:o + ncols]
            dbuf = Buf(None, "wc")
            fw.dma("sp", "ws", dap, dst, reads=[slot], writes=[dbuf])
            wcache[key] = (dap, dbuf)

    fw.dma("sp", "a", sm[:, :], smalls_d[:, :], writes=[smb])
    act(silc[:], S("cT"), AF.Silu, [smb], [silc])
    for l in range(2):
        for q in range(144):
            slot = wslot()
            wload(slot, slot[:, 0:2048], adaw_d[l * 144 + q])
            ps = psq()
            for k in range(KC):
                mm(ps, ps[:, 0:2], slot[:, k * 128:(k + 1) * 128], silc[:, k * 2:k * 2 + 2], k == 0, k == KC - 1, [slot, silc])
            o = (l * 144 + q) * 2
            oa = _SM["adab"][0] + l * 144 + q
            ts("dve", modT[:, o:o + 2], ps[:, 0:2], sm[:, oa:oa + 1], None, ALU.add, None, [ps, smb], [modTb])
    mv = modT[:, :].rearrange("p (l q b) -> p l q b", l=2, q=144)
    Av = Ab[:, :].rearrange("p (s k b) -> p s k b", s=6, k=KC)
    Bv = Bb[:, :].rearrange("p (s k b) -> p s k b", s=6, k=KC)
    Cv = Cb[:, :].rearrange("p (s k b) -> p s k b", s=6, k=KC)
    for l in range(2):
        for s in range(3):
            ls = l * 3 + s
            resw = 1.0 if s == 1 else 0.5
            gp = S("gpre", ls * KC, (ls + 1) * KC)
            gq_ = S("gpost", ls * KC, (ls + 1) * KC)
            for b in range(NBC):
                sh = mv[:, l, (s * 3 + 0) * KC:(s * 3 + 1) * KC, b]
                sc = mv[:, l, (s * 3 + 1) * KC:(s * 3 + 2) * KC, b]
                ga = mv[:, l, (s * 3 + 2) * KC:(s * 3 + 3) * KC, b]
                stt("dve", Av[:, ls, :, b], sc, 1.0, gp, ALU.add, ALU.mult, [modTb, smb], [ABCb])
                copy("dve", Bv[:, ls, :, b], sh, [modTb], [ABCb])
                stt("dve", Cv[:, ls, :, b], ga, 1.0, gq_, ALU.add, ALU.mult, [modTb, smb], [ABCb])
                ts("dve", Cv[:, ls, :, b], Cv[:, ls, :, b], resw, None, ALU.mult, None, [ABCb], [ABCb])

    def Asc(tab, ls, k, b):
        return tab[:, ls, k, b:b + 1]

    def sumsq_rstd(srcs, TT, scale, eps):
        ps = psb()
        n = len(srcs)
        for k, (b, ap) in enumerate(srcs):
            sq = sqb[k % 2]
            act(sq[:, :TT], ap, AF.Square, [b], [sq])
            mm(ps, ps[:, :TT], S("ones"), sq[:, :TT], k == 0, k == n - 1, [sq, smb])
        act(rstd[:, :TT], ps[:, :TT], AF.Sqrt, [ps], [rstd], bias=eps, scale=scale)
        fw.op("dve", lambda e: e.reciprocal(rstd[:, :TT], rstd[:, :TT]), reads=[rstd], writes=[rstd])

    def prologue(ls, t0, TT, b):
        nb = TT // 256
        xbufs = [xsb[t0 // 256 + i] for i in range(nb)]
        fw.dma("sp", "a", XYt[:, :, :TT], xs_v[:, :, t0:t0 + TT], reads=xbufs, writes=XY)
        sumsq_rstd([(XY[k], XY[k][:, :TT]) for k in range(KC)], TT, 1.0 / D, EPS)
        for k in range(KC):
            tm = tmpb[k % 3]
            stt("dve", tm[:, :TT], XY[k][:, :TT], Asc(Av, ls, k, b), rstd[:, :TT], ALU.mult, ALU.mult, [XY[k], rstd, ABCb], [tm])
            act(hT[k][:, :TT], tm[:, :TT], AF.Identity, [tm, ABCb], [hT[k]], bias=Asc(Bv, ls, k, b))

    def epilogue(ls, t0, TT, b):
        nb = TT // 256
        xbufs = [xsb[t0 // 256 + i] for i in range(nb)]
        sumsq_rstd([(XY[k], XY[k][:, :TT]) for k in range(KC)], TT, 1.0 / D, EPS)
        for k in range(KC):
            xc = xch[k % 2]
            fw.dma("sp", "a", xc[:, :TT], xs_d[k, :, t0:t0 + TT], reads=xbufs, writes=[xc])
            tm = tmpb[k % 3]
            stt("dve", tm[:, :TT], XY[k][:, :TT], Asc(Cv, ls, k, b), rstd[:, :TT], ALU.mult, ALU.mult, [XY[k], rstd, ABCb], [tm])
            tt("dve", XY[k][:, :TT], tm[:, :TT], xc[:, :TT], ALU.add, [tm, xc], [XY[k]])
        fw.dma("sp", "a", xs_v[:, :, t0:t0 + TT], XYt[:, :, :TT], reads=XY, writes=xbufs)

    def proj_fm(wd_piece, TT, consume, key):
        slot = wslot_h()
        wget(key, slot, 2048, wd_piece)
        ps = psb()
        for k in range(KC):
            mm(ps, ps[:, :TT], slot[:, k * 128:(k + 1) * 128], hT[k][:, :TT], k == 0, k == KC - 1, [slot, hT[k]])
        consume(ps)

    xin = [Buf(BIG[:, i * 2048:(i + 1) * 2048]) for i in range(4)]
    for t0 in range(0, NTOK, 512):
        for blk in range(4):
            fw.dma("sp", "a", xin[blk][:], x_in[t0 + blk * 128:t0 + (blk + 1) * 128, :], writes=[xin[blk]])
        for k in range(KC):
            ps = psb()
            for blk in range(4):
                tr(ps, ps[:, blk * 128:(blk + 1) * 128], xin[blk][:, k * 128:(k + 1) * 128], [xin[blk]])
            copy(ew2(), XY[k][:], ps[:], [ps], [XY[k]])
        fw.dma("sp", "a", xs_v[:, :, t0:t0 + 512], XYt[:, :, :], reads=XY, writes=[xsb[t0 // 256], xsb[t0 // 256 + 1]])
    _fence(fw)

    def ffn_sandwich(l, f, ls):
        actT = BIG[:, :].bitcast(BF16)
        actb = [Buf(actT[:, j * 512:(j + 1) * 512]) for j in range(FC)]
        TT = 512
        xcb = xch
        sq_, acc_ = sqb
        rstdP = rstd
        tmP, tmE0, tmE1, rstdE = gxb_
        tmE = [tmE0, tmE1]

        def sumsq_acc(k, src_b, src_ap):
            act(sq_[:], src_ap, AF.Square, [src_b], [sq_])
            if k == 0:
                copy("dve", acc_[:], sq_[:], [sq_], [acc_])
            else:
                tt("dve", acc_[:], acc_[:], sq_[:], ALU.add, [acc_, sq_], [acc_])

        def finish_rstd(dst):
            ps = psb()
            mm(ps, ps[:], S("ones"), acc_[:], True, True, [acc_, smb])
            act(dst[:], ps[:], AF.Sqrt, [ps], [dst], bias=EPS, scale=1.0 / D)
            fw.op("dve", lambda e: e.reciprocal(dst[:], dst[:]), reads=[dst], writes=[dst])

        def pro_gen(t0):
            b = t0 // T
            xbufs = [xsb[t0 // 256], xsb[t0 // 256 + 1]]

            def ld(k):
                xc = xcb[k % 2]
                fw.dma("pool", "a", xc[:], xs_d[k, :, t0:t0 + TT], reads=xbufs, writes=[xc])
            ld(0)
            yield
            for k in range(KC):
                if k + 1 < KC:
                    ld(k + 1)
                sumsq_acc(k, xcb[k % 2], xcb[k % 2][:])
                yield
            ld(0)
            finish_rstd(rstdP)
            yield
            for k in range(KC):
                if k + 1 < KC:
                    ld(k + 1)
                xc = xcb[k % 2]
                stt("dve", tmP[:], xc[:], Asc(Av, ls, k, b), rstdP[:], ALU.mult, ALU.mult, [xc, rstdP, ABCb], [tmP])
                act(hT[k][:], tmP[:], AF.Identity, [tmP, ABCb], [hT[k]], bias=Asc(Bv, ls, k, b))
                yield

        def epi_gen(t0):
            b = t0 // T
            xbufs = [xsb[t0 // 256], xsb[t0 // 256 + 1]]

            def ld(k):
                xc = xcb[k % 2]
                fw.dma("pool", "a", xc[:], xs_d[k, :, t0:t0 + TT], reads=xbufs, writes=[xc])
            for k in range(KC):
                sumsq_acc(k, XY[k], XY[k][:])
                yield
            ld(0)
            finish_rstd(rstdE)
            yield
            for k in range(KC):
                if k + 1 < KC:
                    ld(k + 1)
                xc = xcb[k % 2]
                te = tmE[k % 2]
                stt("dve", te[:], XY[k][:], Asc(Cv, ls, k, b), rstdE[:], ALU.mult, ALU.mult, [XY[k], rstdE, ABCb], [te])
                tt("dve", te[:], te[:], xc[:], ALU.add, [te, xc], [te])
                fw.dma("pool", "a", xs_d[k, :, t0:t0 + TT], te[:], reads=[te], writes=xbufs)
                yield

        def pull(g, n):
            if g is None:
                return None
            for _ in range(n):
                try:
                    next(g)
                except StopIteration:
                    return None
            return g

        def drain(g):
            while g is not None:
                g = pull(g, 1)

        tiles = list(range(0, NTOK, TT))
        drain(pro_gen(tiles[0]))
        eg = None
        for ti, t0 in enumerate(tiles):
            for j in range(FC):
                slot = wslot()
                wget(("w13", l, f, j), slot, 4096, w13_d[(l * 2 + f) * FC + j])
                pg = psb()
                pu = psb()
                for k in range(KC):
                    mm(pg, pg[:], slot[:, k * 128:(k + 1) * 128], hT[k][:], k == 0, k == KC - 1, [slot, hT[k]])
                for k in range(KC):
                    mm(pu, pu[:], slot[:, 2048 + k * 128:2048 + (k + 1) * 128], hT[k][:], k == 0, k == KC - 1, [slot, hT[k]])
                tm = tmpb[j % 3]
                act(tm[:], pg[:], AF.Silu, [pg], [tm])
                tt("dve", actb[j][:], tm[:], pu[:], ALU.mult, [tm, pu], [actb[j]])
                eg = pull(eg, 1)
            drain(eg)
            pgn = pro_gen(tiles[ti + 1]) if ti + 1 < len(tiles) else None
            for m in range(KC):
                s0 = wslot()
                s1 = wslot()
                base = (((l * 2 + f) * 16) + m) * 2
                wget(("w2", base), s0, 2816, w2_d[base])
                wget(("w2", base + 1), s1, 2816, w2_d[base + 1])
                ps = psb()
                for c in range(FC):
                    sl = s0 if c < 22 else s1
                    cc = c % 22
                    mm(ps, ps[:], sl[:, cc * 128:(cc + 1) * 128], actb[c][:], c == 0, c == FC - 1, [sl, actb[c]])
                copy(ew2(), XY[m][:], ps[:], [ps], [XY[m]])
                pgn = pull(pgn, 3)
            drain(pgn)
            eg = epi_gen(t0)
        drain(eg)
        _fence(fw)

    def rglru_sandwich(ls):
        TT = 512
        ygt = BIG[:, 0:4096].bitcast(BF16)
        yg = [Buf(ygt[:, k * 512:(k + 1) * 512]) for k in range(KC)]
        xcv = [Buf(BIG[:, 4096 + i * 512:4096 + (i + 1) * 512]) for i in range(4)]
        xqb = [Buf(BIG[:, 6144 + i * 520:6144 + i * 520 + 515]) for i in range(2)]
        rb = [Buf(BIG[:, 7200 + i * 512:7200 + (i + 1) * 512]) for i in range(2)]
        ib = [Buf(BIG[:, 8224 + i * 512:8224 + (i + 1) * 512]) for i in range(2)]
        ab = [Buf(BIG[:, 9248 + i * 512:9248 + (i + 1) * 512]) for i in range(2)]
        hsb = [Buf(BIG[:, 10272:10784])]
        halo = Buf(fw_halo[:, 0:48])
        hstate = Buf(fw_hst[:, 0:16])
        c1 = Buf(fw_c1[:, 0:32])
        act(c1[:, 0:16], S("lam"), AF.Exp, [smb], [c1], scale=-1.0)
        act(c1[:, 0:16], c1[:, 0:16], AF.Ln, [c1], [c1], bias=1.0)
        ts("dve", c1[:, 16:32], c1[:, 0:16], LRU_C, None, ALU.mult, None, [c1], [c1])
        ts("dve", c1[:, 0:16], c1[:, 0:16], -LRU_C, None, ALU.mult, None, [c1], [c1])
        for t0 in range(0, NTOK, TT):
            b = t0 // T
            if t0 % T == 0:
                fw.op("dve", lambda e: e.memset(halo[:], 0.0), writes=[halo])
                fw.op("dve", lambda e: e.memset(hstate[:], 0.0), writes=[hstate])
            prologue(ls, t0, TT, b)
            def y_chunk(m):
                def cons(ps, m=m):
                    t1 = tmpb[0]
                    t2 = tmpb[1]
                    copy("act", t1[:], ps[:], [ps], [t1])
                    tt("pool", t2[:], t1[:], t1[:], ALU.mult, [t1], [t2])
                    ts("pool", t2[:], t2[:], 0.044715, 1.0, ALU.mult, ALU.add, [t2], [t2])
                    tt("pool", t2[:], t2[:], t1[:], ALU.mult, [t1, t2], [t2])
                    act(t2[:], t2[:], AF.Sigmoid, [t2], [t2], scale=1.5957691216057308)
                    tt("dve", yg[m][:], t1[:], t2[:], ALU.mult, [t1, t2], [yg[m]])
                proj_fm(odin_d[m], TT, cons, ("odin", m))

            def x_block(n):
                gsl = fws[n % 2]
                fw.dma("sp", "w", gsl[:, 0:1024], gatew_d[n], writes=[gsl])
                xcs = []
                for ic in range(2):
                    ci = n * 2 + ic
                    xq = xqb[ic]
                    xc = xcv[(n % 2) * 2 + ic]

                    def cons(ps, ci=ci, xq=xq, xc=xc):
                        copy("dve", xq[:, 0:3], halo[:, ci * 3:ci * 3 + 3], [halo], [xq])
                        copy("act", xq[:, 3:515], ps[:], [ps], [xq])
                        copy("dve", halo[:, ci * 3:ci * 3 + 3], xq[:, 512:515], [xq], [halo])
                        o = _SM["odconv"][0] + ci * 4
                        ob = _SM["odconvb"][0] + ci
                        ts("dve", xc[:], xq[:, 0:512], sm[:, o:o + 1], sm[:, ob:ob + 1], ALU.mult, ALU.add, [xq, smb], [xc])
                        for tap in range(1, 4):
                            stt("dve", xc[:], xq[:, tap:tap + 512], sm[:, o + tap:o + tap + 1], xc[:], ALU.mult, ALU.add, [xq, smb, xc], [xc])
                    proj_fm(odin_d[16 + ci], TT, cons, ("odin", 16 + ci))
                    xcs.append(xc)
                return gsl, xcs

            def gates(n, gsl, xcs):
                for jc in range(2):
                    cj = n * 2 + jc
                    pr = psb()
                    pi = psb()
                    for g, pp in ((0, pr), (1, pi)):
                        for ic in range(2):
                            o = ((g * 2 + jc) * 2 + ic) * 128
                            mm(pp, pp[:], gsl[:, o:o + 128], xcs[ic][:], ic == 0, ic == 1, [gsl, xcs[ic]])
                    r_ = rb[jc]
                    i_ = ib[jc]
                    a_ = ab[jc]
                    oga = _SM["gab"][0] + cj
                    ogx = _SM["gxb"][0] + cj
                    act(r_[:], pr[:], AF.Sigmoid, [pr, smb], [r_], bias=sm[:, oga:oga + 1])
                    act(i_[:], pi[:], AF.Sigmoid, [pi, smb], [i_], bias=sm[:, ogx:ogx + 1])
                    act(a_[:], r_[:], AF.Exp, [r_, c1], [a_], scale=c1[:, cj:cj + 1])
                    act(r_[:], r_[:], AF.Tanh, [r_, c1], [r_], scale=c1[:, 16 + cj:16 + cj + 1])
                    t2 = tmpb[2]
                    tt("dve", t2[:], a_[:], a_[:], ALU.mult, [a_], [t2])
                    stt("dve", t2[:], t2[:], 1.0, r_[:], ALU.add, ALU.mult, [t2, r_], [t2])
                    act(t2[:], t2[:], AF.Sqrt, [t2], [t2])
                    tt("pool", i_[:], i_[:], xcs[jc][:], ALU.mult, [i_, xcs[jc]], [i_])
                    tt("dve", t2[:], t2[:], i_[:], ALU.mult, [t2, i_], [t2])
                    hs = hsb[0]
                    fw.op("dve", lambda e, hs=hs, a_=a_, t2=t2, cj=cj: e.tensor_tensor_scan(hs[:], a_[:], t2[:], hstate[:, cj:cj + 1], ALU.mult, ALU.add),
                          reads=[a_, t2, hstate], writes=[hs])
                    copy("dve", hstate[:, cj:cj + 1], hs[:, 511:512], [hs], [hstate])
                    tt("dve", yg[cj][:], hs[:], yg[cj][:], ALU.mult, [hs, yg[cj]], [yg[cj]])

            pend = None
            for n in range(8):
                y_chunk(2 * n)
                y_chunk(2 * n + 1)
                cur = x_block(n)
                if pend is not None:
                    gates(*pend)
                pend = (n,) + cur
            gates(*pend)
            for m in range(KC):
                slot = wslot_h()
                wget(("odout", m), slot, 2048, odout_d[m])
                ps = psb()
                for k in range(KC):
                    mm(ps, ps[:], slot[:, k * 128:(k + 1) * 128], yg[k][:], k == 0, k == KC - 1, [slot, yg[k]])
                copy(ew2(), XY[m][:], ps[:], [ps], [XY[m]])
            epilogue(ls, t0, TT, b)
        _fence(fw)


    def gdn_sandwich(ls):
        TT = 256
        PE_ = ["dve"]
        _, gdec = _consts()
        AX = mybir.AxisListType.X
        oall = [Buf(BIG[:, blk * 2048:(blk + 1) * 2048]) for blk in range(2)]
        xq = [Buf(BIG[:, 4096 + i * 264:4096 + i * 264 + 259]) for i in range(3)]
        qkvS = [[Buf(BIG[:, 4888 + s * 768 + i * 256:4888 + s * 768 + (i + 1) * 256]) for i in range(3)] for s in range(2)]
        ktokS = [Buf(BIG[:, 6424 + s * 768:6424 + s * 768 + 256]) for s in range(2)]
        vtokS = [Buf(BIG[:, 6680 + s * 768:6680 + s * 768 + 256]) for s in range(2)]
        ztokS = [Buf(BIG[:, 6936 + s * 768:6936 + s * 768 + 256]) for s in range(2)]
        G = [Buf(BIG[:, 8192 + i * 128:8192 + (i + 1) * 128]) for i in range(24)]
        G += [Buf(XYt[:, k, 256 + j * 128:256 + (j + 1) * 128]) for k in range(KC) for j in range(2)]

        class _R:
            pass
        roles = {}
        gi = 0
        for s in range(2):
            for blk in range(2):
                C = _R()
                (C.P0, C.N0, C.Pa, C.Na, C.Xa, C.Xb, C.QKT, C.kegT, C.qstT, C.kst) = G[gi:gi + 10]
                gi += 10
                roles[(s, blk)] = C
        hroles = []
        for s in range(2):
            Hh = _R()
            (Hh.Rb, Hh.vnew, Hh.osq, Hh.ob) = G[gi:gi + 4]
            gi += 4
            hroles.append(Hh)
        scT, qin0, qin1 = G[gi:gi + 3]
        osq, ob = hroles[0].osq, hroles[0].ob
        rq = [Buf(BIG[:, 4096 + i * 256:4096 + (i + 1) * 256]) for i in range(2)]
        rk = [Buf(BIG[:, 4608 + i * 256:4608 + (i + 1) * 256]) for i in range(2)]
        rqr = [Buf(BIG[:, 5120 + i * 256:5120 + (i + 1) * 256]) for i in range(2)]
        rkr = [Buf(BIG[:, 5632 + i * 256:5632 + (i + 1) * 256]) for i in range(2)]
        rtm = [Buf(BIG[:, 6144 + i * 256:6144 + (i + 1) * 256]) for i in range(2)]
        rktok = Buf(BIG[:, 6656:7168])
        rvtok = Buf(BIG[:, 7168:7680])
        rgtok = Buf(BIG[:, 7680:8192])
        gsm = fw.sbuf("gsm", [128, 8 * 16 + 8], F32)
        gsmb = Buf(gsm)
        Sg_t = fw.sbuf("Sg", [128, 1024], F32)
        Sg = [Buf(Sg_t[:, h * 128:(h + 1) * 128]) for h in range(8)]
        Sr_t = fw.sbuf("Sr", [128, 2048], F32)
        Sr = [Buf(Sr_t[:, h * 512:(h + 1) * 512]) for h in range(4)]
        cst = fw.sbuf("cst", [128, 2, 256], F32)
        cstb = Buf(cst)
        halo = Buf(fw_halo[:, 0:72])
        ssb_t = fw.sbuf("ssb", [128, 4], F32)
        ssb = Buf(ssb_t[:, 0:2])
        ssbS = [Buf(ssb_t[:, 2 + s:3 + s]) for s in range(2)]

        def gs(i, blk, lo=0, hi=8):
            return gsm[:, (i * 2 + blk) * 8 + lo:(i * 2 + blk) * 8 + hi]
        NEGA = gsm[:, 128:136]
        act(NEGA, S("alog"), AF.Exp, [smb], [gsmb])
        ts("dve", NEGA, NEGA, -1.0, None, ALU.mult, None, [gsmb], [gsmb])

        def rowsum_rstd(ps_ap, width, src):
            act(osq[:, :] if width <= 128 else rtm[0][:, :width], ps_ap, AF.Square, [src], [osq if width <= 128 else rtm[0]])
            sqap = osq[:, :] if width <= 128 else rtm[0][:, :width]
            sqb_ = osq if width <= 128 else rtm[0]
            fw.op("dve", lambda e: e.reduce_sum(ssb[:, 0:1], sqap, AX), reads=[sqb_], writes=[ssb])
            act(ssb[:, 0:1], ssb[:, 0:1], AF.Sqrt, [ssb], [ssb], bias=EPS, scale=1.0 / width)
            fw.op("dve", lambda e: e.reciprocal(ssb[:, 0:1], ssb[:, 0:1]), reads=[ssb], writes=[ssb])


        def run_gens(gens):
            active = list(gens)
            while active:
                for g in list(active):
                    try:
                        next(g)
                    except StopIteration:
                        active.remove(g)

        def stage_a_gen(s, h):
            qkv = qkvS[s]
            for idx, m in enumerate((h, 8 + h, 16 + h)):
                def cons(ps, idx=idx, m=m):
                    x_ = xq[idx]
                    copy("dve", x_[:, 0:3], halo[:, m * 3:m * 3 + 3], [halo], [x_])
                    copy("act", x_[:, 3:259], ps[:, :TT], [ps], [x_])
                    copy("dve", halo[:, m * 3:m * 3 + 3], x_[:, 256:259], [x_], [halo])
                    o = _SM["evconv"][0] + m * 4
                    tm = tmpb[idx]
                    ts("dve", tm[:, :TT], x_[:, 0:TT], sm[:, o:o + 1], None, ALU.mult, None, [x_, smb], [tm])
                    for tap in range(1, 4):
                        stt("dve", tm[:, :TT], x_[:, tap:tap + TT], sm[:, o + tap:o + tap + 1], tm[:, :TT], ALU.mult, ALU.add, [x_, smb, tm], [tm])
                    act(qkv[idx][:], tm[:, :TT], AF.Silu, [tm], [qkv[idx]])
                proj_fm(evin_d[m], TT, cons, ("evin", m))
                yield
            qT, kT, vT = qkv
            sumsq_rstd([(qT, qT[:])], TT, 1.0, EPS)
            stt("dve", qT[:], qT[:], 128.0 ** -0.5, rstd[:, :TT], ALU.mult, ALU.mult, [qT, rstd], [qT])
            yield
            sumsq_rstd([(kT, kT[:])], TT, 1.0, EPS)
            tt("dve", kT[:], kT[:], rstd[:, :TT], ALU.mult, [kT, rstd], [kT])
            yield

        def stage_b(s, h):
            qT, kT, vT = qkvS[s]
            zsl = wslot_h()
            wget(("evin", 24 + h), zsl, 2048, evin_d[24 + h])
            ktok, vtok, ztok = ktokS[s], vtokS[s], ztokS[s]
            for blk in range(2):
                ps = psq()
                for k in range(KC):
                    mm(ps, ps[:], hT[k][:, blk * 128:(blk + 1) * 128], zsl[:, k * 128:(k + 1) * 128], k == 0, k == KC - 1, [zsl, hT[k]])
                act(ztok[:, blk * 128:(blk + 1) * 128], ps[:], AF.Silu, [ps], [ztok])
                pk_ = psq()
                tr(pk_, pk_[:], kT[:, blk * 128:(blk + 1) * 128], [kT])
                copy("act", ktok[:, blk * 128:(blk + 1) * 128], pk_[:], [pk_], [ktok])
                pv_ = psq()
                tr(pv_, pv_[:], vT[:, blk * 128:(blk + 1) * 128], [vT])
                copy("dve", vtok[:, blk * 128:(blk + 1) * 128], pv_[:], [pv_], [vtok])

        def chain_gen(s, blk, h):
            C = roles[(s, blk)]
            qT, kT, vT = qkvS[s]
            ktok = ktokS[s]
            cs_ = slice(blk * 128, (blk + 1) * 128)
            bcol = gs(0, blk, h, h + 1)
            ldcol = gs(1, blk, h, h + 1)
            gcol = gs(2, blk, h, h + 1)
            eglcol = gs(5, blk, h, h + 1)
            ldb, egb, DT = C.Pa, C.Na, C.Xb
            P0, N0, QKT, kegT, qstT, kst = C.P0, C.N0, C.QKT, C.kegT, C.qstT, C.kst
            ts("dve", ldb[:], S("ones"), ldcol, None, ALU.mult, None, [smb, gsmb], [ldb])
            pg = psq()
            mm(pg, pg[:], ldb[:], S("tri"), True, True, [ldb, smb])
            act(egb[:], pg[:], AF.Exp, [pg], [egb])
            stt("dve", DT[:], pg[:], gcol, S("maskT"), ALU.subtract, ALU.add, [pg, gsmb, smb], [DT])
            yield
            act(DT[:], DT[:], AF.Exp, [DT], [DT])
            pkk = psq()
            mm(pkk, pkk[:], kT[:, cs_], kT[:, cs_], True, True, [kT])
            pkq = psq()
            mm(pkq, pkq[:], kT[:, cs_], qT[:, cs_], True, True, [kT, qT])
            tt(PE_[0], kegT[:], kT[:, cs_], egb[:], ALU.mult, [kT, egb], [kegT])
            tt(PE_[0], qstT[:], qT[:, cs_], egb[:], ALU.mult, [qT, egb], [qstT])
            act(kst[:], ktok[:, cs_], AF.Copy, [ktok, gsmb], [kst], scale=eglcol)
            stt("dve", P0[:], pkk[:], bcol, DT[:], ALU.mult, ALU.mult, [pkk, gsmb, DT], [P0])
            tt("dve", QKT[:], pkq[:], DT[:], ALU.mult, [pkq, DT], [QKT])
            yield
            tt(PE_[0], P0[:], P0[:], S("smask"), ALU.mult, [P0, smb], [P0])
            pt = psq()
            tr(pt, pt[:], P0[:], [P0])
            copy("act", N0[:], pt[:], [pt], [N0])
            stt("dve", C.Xa[:], P0[:], -1.0, S("ident"), ALU.mult, ALU.add, [P0, smb], [C.Xa])
            yield
            X, Xo = C.Xa, C.Xb
            Pk, Nk = P0, N0
            Pn, Nn = C.Pa, C.Na
            for lvl in range(6):
                if lvl < 5:
                    pp = psq()
                    mm(pp, pp[:], Nk[:], Pk[:], True, True, [Nk, Pk])
                pn = psq()
                mm(pn, pn[:], Pk[:], Nk[:], True, True, [Nk, Pk])
                if lvl < 5:
                    copy("act", Pn[:], pp[:], [pp], [Pn])
                copy("dve", Nn[:], pn[:], [pn], [Nn])
                yield
                px = psq()
                mm(px, px[:], Nn[:], X[:], True, True, [Nn, X])
                tt("dve", Xo[:], X[:], px[:], ALU.add, [X, px], [Xo])
                yield
                X, Xo = Xo, X
                Pk, Nk, Pn, Nn = Pn, Nn, Pk, Nk
            C.X = X

        def sphase_gen(s, h):
            Hh = hroles[s]
            vtok, ztok = vtokS[s], ztokS[s]
            sb_ = ssbS[s]
            for blk in range(2):
                C = roles[(s, blk)]
                cs_ = slice(blk * 128, (blk + 1) * 128)
                bcol = gs(0, blk, h, h + 1)
                deccol = gs(6, blk, h, h + 1)
                pks = psq()
                mm(pks, pks[:], C.kegT[:], Sg[h][:], True, True, [C.kegT, Sg[h]])
                tt("dve", Hh.Rb[:], vtok[:, cs_], pks[:], ALU.subtract, [vtok, pks], [Hh.Rb])
                yield
                pv = psq()
                mm(pv, pv[:], C.X[:], Hh.Rb[:], True, True, [C.X, Hh.Rb])
                ts("dve", Hh.vnew[:], pv[:], bcol, None, ALU.mult, None, [pv, gsmb], [Hh.vnew])
                yield
                po = psq()
                mm(po, po[:], C.qstT[:], Sg[h][:], True, False, [C.qstT, Sg[h]])
                mm(po, po[:], C.QKT[:], Hh.vnew[:], False, True, [C.QKT, Hh.vnew])
                psn = psq()
                mm(psn, psn[:], C.kst[:], Hh.vnew[:], True, True, [C.kst, Hh.vnew])
                act(Hh.osq[:], po[:], AF.Square, [po], [Hh.osq])
                copy("dve", Hh.ob[:], po[:], [po], [Hh.ob])
                stt("dve", Sg[h][:], Sg[h][:], deccol, psn[:], ALU.mult, ALU.add, [Sg[h], gsmb, psn], [Sg[h]])
                yield
                fw.op("dve", lambda e, Hh=Hh, sb_=sb_: e.reduce_sum(sb_[:], Hh.osq[:], AX), reads=[Hh.osq], writes=[sb_])
                yield
                act(sb_[:], sb_[:], AF.Sqrt, [sb_], [sb_], bias=EPS, scale=1.0 / 128)
                yield
                fw.op("dve", lambda e, sb_=sb_: e.reciprocal(sb_[:], sb_[:]), reads=[sb_], writes=[sb_])
                yield
                stt("dve", Hh.ob[:], Hh.ob[:], sb_[:], S("onorm"), ALU.mult, ALU.mult, [Hh.ob, sb_, smb], [Hh.ob])
                yield
                tt(PE_[0], oall[blk][:, h * 128:(h + 1) * 128], Hh.ob[:], ztok[:, cs_], ALU.mult, [Hh.ob, ztok], [oall[blk]])
                yield


        class _RS:
            pass
        RS = []
        for s_ in range(2):
            if s_ == 0:
                pcs = [Buf(BIG[:, 4096 + i * 256:4096 + (i + 1) * 256]) for i in range(16)]
            else:
                pcs = [Buf(XYt[:, k, 256:512]) for k in range(KC)]
            R = _RS()
            R.rq, R.rk, R.rqr, R.rkr, R.rtm = pcs[0:2], pcs[2:4], pcs[4:6], pcs[6:8], pcs[8:10]
            R.rktok, R.rvtok, R.rgtok = pcs[10:12], pcs[12:14], pcs[14:16]
            R.scT, R.qin0, R.qin1 = G[s_ * 3:s_ * 3 + 3]
            RS.append(R)

        def ret_gen(h, R, sb_):
            for i in range(2):
                proj_fm(evin_d[32 + 2 * h + i], TT, lambda ps, i=i: copy("act", R.rq[i][:], ps[:, :TT], [ps], [R.rq[i]]), ("evin", 32 + 2 * h + i))
                yield
            for i in range(2):
                proj_fm(evin_d[40 + 2 * h + i], TT, lambda ps, i=i: copy("dve", R.rk[i][:], ps[:, :TT], [ps], [R.rk[i]]), ("evin", 40 + 2 * h + i))
                yield
            for src, dst in ((R.rq, R.rqr), (R.rk, R.rkr)):
                x1, x2 = src
                t0_, t1_ = R.rtm
                tt("dve", t0_[:], x1[:], cst[:, 0, :], ALU.mult, [x1, cstb], [t0_])
                tt(PE_[0], t1_[:], x2[:], cst[:, 1, :], ALU.mult, [x2, cstb], [t1_])
                tt("dve", dst[0][:], t0_[:], t1_[:], ALU.subtract, [t0_, t1_], [dst[0]])
                yield
                tt("dve", t0_[:], x2[:], cst[:, 0, :], ALU.mult, [x2, cstb], [t0_])
                tt(PE_[0], t1_[:], x1[:], cst[:, 1, :], ALU.mult, [x1, cstb], [t1_])
                tt("dve", dst[1][:], t0_[:], t1_[:], ALU.add, [t0_, t1_], [dst[1]])
                yield
            for base, dstl, fn in ((48, R.rvtok, None), (56, R.rgtok, AF.Silu)):
                sl = [wslot_h(), wslot_h()]
                for i in range(2):
                    wget(("evin", base + 2 * h + i), sl[i], 2048, evin_d[base + 2 * h + i])
                for blk in range(2):
                    ps = psb()
                    for i in range(2):
                        for k in range(KC):
                            mm(ps, ps[:, i * 128:(i + 1) * 128], hT[k][:, blk * 128:(blk + 1) * 128], sl[i][:, k * 128:(k + 1) * 128], k == 0, k == KC - 1, [sl[i], hT[k]])
                    if fn is None:
                        copy("dve", dstl[blk][:], ps[:, 0:256], [ps], [dstl[blk]])
                    else:
                        act(dstl[blk][:], ps[:, 0:256], fn, [ps], [dstl[blk]])
                    yield
            og = _SM["gk"][0] + h
            for blk in range(2):
                for i in range(2):
                    pk_ = psq()
                    tr(pk_, pk_[:], R.rkr[i][:, blk * 128:(blk + 1) * 128], [R.rkr[i]])
                    ts("dve", R.rktok[blk][:, i * 128:(i + 1) * 128], pk_[:], sm[:, og:og + 1], None, ALU.mult, None, [pk_, smb], [R.rktok[blk]])
                yield
            for blk in range(2):
                cs_ = slice(blk * 128, (blk + 1) * 128)
                vb_ = R.rvtok[blk]
                psc = psq()
                mm(psc, psc[:], R.rkr[0][:, cs_], R.rqr[0][:, cs_], True, False, [R.rkr[0], R.rqr[0]])
                mm(psc, psc[:], R.rkr[1][:, cs_], R.rqr[1][:, cs_], False, True, [R.rkr[1], R.rqr[1]])
                tt("dve", R.scT[:], psc[:], S("decayT", h * 128, (h + 1) * 128), ALU.mult, [psc, smb], [R.scT])
                tt(PE_[0], R.qin0[:], R.rqr[0][:, cs_], S("gq", h * 128, (h + 1) * 128), ALU.mult, [R.rqr[0], smb], [R.qin0])
                tt(PE_[0], R.qin1[:], R.rqr[1][:, cs_], S("gq", h * 128, (h + 1) * 128), ALU.mult, [R.rqr[1], smb], [R.qin1])
                yield
                po = psb()
                mm(po, po[:, 0:256], R.scT[:], vb_[:], True, False, [R.scT, vb_])
                mm(po, po[:, 0:256], R.qin0[:], Sr[h][:, 0:256], False, False, [R.qin0, Sr[h]])
                mm(po, po[:, 0:256], R.qin1[:], Sr[h][:, 256:512], False, True, [R.qin1, Sr[h]])
                for i in range(2):
                    ps = psb()
                    mm(ps, ps[:, 0:256], R.rktok[blk][:, i * 128:(i + 1) * 128], vb_[:], True, True, [R.rktok[blk], vb_])
                    stt("dve", Sr[h][:, i * 256:(i + 1) * 256], Sr[h][:, i * 256:(i + 1) * 256], gdec[h], ps[:, 0:256], ALU.mult, ALU.add, [Sr[h], ps], [Sr[h]])
                act(R.rtm[0][:], po[:, 0:256], AF.Square, [po], [R.rtm[0]])
                copy("dve", R.rtm[1][:], po[:, 0:256], [po], [R.rtm[1]])
                yield
                fw.op("dve", lambda e, R=R, sb_=sb_: e.reduce_sum(sb_[:], R.rtm[0][:], AX), reads=[R.rtm[0]], writes=[sb_])
                yield
                act(sb_[:], sb_[:], AF.Sqrt, [sb_], [sb_], bias=EPS, scale=1.0 / 256)
                yield
                fw.op("dve", lambda e, sb_=sb_: e.reciprocal(sb_[:], sb_[:]), reads=[sb_], writes=[sb_])
                yield
                stt("dve", R.rtm[1][:], R.rtm[1][:], sb_[:], S("retnorm", h * 256, (h + 1) * 256), ALU.mult, ALU.mult, [R.rtm[1], sb_, smb], [R.rtm[1]])
                yield
                tt(PE_[0], oall[blk][:, 1024 + h * 256:1024 + (h + 1) * 256], R.rtm[1][:], R.rgtok[blk][:], ALU.mult, [R.rtm[1], R.rgtok[blk]], [oall[blk]])
                yield

        for t0 in range(0, NTOK, TT):
            b = t0 // T
            tl = t0 % T
            PE_[0] = "pool" if t0 > 0 else "dve"
            if tl == 0:
                fw.op("dve", lambda e: e.memset(halo[:], 0.0), writes=[halo])
                fw.op("dve", lambda e: e.memset(Sg_t[:, :], 0.0), writes=Sg)
                fw.op("dve", lambda e: e.memset(Sr_t[:, :], 0.0), writes=Sr)
            prologue(ls, t0, TT, b)
            fw.dma("sp", "a", cst[:, :, :], cs_d[:, :, tl:tl + TT], writes=[cstb])
            tsl = wslot_h()
            wget(("evtail",), tsl, 256, evtail_d[:, :])
            for blk in range(2):
                ps = psq()
                for k in range(KC):
                    mm(ps, ps[:, 0:16], hT[k][:, blk * 128:(blk + 1) * 128], tsl[:, k * 16:(k + 1) * 16], k == 0, k == KC - 1, [tsl, hT[k]])
                act(gs(0, blk), ps[:, 0:8], AF.Sigmoid, [ps], [gsmb])
                tt("dve", gs(7, blk), ps[:, 8:16], S("dtb"), ALU.add, [ps, smb], [gsmb])
                act(gs(7, blk), gs(7, blk), AF.Exp, [gsmb], [gsmb])
                act(gs(7, blk), gs(7, blk), AF.Ln, [gsmb], [gsmb], bias=1.0)
                tt("dve", gs(1, blk), gs(7, blk), NEGA, ALU.mult, [gsmb], [gsmb])
                p2 = psq()
                mm(p2, p2[:, 0:8], S("tri"), gs(1, blk), True, True, [smb, gsmb])
                copy("dve", gs(2, blk), p2[:, 0:8], [p2], [gsmb])
                p3 = psq()
                mm(p3, p3[:, 0:8], S("ones"), gs(1, blk), True, True, [smb, gsmb])
                copy("dve", gs(3, blk), p3[:, 0:8], [p3], [gsmb])
                act(gs(4, blk), gs(2, blk), AF.Exp, [gsmb], [gsmb])
                tt("dve", gs(5, blk), gs(3, blk), gs(2, blk), ALU.subtract, [gsmb], [gsmb])
                act(gs(5, blk), gs(5, blk), AF.Exp, [gsmb], [gsmb])
                act(gs(6, blk), gs(3, blk), AF.Exp, [gsmb], [gsmb])
            run_gens([stage_a_gen(s, s) for s in range(2)])
            for hp in range(4):
                for s in range(2):
                    stage_b(s, 2 * hp + s)
                run_gens([chain_gen(s, blk, 2 * hp + s) for s in range(2) for blk in range(2)])
                gl = [sphase_gen(s, 2 * hp + s) for s in range(2)]
                if hp < 3:
                    gl += [stage_a_gen(s, 2 * (hp + 1) + s) for s in range(2)]
                run_gens(gl)
            _fence(fw)
            for hp in range(2):
                run_gens([ret_gen(2 * hp + s, RS[s], ssbS[s]) for s in range(2)])
            for blk in range(2):
                for k in range(KC):
                    ps = psq()
                    tr(ps, ps[:], oall[blk][:, k * 128:(k + 1) * 128], [oall[blk]])
                    copy(ew2(), hT[k][:, blk * 128:(blk + 1) * 128], ps[:], [ps], [hT[k]])
            for m in range(KC):
                proj_fm(evout_d[m], TT, lambda ps, m=m: copy(ew2(), XY[m][:, :TT], ps[:, :TT], [ps], [XY[m]]), ("evout", m))
            epilogue(ls, t0, TT, b)
            _fence(fw)
        _fence(fw)

    for item in plan:
        if item[0] == "ffn":
            ffn_sandwich(item[1], item[2], item[3])
        elif item[0] == "rglru":
            rglru_sandwich(item[1])
        elif item[0] == "gdn":
            gdn_sandwich(item[1])

    ot = [Buf(BIG[:, i * 2048:(i + 1) * 2048]) for i in range(4)]
    for t0 in range(0, NTOK, 512):
        fw.dma("sp", "a", XYt[:, :, :], xs_v[:, :, t0:t0 + 512], reads=[xsb[t0 // 256], xsb[t0 // 256 + 1]], writes=XY)
        for blk in range(4):
            for q in range(4):
                ps = psb()
                for kk in range(4):
                    k = q * 4 + kk
                    tr(ps, ps[:, kk * 128:(kk + 1) * 128], XY[k][:, blk * 128:(blk + 1) * 128], [XY[k]])
                copy(ew2(), ot[blk][:, q * 512:(q + 1) * 512], ps[:], [ps], [ot[blk]])
            fw.dma("sp", "a", out_d[t0 + blk * 128:t0 + (blk + 1) * 128, :], ot[blk][:], reads=[ot[blk]], writes=[outb])
    fw.final_wait("sp", [outb])
    fw.emit()
    fw.close()
    return nc


def _consts():
    f8 = np.float64
    i = np.arange(128)
    ident = np.eye(128)
    ones = np.ones((128, 128))
    tri = (i[:, None] <= i[None, :]).astype(f8)
    maskT = np.where(i[None, :] >= i[:, None], 0.0, -1e30)
    smask = (i[None, :] > i[:, None]).astype(f8)
    lg = np.log1p(-np.exp2(-5.0 - np.arange(4, dtype=np.float32))).astype(np.float32).astype(f8)
    decayT = np.zeros((128, 4, 128))
    gq = np.zeros((128, 4, 128))
    gk = np.zeros((128, 4))
    for h in range(4):
        dd = (i[None, :] - i[:, None]).astype(f8)
        decayT[:, h, :] = np.where(dd >= 0, np.exp(dd * lg[h]), 0.0) * (256.0 ** -0.5)
        gq[:, h, :] = np.exp((i[None, :] + 1.0) * lg[h])
        gk[:, h] = np.exp((127.0 - i) * lg[h]) * (256.0 ** -0.5)
    gdec = [float(np.exp(128.0 * lg[h])) for h in range(4)]
    return dict(ident=ident, ones=ones, tri=tri, maskT=maskT, smask=smask,
                decayT=decayT.reshape(128, 512), gq=gq.reshape(128, 512), gk=gk), gdec


def _rope_tables(T):
    half = 128
    inv_freq = (np.float32(10000.0) ** (-np.arange(half, dtype=np.float32) / np.float32(half))).astype(np.float32)
    pos = np.arange(T, dtype=np.float32)
    ang = (inv_freq[:, None] * pos[None, :]).astype(np.float32)
    cs = np.stack([np.cos(ang.astype(np.float64)), np.sin(ang.astype(np.float64))], axis=1)
    return np.ascontiguousarray(cs.astype(np.float32))


def _proj_layout(W):
    n = W.shape[1] // 128
    return np.ascontiguousarray(W.reshape(16, 128, n, 128).transpose(2, 1, 0, 3).reshape(n, 128, 2048))


def _pcol(v, n):
    return v.reshape(n, 128).T


def prep_shared(inp):
    ada_w = inp["ada_w"]
    sh = {}
    sh["adaw"] = np.ascontiguousarray(ada_w.reshape(2, 16, 128, 144, 128).transpose(0, 3, 2, 1, 4).reshape(288, 128, 2048))
    sh["w13r"] = np.ascontiguousarray(inp["ffn_w13"].reshape(2, 2, 16, 128, 2, 44, 128).transpose(0, 1, 5, 3, 4, 2, 6).reshape(176, 128, 4096))
    sh["w2r"] = np.ascontiguousarray(inp["ffn_w2"].reshape(2, 2, 2, 22, 128, 16, 128).transpose(0, 1, 5, 2, 4, 3, 6).reshape(128, 128, 2816))
    evw = inp["ev_w_in"][0]
    sh["evin"] = _proj_layout(np.concatenate([evw[:, :3072], evw[:, 3088:]], axis=1))
    sh["evtail"] = np.ascontiguousarray(evw[:, 3072:3088].reshape(16, 128, 16).transpose(1, 0, 2).reshape(128, 256))
    sh["evout"] = _proj_layout(inp["ev_w_out"][0])
    sh["odin"] = _proj_layout(inp["od_w_in"][0])
    sh["odout"] = _proj_layout(inp["od_w_out"][0])
    g = np.stack([inp["od_gate_a_w"][0], inp["od_gate_x_w"][0]], axis=0)
    g = g.reshape(2, 8, 2, 128, 2, 128)
    sh["gatew"] = np.ascontiguousarray(g.transpose(1, 3, 0, 4, 2, 5).reshape(8, 128, 1024))
    return sh


def prep_smalls(inp, core):
    consts, _ = _consts()
    sm = np.zeros((128, NS), np.float32)

    def put(name, arr):
        o, w = _SM[name]
        arr = np.asarray(arr, np.float32).reshape(128, w)
        sm[:, o:o + w] = arr
    c = inp["c"][core * NBC:(core + 1) * NBC]
    put("cT", c.reshape(NBC, 16, 128).transpose(2, 1, 0))
    put("adab", inp["ada_b"].reshape(2, 144, 128).transpose(2, 0, 1))
    put("gpre", inp["norm_pre"].reshape(2, 3, 16, 128).transpose(3, 0, 1, 2))
    put("gpost", inp["norm_post"].reshape(2, 3, 16, 128).transpose(3, 0, 1, 2))
    put("evconv", inp["ev_conv_w"][0].reshape(4, 24, 128).transpose(2, 1, 0))
    put("alog", np.broadcast_to(inp["ev_a_log"][0][None, :], (128, 8)))
    put("dtb", np.broadcast_to(inp["ev_dt_bias"][0][None, :], (128, 8)))
    put("onorm", np.broadcast_to(inp["ev_o_norm"][0][None, :], (128, 128)))
    put("retnorm", np.broadcast_to(inp["ev_ret_norm"][0][None, :], (128, 1024)))
    put("odconv", inp["od_conv_w"][0].reshape(4, 16, 128).transpose(2, 1, 0))
    put("odconvb", _pcol(inp["od_conv_b"][0], 16))
    put("gab", _pcol(inp["od_gate_a_b"][0], 16))
    put("gxb", _pcol(inp["od_gate_x_b"][0], 16))
    put("lam", _pcol(inp["od_lambda"][0], 16))
    for k, v in consts.items():
        put(k, v)
    return sm


def kernel(**inputs):
    inp = {k: np.asarray(v) for k, v in inputs.items()}
    T = inp["x"].shape[1]
    nc = build(FULL_PLAN, T)
    sh = prep_shared(inp)
    cs = _rope_tables(T)
    in_maps = []
    for core in range(NCORES):
        m = dict(sh)
        m["x"] = np.ascontiguousarray(inp["x"][core * NBC:(core + 1) * NBC].reshape(NBC * T, D))
        m["smalls"] = prep_smalls(inp, core)
        m["cs"] = cs
        in_maps.append(m)
    res = run_bass_kernel_spmd(nc, in_maps, core_ids=list(range(NCORES)))
    outs = [res.results[i]["out"].reshape(NBC, T, D) for i in range(NCORES)]
    return np.concatenate(outs, axis=0).astype(np.float32)
```

```python
import numpy as np
import concourse.bass as bass
import concourse.mybir as mybir
from concourse.bass_utils import run_bass_kernel_spmd


EPOCH = 24000


class _St:
    __slots__ = ("lw", "rd", "excl")

    def __init__(self, excl):
        self.lw = None
        self.rd = {}
        self.excl = excl


class Buf:
    __slots__ = ("ap", "st", "name")

    def __init__(self, ap, name="", excl=False, st=None):
        self.ap = ap
        self.st = st if st is not None else _St(excl)
        self.name = name

    def view(self, ap):
        return Buf(ap, self.name, st=self.st)

    @property
    def lw(self):
        return self.st.lw

    @lw.setter
    def lw(self, v):
        self.st.lw = v

    @property
    def rd(self):
        return self.st.rd

    @rd.setter
    def rd(self, v):
        self.st.rd = v

    @property
    def excl(self):
        return self.st.excl

    def __getitem__(self, k):
        return self.ap[k]


class Track:
    def __init__(self, fw, name, step):
        self.fw = fw
        self.name = name
        self.step = step
        self.epoch = 0
        self.idx = 0
        self.sems = [fw._new_sem(f"{name}_e0")]

    def next_token(self):
        if (self.idx + 1) * self.step > EPOCH:
            self.epoch += 1
            self.idx = 0
            self.sems.append(self.fw._new_sem(f"{self.name}_e{self.epoch}"))
        self.idx += 1
        return (self, self.epoch, self.idx)


class Engine:
    def __init__(self, fw, name, strict_self):
        self.fw = fw
        self.name = name
        self.track = Track(fw, name, 1)
        self.ops = []
        self.seen = {}
        self.strict_self = strict_self


class FW:
    def __init__(self, nc):
        self.nc = nc
        self._stack = []
        self._semcount = 0
        self.eng = {
            "pe": Engine(self, "pe", False),
            "act": Engine(self, "act", True),
            "dve": Engine(self, "dve", True),
            "pool": Engine(self, "pool", True),
            "sp": Engine(self, "sp", True),
        }
        self.lanes = {}
        self.n_inst = 0

    def _new_sem(self, name):
        cm = self.nc.semaphore(name)
        s = cm.__enter__()
        self._stack.append(cm)
        self._semcount += 1
        return s

    def sbuf(self, name, shape, dtype):
        cm = self.nc.sbuf_tensor(name, list(shape), dtype)
        t = cm.__enter__()
        self._stack.append(cm)
        return t

    def psum(self, name, shape, dtype):
        cm = self.nc.psum_tensor(name, list(shape), dtype)
        t = cm.__enter__()
        self._stack.append(cm)
        return t

    def lane_pool(self, name, n):
        self.lanes[name] = dict(tracks=[Track(self, f"{name}{i}", 16) for i in range(n)], rr=0)

    def _waits(self, engine, reads, writes):
        need = {}
        for b in reads:
            if b.lw is not None:
                k = (b.lw[0], b.lw[1])
                need[k] = max(need.get(k, 0), b.lw[2])
        for b in writes:
            if b.lw is not None:
                k = (b.lw[0], b.lw[1])
                need[k] = max(need.get(k, 0), b.lw[2])
            for k, i in b.rd.items():
                need[k] = max(need.get(k, 0), i)
        out = []
        for k, i in need.items():
            tr, ep = k
            if tr is engine.track and not engine.strict_self:
                continue
            if engine.seen.get(k, 0) >= i:
                continue
            engine.seen[k] = i
            out.append((tr.sems[ep], i * tr.step))
        return out

    def _commit(self, tok, reads, writes):
        k = (tok[0], tok[1])
        for b in reads:
            b.rd[k] = max(b.rd.get(k, 0), tok[2])
        for b in writes:
            b.lw = tok
            b.rd = {}

    def op(self, ename, fn, reads=(), writes=()):
        e = self.eng[ename]
        if any(b.excl for b in reads):
            writes = list(writes) + [b for b in reads if b.excl]
            reads = [b for b in reads if not b.excl]
        waits = self._waits(e, reads, writes)
        tok = e.track.next_token()
        sem = tok[0].sems[tok[1]]
        e.seen[(tok[0], tok[1])] = max(e.seen.get((tok[0], tok[1]), 0), 0)

        def run(eng, waits=waits, fn=fn, sem=sem):
            for s, v in waits:
                eng.wait_ge(s, v)
            fn(eng).then_inc(sem, 1)
        e.ops.append(run)
        self._commit(tok, reads, writes)
        self.n_inst += 1
        return tok

    def dma(self, ename, pool, out_ap, in_ap, reads=(), writes=(), **kw):
        e = self.eng[ename]
        lp = self.lanes[pool]
        tr = lp["tracks"][lp["rr"] % len(lp["tracks"])]
        lp["rr"] += 1
        waits = self._waits(e, reads, writes)
        if tr.idx > 0:
            k = (tr, tr.epoch)
            if e.seen.get(k, 0) < tr.idx:
                e.seen[k] = tr.idx
                waits.append((tr.sems[tr.epoch], tr.idx * 16))
        tok = tr.next_token()
        sem = tr.sems[tok[1]]

        def run(eng, waits=waits, sem=sem, out_ap=out_ap, in_ap=in_ap, kw=kw):
            for s, v in waits:
                eng.wait_ge(s, v)
            eng.dma_start(out=out_ap, in_=in_ap, **kw).then_inc(sem, 16)
        e.ops.append(run)
        self._commit(tok, reads, writes)
        self.n_inst += 1
        return tok

    def final_wait(self, ename, bufs):
        e = self.eng[ename]
        waits = self._waits(e, bufs, ())

        def run(eng, waits=waits):
            for s, v in waits:
                eng.wait_ge(s, v)
        e.ops.append(run)

    def emit(self):
        nc = self.nc
        with nc.Block() as block:
            @block.tensor
            def _(eng):
                for f in self.eng["pe"].ops:
                    f(eng)

            @block.scalar
            def _(eng):
                for f in self.eng["act"].ops:
                    f(eng)

            @block.vector
            def _(eng):
                for f in self.eng["dve"].ops:
                    f(eng)

            @block.gpsimd
            def _(eng):
                for f in self.eng["pool"].ops:
                    f(eng)

            @block.sync
            def _(eng):
                for f in self.eng["sp"].ops:
                    f(eng)

    def close(self):
        while self._stack:
            self._stack.pop().__exit__(None, None, None)


def _fence(fw):
    tracks = [e.track for e in fw.eng.values()]
    for lp in fw.lanes.values():
        tracks += lp["tracks"]
    for e in fw.eng.values():
        waits = []
        for tr in tracks:
            if tr is e.track or tr.idx == 0:
                continue
            k = (tr, tr.epoch)
            if e.seen.get(k, 0) >= tr.idx:
                continue
            e.seen[k] = tr.idx
            waits.append((tr.sems[tr.epoch], tr.idx * tr.step))

        def run(eng, waits=waits):
            for s, v in waits:
                eng.wait_ge(s, v)
        e.ops.append(run)


def _fence_compute(fw):
    names = ("pe", "act", "dve", "pool")
    for n in names:
        e = fw.eng[n]
        waits = []
        for n2 in names:
            tr = fw.eng[n2].track
            if n2 == n or tr.idx == 0:
                continue
            k = (tr, tr.epoch)
            if e.seen.get(k, 0) >= tr.idx:
                continue
            e.seen[k] = tr.idx
            waits.append((tr.sems[tr.epoch], tr.idx * tr.step))

        def run(eng, waits=waits):
            for s, v in waits:
                eng.wait_ge(s, v)
        e.ops.append(run)


F32 = mybir.dt.float32
BF16 = mybir.dt.bfloat16
AF = mybir.ActivationFunctionType
ALU = mybir.AluOpType

D = 2048
KC = 16
DFF = 5632
FC = 44
NBC = 2
NCORES = 8
EPS = 1e-6
LRU_C = 8.0

_SM = {}
_off = 0
for _n, _w in [("cT", 32), ("adab", 288), ("gpre", 96), ("gpost", 96), ("evconv", 96),
               ("alog", 8), ("dtb", 8), ("onorm", 128), ("retnorm", 1024),
               ("odconv", 64), ("odconvb", 16), ("gab", 16), ("gxb", 16), ("lam", 16),
               ("ident", 128), ("ones", 128), ("tri", 128), ("maskT", 128), ("smask", 128),
               ("decayT", 512), ("gq", 512), ("gk", 4)]:
    _SM[_n] = (_off, _w)
    _off += _w
NS = _off


FULL_PLAN = [("ffn", 0, 0, 0), ("gdn", 1), ("ffn", 0, 1, 2), ("ffn", 1, 0, 3), ("rglru", 4), ("ffn", 1, 1, 5)]


def build(plan=FULL_PLAN, T=2048):
    NTOK = NBC * T
    nc = bass.Bass("TRN2", target_bir_lowering=False)

    def din(name, shape, dt=F32):
        return nc.dram_tensor(name, list(shape), dt, kind="ExternalInput").ap()

    x_in = din("x", [NTOK, D])
    smalls_d = din("smalls", [128, NS])
    cs_d = din("cs", [128, 2, T])
    adaw_d = din("adaw", [2 * 144, 128, 2048])
    w13_d = din("w13r", [2 * 2 * FC, 128, 4096])
    w2_d = din("w2r", [2 * 2 * 16 * 2, 128, 22 * 128])
    evin_d = din("evin", [64, 128, 2048])
    evtail_d = din("evtail", [128, 256])
    evout_d = din("evout", [16, 128, 2048])
    odin_d = din("odin", [32, 128, 2048])
    odout_d = din("odout", [16, 128, 2048])
    gatew_d = din("gatew", [8, 128, 1024])
    out_d = nc.dram_tensor("out", [NTOK, D], F32, kind="ExternalOutput").ap()
    xs_d = nc.dram_tensor("xs", [KC, 128, NTOK], F32).ap()
    xs_v = xs_d.rearrange("k p t -> p k t")
    wcs_ffn = [nc.dram_tensor(f"wcs{i}", [128, 44 * 4096 + 32 * 2816], BF16).ap() for i in range(4)]
    wcs_mix = nc.dram_tensor("wcsm", [128, 64 * 2048 + 256 + 16 * 2048 + 32 * 2048 + 16 * 2048], BF16).ap()

    fw = FW(nc)
    fw_halo = fw.sbuf("halo", [128, 96], F32)
    fw_hst = fw.sbuf("hst", [128, 16], F32)
    fw_c1 = fw.sbuf("c1", [128, 32], F32)
    fw.lane_pool("w", 6)
    fw.lane_pool("wb", 6)
    fw.lane_pool("ws", 4)
    fw.lane_pool("a", 6)

    sm = fw.sbuf("sm", [128, NS], F32)
    smb = Buf(sm)

    def S(name, lo=0, hi=None):
        o, w = _SM[name]
        hi = w if hi is None else hi
        return sm[:, o + lo:o + hi]

    XYt = fw.sbuf("XY", [128, KC, 512], F32)
    XY = [Buf(XYt[:, k, :]) for k in range(KC)]
    hTt = fw.sbuf("hT", [128, KC, 512], BF16)
    hT = [Buf(hTt[:, k, :]) for k in range(KC)]
    BIG = fw.sbuf("BIG", [128, 11264], F32)
    WS = [Buf(fw.sbuf(f"ws{i}", [128, 4096], BF16)) for i in range(5)]
    wsi = [0]

    def wslot():
        b = WS[wsi[0] % len(WS)]
        wsi[0] += 1
        return b
    WSH = [Buf(WS[i][:, j * 2048:(j + 1) * 2048]) for i in range(len(WS)) for j in range(2)]
    wshi = [0]

    def wslot_h():
        b = WSH[wshi[0] % len(WSH)]
        wshi[0] += 1
        return b
    xch = [Buf(fw.sbuf(f"xch{i}", [128, 512], F32)) for i in range(2)]
    sqb = [Buf(fw.sbuf(f"sq{i}", [128, 512], F32)) for i in range(2)]
    tmpb = [Buf(fw.sbuf(f"tmp{i}", [128, 512], F32)) for i in range(3)]
    rstd = Buf(fw.sbuf("rstd", [128, 512], F32))
    modT = fw.sbuf("modT", [128, 2 * 144 * 2], F32)
    modTb = Buf(modT)
    Ab = fw.sbuf("Ab", [128, 6 * KC * 2], F32)
    Bb = fw.sbuf("Bb", [128, 6 * KC * 2], F32)
    Cb = fw.sbuf("Cb", [128, 6 * KC * 2], F32)
    ABCb = Buf(Ab)
    silc = Buf(fw.sbuf("silc", [128, 32], BF16))
    fws = [Buf(fw.sbuf(f"fws{i}", [128, 1024], F32)) for i in range(2)]
    gxb_ = [Buf(fw.sbuf(f"gx{i}", [128, 512], F32)) for i in range(4)]

    pst = [fw.psum(f"ps{i}", [128, 512], F32) for i in range(8)]
    BANK = [Buf(pst[i], excl=True) for i in range(8)]
    PSB = BANK[0:4]
    PSQ = [BANK[2 + i].view(pst[2 + i][:, 0:128]) for i in range(6)]
    pbi = [0]
    pqi = [0]

    def psb():
        b = PSB[pbi[0] % 4]
        pbi[0] += 1
        return b

    def psq():
        b = PSQ[pqi[0] % 6]
        pqi[0] += 1
        return b

    xsb = [Buf(None, f"xs{i}") for i in range(NTOK // 256)]
    outb = Buf(None, "out")

    rr = [0]

    def ew2():
        rr[0] += 1
        return "dve" if rr[0] % 2 else "act"

    def mm(ps, ps_ap, lhsT, rhs, start, stop, reads):
        fw.op("pe", lambda e: e.matmul(ps_ap, lhsT, rhs, start=start, stop=stop), reads=reads, writes=[ps])

    def tr(ps, ps_ap, in_ap, reads):
        fw.op("pe", lambda e: e.transpose(ps_ap, in_ap, S("ident")), reads=reads + [smb], writes=[ps])

    def act(out, in_, func, reads, writes, bias=None, scale=None):
        kw = {}
        if bias is not None:
            kw["bias"] = bias
        if scale is not None:
            kw["scale"] = scale
        fw.op("act", lambda e: e.activation(out=out, in_=in_, func=func, **kw), reads=reads, writes=writes)

    def copy(eng, out, in_, reads, writes):
        if eng == "act":
            act(out, in_, AF.Copy, reads, writes)
        else:
            fw.op(eng, lambda e: e.tensor_copy(out, in_), reads=reads, writes=writes)

    def ts(eng, out, in0, s1, s2, op0, op1, reads, writes):
        if op1 is None:
            fw.op(eng, lambda e: e.tensor_scalar(out, in0, s1, None, op0), reads=reads, writes=writes)
        else:
            fw.op(eng, lambda e: e.tensor_scalar(out, in0, s1, s2, op0, op1), reads=reads, writes=writes)

    def stt(eng, out, in0, sc, in1, op0, op1, reads, writes):
        fw.op(eng, lambda e: e.scalar_tensor_tensor(out, in0, sc, in1, op0, op1), reads=reads, writes=writes)

    def tt(eng, out, in0, in1, op, reads, writes):
        fw.op(eng, lambda e: e.tensor_tensor(out, in0, in1, op), reads=reads, writes=writes)

    def wload(slot, dst_ap, src_ap):
        fw.dma("pool", "w", dst_ap, src_ap, writes=[slot])

    wcache = {}
    wc_off = {}

    def wget(key, slot, ncols, src_ap):
        dst = slot[:, 0:ncols]
        if key in wcache:
            dap, dbuf = wcache[key]
            fw.dma("sp", "wb", dst, dap, reads=[dbuf], writes=[slot])
        else:
            wload(slot, dst, src_ap)
            if key[0] == "w13":
                gidx = key[1] * 2 + key[2]
            elif key[0] == "w2":
                gidx = key[1] // 32
            else:
                gidx = 4
            wt = wcs_ffn[gidx] if gidx < 4 else wcs_mix
            o = wc_off.get(gidx, 0)
            wc_off[gidx] = o + ncols
            dap = wt[:, o:o + ncols]
            dbuf = Buf(None, "wc")
            fw.dma("sp", "ws", dap, dst, reads=[slot], writes=[dbuf])
            wcache[key] = (dap, dbuf)

    fw.dma("sp", "a", sm[:, :], smalls_d[:, :], writes=[smb])
    act(silc[:], S("cT"), AF.Silu, [smb], [silc])
    def ada_q(l, q, slot):
        ps = psq()
        for k in range(KC):
            mm(ps, ps[:, 0:2], slot[:, k * 128:(k + 1) * 128], silc[:, k * 2:k * 2 + 2], k == 0, k == KC - 1, [slot, silc])
        o = (l * 144 + q) * 2
        oa = _SM["adab"][0] + l * 144 + q
        ts("dve", modT[:, o:o + 2], ps[:, 0:2], sm[:, oa:oa + 1], None, ALU.add, None, [ps, smb], [modTb])

    mv = modT[:, :].rearrange("p (l q b) -> p l q b", l=2, q=144)
    Av = Ab[:, :].rearrange("p (s k b) -> p s k b", s=6, k=KC)
    Bv = Bb[:, :].rearrange("p (s k b) -> p s k b", s=6, k=KC)
    Cv = Cb[:, :].rearrange("p (s k b) -> p s k b", s=6, k=KC)

    def abc(l):
        for s in range(3):
            ls = l * 3 + s
            resw = 1.0 if s == 1 else 0.5
            gp = S("gpre", ls * KC, (ls + 1) * KC)
            gq_ = S("gpost", ls * KC, (ls + 1) * KC)
            for b in range(NBC):
                sh = mv[:, l, (s * 3 + 0) * KC:(s * 3 + 1) * KC, b]
                sc = mv[:, l, (s * 3 + 1) * KC:(s * 3 + 2) * KC, b]
                ga = mv[:, l, (s * 3 + 2) * KC:(s * 3 + 3) * KC, b]
                stt("dve", Av[:, ls, :, b], sc, 1.0, gp, ALU.add, ALU.mult, [modTb, smb], [ABCb])
                copy("dve", Bv[:, ls, :, b], sh, [modTb], [ABCb])
                stt("dve", Cv[:, ls, :, b], ga, 1.0, gq_, ALU.add, ALU.mult, [modTb, smb], [ABCb])
                ts("dve", Cv[:, ls, :, b], Cv[:, ls, :, b], resw, None, ALU.mult, None, [ABCb], [ABCb])

    for q in range(144):
        slot = wslot()
        wload(slot, slot[:, 0:2048], adaw_d[q])
        ada_q(0, q, slot)
    abc(0)

    adaslots = [Buf(fws[i][:, :].bitcast(BF16)) for i in range(2)]

    def ada1_gen():
        def ld(q):
            sl = adaslots[q % 2]
            fw.dma("pool", "w", sl[:, 0:2048], adaw_d[144 + q], writes=[sl])
        ld(0)
        yield
        for q in range(144):
            if q + 1 < 144:
                ld(q + 1)
            ada_q(1, q, adaslots[q % 2])
            yield
        abc(1)
        yield
    ada1 = [ada1_gen()]

    def ada_pull(n):
        g = ada1[0]
        if g is None:
            return
        for _ in range(n):
            try:
                next(g)
            except StopIteration:
                ada1[0] = None
                return

    def ada_finish():
        while ada1[0] is not None:
            ada_pull(8)

    def Asc(tab, ls, k, b):
        return tab[:, ls, k, b:b + 1]

    def sumsq_rstd(srcs, TT, scale, eps):
        ps = psb()
        n = len(srcs)
        for k, (b, ap) in enumerate(srcs):
            sq = sqb[k % 2]
            act(sq[:, :TT], ap, AF.Square, [b], [sq])
            mm(ps, ps[:, :TT], S("ones"), sq[:, :TT], k == 0, k == n - 1, [sq, smb])
        act(rstd[:, :TT], ps[:, :TT], AF.Sqrt, [ps], [rstd], bias=eps, scale=scale)
        fw.op("dve", lambda e: e.reciprocal(rstd[:, :TT], rstd[:, :TT]), reads=[rstd], writes=[rstd])

    def prologue(ls, t0, TT, b):
        nb = TT // 256
        xbufs = [xsb[t0 // 256 + i] for i in range(nb)]
        fw.dma("sp", "a", XYt[:, :, :TT], xs_v[:, :, t0:t0 + TT], reads=xbufs, writes=XY)
        sumsq_rstd([(XY[k], XY[k][:, :TT]) for k in range(KC)], TT, 1.0 / D, EPS)
        for k in range(KC):
            tm = tmpb[k % 3]
            stt("dve", tm[:, :TT], XY[k][:, :TT], Asc(Av, ls, k, b), rstd[:, :TT], ALU.mult, ALU.mult, [XY[k], rstd, ABCb], [tm])
            act(hT[k][:, :TT], tm[:, :TT], AF.Identity, [tm, ABCb], [hT[k]], bias=Asc(Bv, ls, k, b))

    def epilogue(ls, t0, TT, b):
        nb = TT // 256
        xbufs = [xsb[t0 // 256 + i] for i in range(nb)]
        sumsq_rstd([(XY[k], XY[k][:, :TT]) for k in range(KC)], TT, 1.0 / D, EPS)
        for k in range(KC):
            xc = xch[k % 2]
            fw.dma("sp", "a", xc[:, :TT], xs_d[k, :, t0:t0 + TT], reads=xbufs, writes=[xc])
            tm = tmpb[k % 3]
            stt("dve", tm[:, :TT], XY[k][:, :TT], Asc(Cv, ls, k, b), rstd[:, :TT], ALU.mult, ALU.mult, [XY[k], rstd, ABCb], [tm])
            tt("dve", XY[k][:, :TT], tm[:, :TT], xc[:, :TT], ALU.add, [tm, xc], [XY[k]])
        fw.dma("sp", "a", xs_v[:, :, t0:t0 + TT], XYt[:, :, :TT], reads=XY, writes=xbufs)

    def proj_fm(wd_piece, TT, consume, key):
        slot = wslot_h()
        wget(key, slot, 2048, wd_piece)
        ps = psb()
        for k in range(KC):
            mm(ps, ps[:, :TT], slot[:, k * 128:(k + 1) * 128], hT[k][:, :TT], k == 0, k == KC - 1, [slot, hT[k]])
        consume(ps)

    xin = [Buf(BIG[:, i * 2048:(i + 1) * 2048]) for i in range(4)]
    for t0 in range(0, NTOK, 512):
        for blk in range(4):
            fw.dma("sp", "a", xin[blk][:], x_in[t0 + blk * 128:t0 + (blk + 1) * 128, :], writes=[xin[blk]])
        for k in range(KC):
            ps = psb()
            for blk in range(4):
                tr(ps, ps[:, blk * 128:(blk + 1) * 128], xin[blk][:, k * 128:(k + 1) * 128], [xin[blk]])
            copy(ew2(), XY[k][:], ps[:], [ps], [XY[k]])
        fw.dma("sp", "a", xs_v[:, :, t0:t0 + 512], XYt[:, :, :], reads=XY, writes=[xsb[t0 // 256], xsb[t0 // 256 + 1]])
    _fence(fw)

    def ffn_sandwich(l, f, ls):
        actT = BIG[:, :].bitcast(BF16)
        actb = [Buf(actT[:, j * 512:(j + 1) * 512]) for j in range(FC)]
        TT = 512
        xcb = xch
        sq_, acc_ = sqb
        rstdP = rstd
        tmP, tmE0, tmE1, rstdE = gxb_
        tmE = [tmE0, tmE1]

        def sumsq_acc(k, src_b, src_ap):
            act(sq_[:], src_ap, AF.Square, [src_b], [sq_])
            if k == 0:
                copy("dve", acc_[:], sq_[:], [sq_], [acc_])
            else:
                tt("dve", acc_[:], acc_[:], sq_[:], ALU.add, [acc_, sq_], [acc_])

        def finish_rstd(dst):
            ps = psb()
            mm(ps, ps[:], S("ones"), acc_[:], True, True, [acc_, smb])
            act(dst[:], ps[:], AF.Sqrt, [ps], [dst], bias=EPS, scale=1.0 / D)
            fw.op("dve", lambda e: e.reciprocal(dst[:], dst[:]), reads=[dst], writes=[dst])

        def pro_gen(t0):
            b = t0 // T
            xbufs = [xsb[t0 // 256], xsb[t0 // 256 + 1]]

            def ld(k):
                xc = xcb[k % 2]
                fw.dma("pool", "a", xc[:], xs_d[k, :, t0:t0 + TT], reads=xbufs, writes=[xc])
            ld(0)
            yield
            for k in range(KC):
                if k + 1 < KC:
                    ld(k + 1)
                sumsq_acc(k, xcb[k % 2], xcb[k % 2][:])
                yield
            ld(0)
            finish_rstd(rstdP)
            yield
            for k in range(KC):
                if k + 1 < KC:
                    ld(k + 1)
                xc = xcb[k % 2]
                stt("dve", tmP[:], xc[:], Asc(Av, ls, k, b), rstdP[:], ALU.mult, ALU.mult, [xc, rstdP, ABCb], [tmP])
                act(hT[k][:], tmP[:], AF.Identity, [tmP, ABCb], [hT[k]], bias=Asc(Bv, ls, k, b))
                yield

        def epi_gen(t0):
            b = t0 // T
            xbufs = [xsb[t0 // 256], xsb[t0 // 256 + 1]]

            def ld(k):
                xc = xcb[k % 2]
                fw.dma("pool", "a", xc[:], xs_d[k, :, t0:t0 + TT], reads=xbufs, writes=[xc])
            for k in range(KC):
                sumsq_acc(k, XY[k], XY[k][:])
                yield
            ld(0)
            finish_rstd(rstdE)
            yield
            for k in range(KC):
                if k + 1 < KC:
                    ld(k + 1)
                xc = xcb[k % 2]
                te = tmE[k % 2]
                stt("dve", te[:], XY[k][:], Asc(Cv, ls, k, b), rstdE[:], ALU.mult, ALU.mult, [XY[k], rstdE, ABCb], [te])
                tt("dve", te[:], te[:], xc[:], ALU.add, [te, xc], [te])
                fw.dma("pool", "a", xs_d[k, :, t0:t0 + TT], te[:], reads=[te], writes=xbufs)
                yield

        def pull(g, n):
            if g is None:
                return None
            for _ in range(n):
                try:
                    next(g)
                except StopIteration:
                    return None
            return g

        def drain(g):
            while g is not None:
                g = pull(g, 1)

        tiles = list(range(0, NTOK, TT))
        drain(pro_gen(tiles[0]))
        eg = None
        for ti, t0 in enumerate(tiles):
            for j in range(FC):
                slot = wslot()
                wget(("w13", l, f, j), slot, 4096, w13_d[(l * 2 + f) * FC + j])
                pg = psb()
                pu = psb()
                for k in range(KC):
                    mm(pg, pg[:], slot[:, k * 128:(k + 1) * 128], hT[k][:], k == 0, k == KC - 1, [slot, hT[k]])
                for k in range(KC):
                    mm(pu, pu[:], slot[:, 2048 + k * 128:2048 + (k + 1) * 128], hT[k][:], k == 0, k == KC - 1, [slot, hT[k]])
                tm = tmpb[j % 3]
                act(tm[:], pg[:], AF.Silu, [pg], [tm])
                tt("dve", actb[j][:], tm[:], pu[:], ALU.mult, [tm, pu], [actb[j]])
                eg = pull(eg, 1)
            drain(eg)
            pgn = pro_gen(tiles[ti + 1]) if ti + 1 < len(tiles) else None
            for m in range(KC):
                s0 = wslot()
                s1 = wslot()
                base = (((l * 2 + f) * 16) + m) * 2
                wget(("w2", base), s0, 2816, w2_d[base])
                wget(("w2", base + 1), s1, 2816, w2_d[base + 1])
                ps = psb()
                for c in range(FC):
                    sl = s0 if c < 22 else s1
                    cc = c % 22
                    mm(ps, ps[:], sl[:, cc * 128:(cc + 1) * 128], actb[c][:], c == 0, c == FC - 1, [sl, actb[c]])
                copy(ew2(), XY[m][:], ps[:], [ps], [XY[m]])
                pgn = pull(pgn, 3)
            drain(pgn)
            eg = epi_gen(t0)
        drain(eg)
        _fence(fw)

    def rglru_sandwich(ls):
        TT = 512
        ygt = BIG[:, 0:4096].bitcast(BF16)
        yg = [Buf(ygt[:, k * 512:(k + 1) * 512]) for k in range(KC)]
        xcv = [Buf(BIG[:, 4096 + i * 512:4096 + (i + 1) * 512]) for i in range(4)]
        xqb = [Buf(BIG[:, 6144 + i * 520:6144 + i * 520 + 515]) for i in range(2)]
        rb = [Buf(BIG[:, 7200 + i * 512:7200 + (i + 1) * 512]) for i in range(2)]
        ib = [Buf(BIG[:, 8224 + i * 512:8224 + (i + 1) * 512]) for i in range(2)]
        ab = [Buf(BIG[:, 9248 + i * 512:9248 + (i + 1) * 512]) for i in range(2)]
        hsb = [Buf(BIG[:, 10272:10784])]
        halo = Buf(fw_halo[:, 0:48])
        hstate = Buf(fw_hst[:, 0:16])
        c1 = Buf(fw_c1[:, 0:32])
        act(c1[:, 0:16], S("lam"), AF.Exp, [smb], [c1], scale=-1.0)
        act(c1[:, 0:16], c1[:, 0:16], AF.Ln, [c1], [c1], bias=1.0)
        ts("dve", c1[:, 16:32], c1[:, 0:16], LRU_C, None, ALU.mult, None, [c1], [c1])
        ts("dve", c1[:, 0:16], c1[:, 0:16], -LRU_C, None, ALU.mult, None, [c1], [c1])
        for t0 in range(0, NTOK, TT):
            b = t0 // T
            if t0 % T == 0:
                fw.op("dve", lambda e: e.memset(halo[:], 0.0), writes=[halo])
                fw.op("dve", lambda e: e.memset(hstate[:], 0.0), writes=[hstate])
            prologue(ls, t0, TT, b)
            def y_chunk(m):
                def cons(ps, m=m):
                    t1 = tmpb[0]
                    t2 = tmpb[1]
                    copy("act", t1[:], ps[:], [ps], [t1])
                    tt("pool", t2[:], t1[:], t1[:], ALU.mult, [t1], [t2])
                    ts("pool", t2[:], t2[:], 0.044715, 1.0, ALU.mult, ALU.add, [t2], [t2])
                    tt("pool", t2[:], t2[:], t1[:], ALU.mult, [t1, t2], [t2])
                    act(t2[:], t2[:], AF.Sigmoid, [t2], [t2], scale=1.5957691216057308)
                    tt("dve", yg[m][:], t1[:], t2[:], ALU.mult, [t1, t2], [yg[m]])
                proj_fm(odin_d[m], TT, cons, ("odin", m))

            def x_block(n):
                gsl = fws[n % 2]
                fw.dma("sp", "w", gsl[:, 0:1024], gatew_d[n], writes=[gsl])
                xcs = []
                for ic in range(2):
                    ci = n * 2 + ic
                    xq = xqb[ic]
                    xc = xcv[(n % 2) * 2 + ic]

                    def cons(ps, ci=ci, xq=xq, xc=xc):
                        copy("dve", xq[:, 0:3], halo[:, ci * 3:ci * 3 + 3], [halo], [xq])
                        copy("act", xq[:, 3:515], ps[:], [ps], [xq])
                        copy("dve", halo[:, ci * 3:ci * 3 + 3], xq[:, 512:515], [xq], [halo])
                        o = _SM["odconv"][0] + ci * 4
                        ob = _SM["odconvb"][0] + ci
                        ts("dve", xc[:], xq[:, 0:512], sm[:, o:o + 1], sm[:, ob:ob + 1], ALU.mult, ALU.add, [xq, smb], [xc])
                        for tap in range(1, 4):
                            stt("dve", xc[:], xq[:, tap:tap + 512], sm[:, o + tap:o + tap + 1], xc[:], ALU.mult, ALU.add, [xq, smb, xc], [xc])
                    proj_fm(odin_d[16 + ci], TT, cons, ("odin", 16 + ci))
                    xcs.append(xc)
                return gsl, xcs

            def gates(n, gsl, xcs):
                for jc in range(2):
                    cj = n * 2 + jc
                    pr = psb()
                    pi = psb()
                    for g, pp in ((0, pr), (1, pi)):
                        for ic in range(2):
                            o = ((g * 2 + jc) * 2 + ic) * 128
                            mm(pp, pp[:], gsl[:, o:o + 128], xcs[ic][:], ic == 0, ic == 1, [gsl, xcs[ic]])
                    r_ = rb[jc]
                    i_ = ib[jc]
                    a_ = ab[jc]
                    oga = _SM["gab"][0] + cj
                    ogx = _SM["gxb"][0] + cj
                    act(r_[:], pr[:], AF.Sigmoid, [pr, smb], [r_], bias=sm[:, oga:oga + 1])
                    act(i_[:], pi[:], AF.Sigmoid, [pi, smb], [i_], bias=sm[:, ogx:ogx + 1])
                    act(a_[:], r_[:], AF.Exp, [r_, c1], [a_], scale=c1[:, cj:cj + 1])
                    act(r_[:], r_[:], AF.Tanh, [r_, c1], [r_], scale=c1[:, 16 + cj:16 + cj + 1])
                    t2 = tmpb[2]
                    tt("dve", t2[:], a_[:], a_[:], ALU.mult, [a_], [t2])
                    stt("dve", t2[:], t2[:], 1.0, r_[:], ALU.add, ALU.mult, [t2, r_], [t2])
                    act(t2[:], t2[:], AF.Sqrt, [t2], [t2])
                    tt("pool", i_[:], i_[:], xcs[jc][:], ALU.mult, [i_, xcs[jc]], [i_])
                    tt("dve", t2[:], t2[:], i_[:], ALU.mult, [t2, i_], [t2])
                    hs = hsb[0]
                    fw.op("dve", lambda e, hs=hs, a_=a_, t2=t2, cj=cj: e.tensor_tensor_scan(hs[:], a_[:], t2[:], hstate[:, cj:cj + 1], ALU.mult, ALU.add),
                          reads=[a_, t2, hstate], writes=[hs])
                    copy("dve", hstate[:, cj:cj + 1], hs[:, 511:512], [hs], [hstate])
                    tt("dve", yg[cj][:], hs[:], yg[cj][:], ALU.mult, [hs, yg[cj]], [yg[cj]])

            pend = None
            for n in range(8):
                y_chunk(2 * n)
                y_chunk(2 * n + 1)
                cur = x_block(n)
                if pend is not None:
                    gates(*pend)
                pend = (n,) + cur
            gates(*pend)
            for m in range(KC):
                slot = wslot_h()
                wget(("odout", m), slot, 2048, odout_d[m])
                ps = psb()
                for k in range(KC):
                    mm(ps, ps[:], slot[:, k * 128:(k + 1) * 128], yg[k][:], k == 0, k == KC - 1, [slot, yg[k]])
                copy(ew2(), XY[m][:], ps[:], [ps], [XY[m]])
            epilogue(ls, t0, TT, b)
        _fence(fw)


    def gdn_sandwich(ls):
        TT = 256
        PE_ = ["dve"]
        _, gdec = _consts()
        AX = mybir.AxisListType.X
        oall = [Buf(BIG[:, blk * 2048:(blk + 1) * 2048]) for blk in range(2)]
        xq = [Buf(BIG[:, 4096 + i * 264:4096 + i * 264 + 259]) for i in range(3)]
        qkvS = [[Buf(BIG[:, 4888 + s * 768 + i * 256:4888 + s * 768 + (i + 1) * 256]) for i in range(3)] for s in range(2)]
        ktokS = [Buf(BIG[:, 6424 + s * 768:6424 + s * 768 + 256]) for s in range(2)]
        vtokS = [Buf(BIG[:, 6680 + s * 768:6680 + s * 768 + 256]) for s in range(2)]
        ztokS = [Buf(BIG[:, 6936 + s * 768:6936 + s * 768 + 256]) for s in range(2)]
        G = [Buf(BIG[:, 8192 + i * 128:8192 + (i + 1) * 128]) for i in range(24)]
        G += [Buf(XYt[:, k, 256 + j * 128:256 + (j + 1) * 128]) for k in range(KC) for j in range(2)]

        class _R:
            pass
        roles = {}
        gi = 0
        for s in range(2):
            for blk in range(2):
                C = _R()
                (C.P0, C.N0, C.Pa, C.Na, C.Xa, C.Xb, C.QKT, C.kegT, C.qstT, C.kst) = G[gi:gi + 10]
                gi += 10
                roles[(s, blk)] = C
        hroles = []
        for s in range(2):
            Hh = _R()
            (Hh.Rb, Hh.vnew, Hh.osq, Hh.ob) = G[gi:gi + 4]
            gi += 4
            hroles.append(Hh)
        scT, qin0, qin1 = G[gi:gi + 3]
        osq, ob = hroles[0].osq, hroles[0].ob
        rq = [Buf(BIG[:, 4096 + i * 256:4096 + (i + 1) * 256]) for i in range(2)]
        rk = [Buf(BIG[:, 4608 + i * 256:4608 + (i + 1) * 256]) for i in range(2)]
        rqr = [Buf(BIG[:, 5120 + i * 256:5120 + (i + 1) * 256]) for i in range(2)]
        rkr = [Buf(BIG[:, 5632 + i * 256:5632 + (i + 1) * 256]) for i in range(2)]
        rtm = [Buf(BIG[:, 6144 + i * 256:6144 + (i + 1) * 256]) for i in range(2)]
        rktok = Buf(BIG[:, 6656:7168])
        rvtok = Buf(BIG[:, 7168:7680])
        rgtok = Buf(BIG[:, 7680:8192])
        gsm = fw.sbuf("gsm", [128, 8 * 16 + 8], F32)
        gsmb = Buf(gsm)
        Sg_t = fw.sbuf("Sg", [128, 1024], F32)
        Sg = [Buf(Sg_t[:, h * 128:(h + 1) * 128]) for h in range(8)]
        Sr_t = fw.sbuf("Sr", [128, 2048], F32)
        Sr = [Buf(Sr_t[:, h * 512:(h + 1) * 512]) for h in range(4)]
        cst = fw.sbuf("cst", [128, 2, 256], F32)
        cstb = Buf(cst)
        halo = Buf(fw_halo[:, 0:72])
        ssb_t = fw.sbuf("ssb", [128, 4], F32)
        ssb = Buf(ssb_t[:, 0:2])
        ssbS = [Buf(ssb_t[:, 2 + s:3 + s]) for s in range(2)]

        def gs(i, blk, lo=0, hi=8):
            return gsm[:, (i * 2 + blk) * 8 + lo:(i * 2 + blk) * 8 + hi]
        NEGA = gsm[:, 128:136]
        act(NEGA, S("alog"), AF.Exp, [smb], [gsmb])
        ts("dve", NEGA, NEGA, -1.0, None, ALU.mult, None, [gsmb], [gsmb])

        def rowsum_rstd(ps_ap, width, src):
            act(osq[:, :] if width <= 128 else rtm[0][:, :width], ps_ap, AF.Square, [src], [osq if width <= 128 else rtm[0]])
            sqap = osq[:, :] if width <= 128 else rtm[0][:, :width]
            sqb_ = osq if width <= 128 else rtm[0]
            fw.op("dve", lambda e: e.reduce_sum(ssb[:, 0:1], sqap, AX), reads=[sqb_], writes=[ssb])
            act(ssb[:, 0:1], ssb[:, 0:1], AF.Sqrt, [ssb], [ssb], bias=EPS, scale=1.0 / width)
            fw.op("dve", lambda e: e.reciprocal(ssb[:, 0:1], ssb[:, 0:1]), reads=[ssb], writes=[ssb])


        def run_gens(gens):
            active = list(gens)
            while active:
                for g in list(active):
                    try:
                        next(g)
                    except StopIteration:
                        active.remove(g)

        def stage_a_gen(s, h):
            qkv = qkvS[s]
            for idx, m in enumerate((h, 8 + h, 16 + h)):
                def cons(ps, idx=idx, m=m):
                    x_ = xq[idx]
                    copy("dve", x_[:, 0:3], halo[:, m * 3:m * 3 + 3], [halo], [x_])
                    copy("act", x_[:, 3:259], ps[:, :TT], [ps], [x_])
                    copy("dve", halo[:, m * 3:m * 3 + 3], x_[:, 256:259], [x_], [halo])
                    o = _SM["evconv"][0] + m * 4
                    tm = tmpb[idx]
                    ts("dve", tm[:, :TT], x_[:, 0:TT], sm[:, o:o + 1], None, ALU.mult, None, [x_, smb], [tm])
                    for tap in range(1, 4):
                        stt("dve", tm[:, :TT], x_[:, tap:tap + TT], sm[:, o + tap:o + tap + 1], tm[:, :TT], ALU.mult, ALU.add, [x_, smb, tm], [tm])
                    act(qkv[idx][:], tm[:, :TT], AF.Silu, [tm], [qkv[idx]])
                proj_fm(evin_d[m], TT, cons, ("evin", m))
                yield
            qT, kT, vT = qkv
            sumsq_rstd([(qT, qT[:])], TT, 1.0, EPS)
            stt("dve", qT[:], qT[:], 128.0 ** -0.5, rstd[:, :TT], ALU.mult, ALU.mult, [qT, rstd], [qT])
            yield
            sumsq_rstd([(kT, kT[:])], TT, 1.0, EPS)
            tt("dve", kT[:], kT[:], rstd[:, :TT], ALU.mult, [kT, rstd], [kT])
            yield

        def stage_b(s, h):
            qT, kT, vT = qkvS[s]
            zsl = wslot_h()
            wget(("evin", 24 + h), zsl, 2048, evin_d[24 + h])
            ktok, vtok, ztok = ktokS[s], vtokS[s], ztokS[s]
            for blk in range(2):
                ps = psq()
                for k in range(KC):
                    mm(ps, ps[:], hT[k][:, blk * 128:(blk + 1) * 128], zsl[:, k * 128:(k + 1) * 128], k == 0, k == KC - 1, [zsl, hT[k]])
                act(ztok[:, blk * 128:(blk + 1) * 128], ps[:], AF.Silu, [ps], [ztok])
                pk_ = psq()
                tr(pk_, pk_[:], kT[:, blk * 128:(blk + 1) * 128], [kT])
                copy("act", ktok[:, blk * 128:(blk + 1) * 128], pk_[:], [pk_], [ktok])
                pv_ = psq()
                tr(pv_, pv_[:], vT[:, blk * 128:(blk + 1) * 128], [vT])
                copy("dve", vtok[:, blk * 128:(blk + 1) * 128], pv_[:], [pv_], [vtok])

        def chain_gen(s, blk, h):
            C = roles[(s, blk)]
            qT, kT, vT = qkvS[s]
            ktok = ktokS[s]
            cs_ = slice(blk * 128, (blk + 1) * 128)
            bcol = gs(0, blk, h, h + 1)
            ldcol = gs(1, blk, h, h + 1)
            gcol = gs(2, blk, h, h + 1)
            eglcol = gs(5, blk, h, h + 1)
            ldb, egb, DT = C.Pa, C.Na, C.Xb
            P0, N0, QKT, kegT, qstT, kst = C.P0, C.N0, C.QKT, C.kegT, C.qstT, C.kst
            ts("dve", ldb[:], S("ones"), ldcol, None, ALU.mult, None, [smb, gsmb], [ldb])
            pg = psq()
            mm(pg, pg[:], ldb[:], S("tri"), True, True, [ldb, smb])
            act(egb[:], pg[:], AF.Exp, [pg], [egb])
            stt("dve", DT[:], pg[:], gcol, S("maskT"), ALU.subtract, ALU.add, [pg, gsmb, smb], [DT])
            yield
            act(DT[:], DT[:], AF.Exp, [DT], [DT])
            pkk = psq()
            mm(pkk, pkk[:], kT[:, cs_], kT[:, cs_], True, True, [kT])
            pkq = psq()
            mm(pkq, pkq[:], kT[:, cs_], qT[:, cs_], True, True, [kT, qT])
            tt(PE_[0], kegT[:], kT[:, cs_], egb[:], ALU.mult, [kT, egb], [kegT])
            tt(PE_[0], qstT[:], qT[:, cs_], egb[:], ALU.mult, [qT, egb], [qstT])
            act(kst[:], ktok[:, cs_], AF.Copy, [ktok, gsmb], [kst], scale=eglcol)
            stt("dve", P0[:], pkk[:], bcol, DT[:], ALU.mult, ALU.mult, [pkk, gsmb, DT], [P0])
            tt("dve", QKT[:], pkq[:], DT[:], ALU.mult, [pkq, DT], [QKT])
            yield
            tt(PE_[0], P0[:], P0[:], S("smask"), ALU.mult, [P0, smb], [P0])
            pt = psq()
            tr(pt, pt[:], P0[:], [P0])
            copy("act", N0[:], pt[:], [pt], [N0])
            stt("dve", C.Xa[:], P0[:], -1.0, S("ident"), ALU.mult, ALU.add, [P0, smb], [C.Xa])
            yield
            X, Xo = C.Xa, C.Xb
            Pk, Nk = P0, N0
            Pn, Nn = C.Pa, C.Na
            for lvl in range(6):
                if lvl < 5:
                    pp = psq()
                    mm(pp, pp[:], Nk[:], Pk[:], True, True, [Nk, Pk])
                pn = psq()
                mm(pn, pn[:], Pk[:], Nk[:], True, True, [Nk, Pk])
                if lvl < 5:
                    copy("act", Pn[:], pp[:], [pp], [Pn])
                copy("dve", Nn[:], pn[:], [pn], [Nn])
                yield
                px = psq()
                mm(px, px[:], Nn[:], X[:], True, True, [Nn, X])
                tt("dve", Xo[:], X[:], px[:], ALU.add, [X, px], [Xo])
                yield
                X, Xo = Xo, X
                Pk, Nk, Pn, Nn = Pn, Nn, Pk, Nk
            C.X = X

        def sphase_gen(s, h):
            Hh = hroles[s]
            vtok, ztok = vtokS[s], ztokS[s]
            sb_ = ssbS[s]
            for blk in range(2):
                C = roles[(s, blk)]
                cs_ = slice(blk * 128, (blk + 1) * 128)
                bcol = gs(0, blk, h, h + 1)
                deccol = gs(6, blk, h, h + 1)
                pks = psq()
                mm(pks, pks[:], C.kegT[:], Sg[h][:], True, True, [C.kegT, Sg[h]])
                tt("dve", Hh.Rb[:], vtok[:, cs_], pks[:], ALU.subtract, [vtok, pks], [Hh.Rb])
                yield
                pv = psq()
                mm(pv, pv[:], C.X[:], Hh.Rb[:], True, True, [C.X, Hh.Rb])
                ts("dve", Hh.vnew[:], pv[:], bcol, None, ALU.mult, None, [pv, gsmb], [Hh.vnew])
                yield
                po = psq()
                mm(po, po[:], C.qstT[:], Sg[h][:], True, False, [C.qstT, Sg[h]])
                mm(po, po[:], C.QKT[:], Hh.vnew[:], False, True, [C.QKT, Hh.vnew])
                psn = psq()
                mm(psn, psn[:], C.kst[:], Hh.vnew[:], True, True, [C.kst, Hh.vnew])
                act(Hh.osq[:], po[:], AF.Square, [po], [Hh.osq])
                copy("dve", Hh.ob[:], po[:], [po], [Hh.ob])
                stt("dve", Sg[h][:], Sg[h][:], deccol, psn[:], ALU.mult, ALU.add, [Sg[h], gsmb, psn], [Sg[h]])
                yield
                fw.op("dve", lambda e, Hh=Hh, sb_=sb_: e.reduce_sum(sb_[:], Hh.osq[:], AX), reads=[Hh.osq], writes=[sb_])
                yield
                act(sb_[:], sb_[:], AF.Sqrt, [sb_], [sb_], bias=EPS, scale=1.0 / 128)
                yield
                fw.op("dve", lambda e, sb_=sb_: e.reciprocal(sb_[:], sb_[:]), reads=[sb_], writes=[sb_])
                yield
                stt("dve", Hh.ob[:], Hh.ob[:], sb_[:], S("onorm"), ALU.mult, ALU.mult, [Hh.ob, sb_, smb], [Hh.ob])
                yield
                tt(PE_[0], oall[blk][:, h * 128:(h + 1) * 128], Hh.ob[:], ztok[:, cs_], ALU.mult, [Hh.ob, ztok], [oall[blk]])
                yield


        class _RS:
            pass
        RS = []
        for s_ in range(2):
            if s_ == 0:
                pcs = [Buf(BIG[:, 4096 + i * 256:4096 + (i + 1) * 256]) for i in range(16)]
            else:
                pcs = [Buf(XYt[:, k, 256:512]) for k in range(KC)]
            R = _RS()
            R.rq, R.rk, R.rqr, R.rkr, R.rtm = pcs[0:2], pcs[2:4], pcs[4:6], pcs[6:8], pcs[8:10]
            R.rktok, R.rvtok, R.rgtok = pcs[10:12], pcs[12:14], pcs[14:16]
            R.scT, R.qin0, R.qin1 = G[s_ * 3:s_ * 3 + 3]
            RS.append(R)

        def ret_gen(h, R, sb_):
            for i in range(2):
                proj_fm(evin_d[32 + 2 * h + i], TT, lambda ps, i=i: copy("act", R.rq[i][:], ps[:, :TT], [ps], [R.rq[i]]), ("evin", 32 + 2 * h + i))
                yield
            for i in range(2):
                proj_fm(evin_d[40 + 2 * h + i], TT, lambda ps, i=i: copy("dve", R.rk[i][:], ps[:, :TT], [ps], [R.rk[i]]), ("evin", 40 + 2 * h + i))
                yield
            for src, dst in ((R.rq, R.rqr), (R.rk, R.rkr)):
                x1, x2 = src
                t0_, t1_ = R.rtm
                tt("dve", t0_[:], x1[:], cst[:, 0, :], ALU.mult, [x1, cstb], [t0_])
                tt(PE_[0], t1_[:], x2[:], cst[:, 1, :], ALU.mult, [x2, cstb], [t1_])
                tt("dve", dst[0][:], t0_[:], t1_[:], ALU.subtract, [t0_, t1_], [dst[0]])
                yield
                tt("dve", t0_[:], x2[:], cst[:, 0, :], ALU.mult, [x2, cstb], [t0_])
                tt(PE_[0], t1_[:], x1[:], cst[:, 1, :], ALU.mult, [x1, cstb], [t1_])
                tt("dve", dst[1][:], t0_[:], t1_[:], ALU.add, [t0_, t1_], [dst[1]])
                yield
            for base, dstl, fn in ((48, R.rvtok, None), (56, R.rgtok, AF.Silu)):
                sl = [wslot_h(), wslot_h()]
                for i in range(2):
                    wget(("evin", base + 2 * h + i), sl[i], 2048, evin_d[base + 2 * h + i])
                for blk in range(2):
                    ps = psb()
                    for i in range(2):
                        for k in range(KC):
                            mm(ps, ps[:, i * 128:(i + 1) * 128], hT[k][:, blk * 128:(blk + 1) * 128], sl[i][:, k * 128:(k + 1) * 128], k == 0, k == KC - 1, [sl[i], hT[k]])
                    if fn is None:
                        copy("dve", dstl[blk][:], ps[:, 0:256], [ps], [dstl[blk]])
                    else:
                        act(dstl[blk][:], ps[:, 0:256], fn, [ps], [dstl[blk]])
                    yield
            og = _SM["gk"][0] + h
            for blk in range(2):
                for i in range(2):
                    pk_ = psq()
                    tr(pk_, pk_[:], R.rkr[i][:, blk * 128:(blk + 1) * 128], [R.rkr[i]])
                    ts("dve", R.rktok[blk][:, i * 128:(i + 1) * 128], pk_[:], sm[:, og:og + 1], None, ALU.mult, None, [pk_, smb], [R.rktok[blk]])
                yield
            for blk in range(2):
                cs_ = slice(blk * 128, (blk + 1) * 128)
                vb_ = R.rvtok[blk]
                psc = psq()
                mm(psc, psc[:], R.rkr[0][:, cs_], R.rqr[0][:, cs_], True, False, [R.rkr[0], R.rqr[0]])
                mm(psc, psc[:], R.rkr[1][:, cs_], R.rqr[1][:, cs_], False, True, [R.rkr[1], R.rqr[1]])
                tt("dve", R.scT[:], psc[:], S("decayT", h * 128, (h + 1) * 128), ALU.mult, [psc, smb], [R.scT])
                tt(PE_[0], R.qin0[:], R.rqr[0][:, cs_], S("gq", h * 128, (h + 1) * 128), ALU.mult, [R.rqr[0], smb], [R.qin0])
                tt(PE_[0], R.qin1[:], R.rqr[1][:, cs_], S("gq", h * 128, (h + 1) * 128), ALU.mult, [R.rqr[1], smb], [R.qin1])
                yield
                po = psb()
                mm(po, po[:, 0:256], R.scT[:], vb_[:], True, False, [R.scT, vb_])
                mm(po, po[:, 0:256], R.qin0[:], Sr[h][:, 0:256], False, False, [R.qin0, Sr[h]])
                mm(po, po[:, 0:256], R.qin1[:], Sr[h][:, 256:512], False, True, [R.qin1, Sr[h]])
                for i in range(2):
                    ps = psb()
                    mm(ps, ps[:, 0:256], R.rktok[blk][:, i * 128:(i + 1) * 128], vb_[:], True, True, [R.rktok[blk], vb_])
                    stt("dve", Sr[h][:, i * 256:(i + 1) * 256], Sr[h][:, i * 256:(i + 1) * 256], gdec[h], ps[:, 0:256], ALU.mult, ALU.add, [Sr[h], ps], [Sr[h]])
                act(R.rtm[0][:], po[:, 0:256], AF.Square, [po], [R.rtm[0]])
                copy("dve", R.rtm[1][:], po[:, 0:256], [po], [R.rtm[1]])
                yield
                fw.op("dve", lambda e, R=R, sb_=sb_: e.reduce_sum(sb_[:], R.rtm[0][:], AX), reads=[R.rtm[0]], writes=[sb_])
                yield
                act(sb_[:], sb_[:], AF.Sqrt, [sb_], [sb_], bias=EPS, scale=1.0 / 256)
                yield
                fw.op("dve", lambda e, sb_=sb_: e.reciprocal(sb_[:], sb_[:]), reads=[sb_], writes=[sb_])
                yield
                stt("dve", R.rtm[1][:], R.rtm[1][:], sb_[:], S("retnorm", h * 256, (h + 1) * 256), ALU.mult, ALU.mult, [R.rtm[1], sb_, smb], [R.rtm[1]])
                yield
                tt(PE_[0], oall[blk][:, 1024 + h * 256:1024 + (h + 1) * 256], R.rtm[1][:], R.rgtok[blk][:], ALU.mult, [R.rtm[1], R.rgtok[blk]], [oall[blk]])
                yield

        for t0 in range(0, NTOK, TT):
            b = t0 // T
            tl = t0 % T
            PE_[0] = "pool" if t0 > 0 else "dve"
            if tl == 0:
                fw.op("dve", lambda e: e.memset(halo[:], 0.0), writes=[halo])
                fw.op("dve", lambda e: e.memset(Sg_t[:, :], 0.0), writes=Sg)
                fw.op("dve", lambda e: e.memset(Sr_t[:, :], 0.0), writes=Sr)
            prologue(ls, t0, TT, b)
            fw.dma("sp", "a", cst[:, :, :], cs_d[:, :, tl:tl + TT], writes=[cstb])
            tsl = wslot_h()
            wget(("evtail",), tsl, 256, evtail_d[:, :])
            for blk in range(2):
                ps = psq()
                for k in range(KC):
                    mm(ps, ps[:, 0:16], hT[k][:, blk * 128:(blk + 1) * 128], tsl[:, k * 16:(k + 1) * 16], k == 0, k == KC - 1, [tsl, hT[k]])
                act(gs(0, blk), ps[:, 0:8], AF.Sigmoid, [ps], [gsmb])
                tt("dve", gs(7, blk), ps[:, 8:16], S("dtb"), ALU.add, [ps, smb], [gsmb])
                act(gs(7, blk), gs(7, blk), AF.Exp, [gsmb], [gsmb])
                act(gs(7, blk), gs(7, blk), AF.Ln, [gsmb], [gsmb], bias=1.0)
                tt("dve", gs(1, blk), gs(7, blk), NEGA, ALU.mult, [gsmb], [gsmb])
                p2 = psq()
                mm(p2, p2[:, 0:8], S("tri"), gs(1, blk), True, True, [smb, gsmb])
                copy("dve", gs(2, blk), p2[:, 0:8], [p2], [gsmb])
                p3 = psq()
                mm(p3, p3[:, 0:8], S("ones"), gs(1, blk), True, True, [smb, gsmb])
                copy("dve", gs(3, blk), p3[:, 0:8], [p3], [gsmb])
                act(gs(4, blk), gs(2, blk), AF.Exp, [gsmb], [gsmb])
                tt("dve", gs(5, blk), gs(3, blk), gs(2, blk), ALU.subtract, [gsmb], [gsmb])
                act(gs(5, blk), gs(5, blk), AF.Exp, [gsmb], [gsmb])
                act(gs(6, blk), gs(3, blk), AF.Exp, [gsmb], [gsmb])
            run_gens([stage_a_gen(s, s) for s in range(2)])
            for hp in range(4):
                if t0 > 0:
                    ada_pull(3)
                for s in range(2):
                    stage_b(s, 2 * hp + s)
                run_gens([chain_gen(s, blk, 2 * hp + s) for s in range(2) for blk in range(2)])
                gl = [sphase_gen(s, 2 * hp + s) for s in range(2)]
                if hp < 3:
                    gl += [stage_a_gen(s, 2 * (hp + 1) + s) for s in range(2)]
                run_gens(gl)
            _fence_compute(fw)
            for hp in range(2):
                run_gens([ret_gen(2 * hp + s, RS[s], ssbS[s]) for s in range(2)])
            for blk in range(2):
                for k in range(KC):
                    ps = psq()
                    tr(ps, ps[:], oall[blk][:, k * 128:(k + 1) * 128], [oall[blk]])
                    copy(ew2(), hT[k][:, blk * 128:(blk + 1) * 128], ps[:], [ps], [hT[k]])
            for m in range(KC):
                proj_fm(evout_d[m], TT, lambda ps, m=m: copy(ew2(), XY[m][:, :TT], ps[:, :TT], [ps], [XY[m]]), ("evout", m))
            epilogue(ls, t0, TT, b)
            _fence_compute(fw)
        _fence(fw)

    for item in plan:
        if item[0] == "ffn":
            if item[1] == 1:
                ada_finish()
            ffn_sandwich(item[1], item[2], item[3])
        elif item[0] == "rglru":
            ada_finish()
            rglru_sandwich(item[1])
        elif item[0] == "gdn":
            gdn_sandwich(item[1])

    ot = [Buf(BIG[:, i * 2048:(i + 1) * 2048]) for i in range(4)]
    for t0 in range(0, NTOK, 512):
        fw.dma("sp", "a", XYt[:, :, :], xs_v[:, :, t0:t0 + 512], reads=[xsb[t0 // 256], xsb[t0 // 256 + 1]], writes=XY)
        for blk in range(4):
            for q in range(4):
                ps = psb()
                for kk in range(4):
                    k = q * 4 + kk
                    tr(ps, ps[:, kk * 128:(kk + 1) * 128], XY[k][:, blk * 128:(blk + 1) * 128], [XY[k]])
                copy(ew2(), ot[blk][:, q * 512:(q + 1) * 512], ps[:], [ps], [ot[blk]])
            fw.dma("sp", "a", out_d[t0 + blk * 128:t0 + (blk + 1) * 128, :], ot[blk][:], reads=[ot[blk]], writes=[outb])
    fw.final_wait("sp", [outb])
    fw.emit()
    fw.close()
    return nc


def _consts():
    f8 = np.float64
    i = np.arange(128)
    ident = np.eye(128)
    ones = np.ones((128, 128))
    tri = (i[:, None] <= i[None, :]).astype(f8)
    maskT = np.where(i[None, :] >= i[:, None], 0.0, -1e30)
    smask = (i[None, :] > i[:, None]).astype(f8)
    lg = np.log1p(-np.exp2(-5.0 - np.arange(4, dtype=np.float32))).astype(np.float32).astype(f8)
    decayT = np.zeros((128, 4, 128))
    gq = np.zeros((128, 4, 128))
    gk = np.zeros((128, 4))
    for h in range(4):
        dd = (i[None, :] - i[:, None]).astype(f8)
        decayT[:, h, :] = np.where(dd >= 0, np.exp(dd * lg[h]), 0.0) * (256.0 ** -0.5)
        gq[:, h, :] = np.exp((i[None, :] + 1.0) * lg[h])
        gk[:, h] = np.exp((127.0 - i) * lg[h]) * (256.0 ** -0.5)
    gdec = [float(np.exp(128.0 * lg[h])) for h in range(4)]
    return dict(ident=ident, ones=ones, tri=tri, maskT=maskT, smask=smask,
                decayT=decayT.reshape(128, 512), gq=gq.reshape(128, 512), gk=gk), gdec


def _rope_tables(T):
    half = 128
    inv_freq = (np.float32(10000.0) ** (-np.arange(half, dtype=np.float32) / np.float32(half))).astype(np.float32)
    pos = np.arange(T, dtype=np.float32)
    ang = (inv_freq[:, None] * pos[None, :]).astype(np.float32)
    cs = np.stack([np.cos(ang.astype(np.float64)), np.sin(ang.astype(np.float64))], axis=1)
    return np.ascontiguousarray(cs.astype(np.float32))


def _proj_layout(W):
    n = W.shape[1] // 128
    return np.ascontiguousarray(W.reshape(16, 128, n, 128).transpose(2, 1, 0, 3).reshape(n, 128, 2048))


def _pcol(v, n):
    return v.reshape(n, 128).T


def prep_shared(inp):
    ada_w = inp["ada_w"]
    sh = {}
    sh["adaw"] = np.ascontiguousarray(ada_w.reshape(2, 16, 128, 144, 128).transpose(0, 3, 2, 1, 4).reshape(288, 128, 2048))
    sh["w13r"] = np.ascontiguousarray(inp["ffn_w13"].reshape(2, 2, 16, 128, 2, 44, 128).transpose(0, 1, 5, 3, 4, 2, 6).reshape(176, 128, 4096))
    sh["w2r"] = np.ascontiguousarray(inp["ffn_w2"].reshape(2, 2, 2, 22, 128, 16, 128).transpose(0, 1, 5, 2, 4, 3, 6).reshape(128, 128, 2816))
    evw = inp["ev_w_in"][0]
    sh["evin"] = _proj_layout(np.concatenate([evw[:, :3072], evw[:, 3088:]], axis=1))
    sh["evtail"] = np.ascontiguousarray(evw[:, 3072:3088].reshape(16, 128, 16).transpose(1, 0, 2).reshape(128, 256))
    sh["evout"] = _proj_layout(inp["ev_w_out"][0])
    sh["odin"] = _proj_layout(inp["od_w_in"][0])
    sh["odout"] = _proj_layout(inp["od_w_out"][0])
    g = np.stack([inp["od_gate_a_w"][0], inp["od_gate_x_w"][0]], axis=0)
    g = g.reshape(2, 8, 2, 128, 2, 128)
    sh["gatew"] = np.ascontiguousarray(g.transpose(1, 3, 0, 4, 2, 5).reshape(8, 128, 1024))
    return sh


def prep_smalls(inp, core):
    consts, _ = _consts()
    sm = np.zeros((128, NS), np.float32)

    def put(name, arr):
        o, w = _SM[name]
        arr = np.asarray(arr, np.float32).reshape(128, w)
        sm[:, o:o + w] = arr
    c = inp["c"][core * NBC:(core + 1) * NBC]
    put("cT", c.reshape(NBC, 16, 128).transpose(2, 1, 0))
    put("adab", inp["ada_b"].reshape(2, 144, 128).transpose(2, 0, 1))
    put("gpre", inp["norm_pre"].reshape(2, 3, 16, 128).transpose(3, 0, 1, 2))
    put("gpost", inp["norm_post"].reshape(2, 3, 16, 128).transpose(3, 0, 1, 2))
    put("evconv", inp["ev_conv_w"][0].reshape(4, 24, 128).transpose(2, 1, 0))
    put("alog", np.broadcast_to(inp["ev_a_log"][0][None, :], (128, 8)))
    put("dtb", np.broadcast_to(inp["ev_dt_bias"][0][None, :], (128, 8)))
    put("onorm", np.broadcast_to(inp["ev_o_norm"][0][None, :], (128, 128)))
    put("retnorm", np.broadcast_to(inp["ev_ret_norm"][0][None, :], (128, 1024)))
    put("odconv", inp["od_conv_w"][0].reshape(4, 16, 128).transpose(2, 1, 0))
    put("odconvb", _pcol(inp["od_conv_b"][0], 16))
    put("gab", _pcol(inp["od_gate_a_b"][0], 16))
    put("gxb", _pcol(inp["od_gate_x_b"][0], 16))
    put("lam", _pcol(inp["od_lambda"][0], 16))
    for k, v in consts.items():
        put(k, v)
    return sm


def kernel(**inputs):
    inp = {k: np.asarray(v) for k, v in inputs.items()}
    T = inp["x"].shape[1]
    nc = build(FULL_PLAN, T)
    sh = prep_shared(inp)
    cs = _rope_tables(T)
    in_maps = []
    for core in range(NCORES):
        m = dict(sh)
        m["x"] = np.ascontiguousarray(inp["x"][core * NBC:(core + 1) * NBC].reshape(NBC * T, D))
        m["smalls"] = prep_smalls(inp, core)
        m["cs"] = cs
        in_maps.append(m)
    res = run_bass_kernel_spmd(nc, in_maps, core_ids=list(range(NCORES)))
    outs = [res.results[i]["out"].reshape(NBC, T, D) for i in range(NCORES)]
    return np.concatenate(outs, axis=0).astype(np.float32)
```
